# Optimizing a Trainium2 kernel written in Bass

```python
import jax, jax.numpy as jnp
from jax import lax
import numpy as np

D_MODEL = 1024
BATCH = 2
SEQ = 8192
DEPTH = 1

GRID_W = 64
CTX_LEN = 256
HG_HEADS = 8
HG_DK = 128
HG_DV = 128
HG_WIDTH = HG_HEADS * HG_DK
HG_CHUNK = 64
NA_HEADS = 16
NA_HD = 64
NA_WIDTH = NA_HEADS * NA_HD
NA_WIN_R = 8
NA_WIN_C = 16
NA_QCOLS = 16
NA_KCOLS = 32
ROPE_THETA = 10000.0
NEG_INF = -1e30
N_EXPERTS = 64
EXPERT_DIM = 256
TOP_K = 8
N_GROUPS = 8
TOPK_GROUPS = 4
ROUTED_SCALE = 2.5
SHARED_DIM = 256
MOE_BLOCK = 128
LN_EPS = 1e-6
IN_SPLITS = (HG_WIDTH, HG_WIDTH, HG_WIDTH, HG_WIDTH, HG_WIDTH, NA_WIDTH, NA_WIDTH, NA_WIDTH, D_MODEL, D_MODEL)
IN_WIDTH = 5 * HG_WIDTH + 3 * NA_WIDTH + 2 * D_MODEL

kernel_name = "hybrid_hgrn2_natten_moe_dit_block"


def _normalize(x):
    xf = x.astype(jnp.float32)
    mu = jnp.mean(xf, axis=-1, keepdims=True)
    var = jnp.mean(jnp.square(xf - mu), axis=-1, keepdims=True)
    return (xf - mu) * lax.rsqrt(var + LN_EPS)


def layer_norm(x, g, b):
    return (_normalize(x) * g.astype(jnp.float32) + b.astype(jnp.float32)).astype(x.dtype)


def modulate(x, shift, scale):
    return (_normalize(x) * (1.0 + scale.astype(jnp.float32)) + shift.astype(jnp.float32)).astype(x.dtype)


def split_cols(p):
    offs = np.cumsum(IN_SPLITS)[:-1].tolist()
    return jnp.split(p, offs, axis=-1)


def to_heads(t, n_heads):
    b, s, w = t.shape
    return t.reshape(b, s, n_heads, w // n_heads).transpose(0, 2, 1, 3)


def from_heads(t):
    b, h, s, d = t.shape
    return t.transpose(0, 2, 1, 3).reshape(b, s, h * d)


def gla_chunked(q, k, v, log_f, s0):
    b, h, t, dk = k.shape
    dv = v.shape[-1]
    n = t // HG_CHUNK
    ch = lambda u: u.reshape(b, h, n, HG_CHUNK, u.shape[-1])
    k, v, log_f = ch(k), ch(v), ch(log_f)
    a = jnp.cumsum(log_f, axis=3)
    a_last = a[:, :, :, -1:, :]
    u = jnp.einsum('bhnck,bhncv->nbhkv', k * jnp.exp(a_last - a), v)
    decay = jnp.moveaxis(jnp.exp(a_last[:, :, :, 0, :]), 2, 0)

    def step(s, inp):
        d, uc = inp
        return d[..., None] * s + uc, s

    s_final, s_prev = lax.scan(step, s0, (decay, u))
    if q is None:
        return None, s_final
    q = ch(q)
    qa = q * jnp.exp(a)
    kb = k * jnp.exp(-a)
    scores = jnp.einsum('bhnik,bhnjk->bhnij', qa, kb)
    causal = jnp.tril(jnp.ones((HG_CHUNK, HG_CHUNK), dtype=bool))
    scores = jnp.where(causal, scores, 0.0)
    o = jnp.einsum('bhnij,bhnjv->bhniv', scores, v) + jnp.einsum('bhnik,nbhkv->bhniv', qa, s_prev)
    return o.reshape(b, h, t, dv), s_final


def hgrn2_bidir(q, i, f_fwd, f_bwd, lb_fwd, lb_bwd, s_fwd0, s_bwd0, with_output):
    def gates(f_raw, lb):
        lb = lb[None, :, None, :]
        f = lb + (1.0 - lb) * jax.nn.sigmoid(f_raw)
        return 1.0 - f, jnp.log(f)

    rev = lambda t: jnp.flip(t, axis=2)
    k_f, lf_f = gates(f_fwd, lb_fwd)
    k_b, lf_b = gates(f_bwd, lb_bwd)
    qs = jax.nn.silu(q) if with_output else None
    o_f, s_f = gla_chunked(qs, k_f, i, lf_f, s_fwd0)
    o_b, s_b = gla_chunked(rev(qs) if with_output else None, rev(k_b), rev(i), rev(lf_b), s_bwd0)
    o = (o_f + rev(o_b)) if with_output else None
    return o, s_f, s_b


def hgrn2_readout(o, og, g):
    on = o * lax.rsqrt(jnp.mean(jnp.square(o), axis=-1, keepdims=True) + LN_EPS)
    on = from_heads(on) * g.astype(jnp.float32)
    return (on * jax.nn.silu(og.astype(jnp.float32))).astype(og.dtype)


def axial_rope(t):
    s, hd = t.shape[2], t.shape[3]
    half = hd // 2
    pos = jnp.arange(s)
    row = (pos // GRID_W).astype(jnp.float32)
    col = (pos % GRID_W).astype(jnp.float32)
    inv = jnp.power(ROPE_THETA, -jnp.arange(0, half, 2, dtype=jnp.float32) / half)

    def rot(u, p):
        ang = p[:, None] * inv[None, :]
        cos = jnp.cos(ang).astype(u.dtype)
        sin = jnp.sin(ang).astype(u.dtype)
        u1, u2 = jnp.split(u, 2, axis=-1)
        return jnp.concatenate([u1 * cos - u2 * sin, u1 * sin + u2 * cos], axis=-1)

    return jnp.concatenate([rot(t[..., :half], row), rot(t[..., half:], col)], axis=-1)


def na_indices(rows):
    wr = min(NA_WIN_R, rows)
    r = np.arange(rows)
    row_start = np.clip(r - wr // 2, 0, rows - wr)
    key_rows = row_start[:, None] + np.arange(wr)[None, :]
    ng = GRID_W // NA_QCOLS
    g = np.arange(ng)
    blk_start = np.clip(g * NA_QCOLS - NA_WIN_C // 2, 0, GRID_W - NA_KCOLS)
    key_cols = blk_start[:, None] + np.arange(NA_KCOLS)[None, :]
    qcol = g[:, None] * NA_QCOLS + np.arange(NA_QCOLS)[None, :]
    col_start = np.clip(qcol - NA_WIN_C // 2, 0, GRID_W - NA_WIN_C)
    in_win = (key_cols[:, None, :] >= col_start[:, :, None]) & (key_cols[:, None, :] < col_start[:, :, None] + NA_WIN_C)
    mask = np.broadcast_to(in_win[:, :, None, :], (ng, NA_QCOLS, wr, NA_KCOLS)).reshape(ng, NA_QCOLS, wr * NA_KCOLS)
    flat = (key_rows[:, None, :, None] * GRID_W + key_cols[None, :, None, :]).reshape(rows, ng, wr * NA_KCOLS)
    dr = key_rows - r[:, None] + (NA_WIN_R - 1)
    dc = np.clip(key_cols[:, None, :] - qcol[:, :, None] + (NA_WIN_C - 1), 0, 2 * NA_WIN_C - 2)
    return wr, ng, flat, mask, dr, dc


def neighborhood_attention(q_rot, k_rot, v, q, k_ctx, v_ctx, rpb):
    b, h, s, hd = q.shape
    rows = s // GRID_W
    wr, ng, flat, mask, dr, dc = na_indices(rows)
    nk = wr * NA_KCOLS
    idx = jnp.asarray(flat.reshape(-1))
    k_blk = jnp.take(k_rot, idx, axis=2).reshape(b, h, rows, ng, nk, hd)
    v_blk = jnp.take(v, idx, axis=2).reshape(b, h, rows, ng, nk, hd)
    qr = q_rot.reshape(b, h, rows, ng, NA_QCOLS, hd)
    qp = q.reshape(b, h, rows, ng, NA_QCOLS, hd)
    scale = NA_HD ** -0.5
    bias = rpb[:, dr[:, None, None, :, None], dc[None, :, :, None, :]]
    bias = bias.reshape(h, rows, ng, NA_QCOLS, nk).astype(jnp.float32)
    s_win = jnp.einsum('bhrgqd,bhrgkd->bhrgqk', qr, k_blk, preferred_element_type=jnp.float32) * scale + bias
    s_win = jnp.where(jnp.asarray(mask)[None, None, None], s_win, NEG_INF)
    s_ctx = jnp.einsum('bhrgqd,bhcd->bhrgqc', qp, k_ctx, preferred_element_type=jnp.float32) * scale
    p = jax.nn.softmax(jnp.concatenate([s_win, s_ctx], axis=-1), axis=-1).astype(v.dtype)
    o = (jnp.einsum('bhrgqk,bhrgkd->bhrgqd', p[..., :nk], v_blk)
         + jnp.einsum('bhrgqc,bhcd->bhrgqd', p[..., nk:], v_ctx))
    return o.reshape(b, h, s, hd)


def context_attention(q, k, v):
    s = jnp.einsum('bhqd,bhkd->bhqk', q, k, preferred_element_type=jnp.float32) * NA_HD ** -0.5
    p = jax.nn.softmax(s, axis=-1).astype(v.dtype)
    return jnp.einsum('bhqk,bhkd->bhqd', p, v)


def token_mixer(h, hc, w_in, lb_fwd, lb_bwd, hg_norm_g, rpb, w_branch_a, w_branch_b, w_out, ctx_out):
    b = h.shape[0]
    q_hg, ff, fb, i_hg, og, q_na, k_na, v_na, g_a, g_b = split_cols(h @ w_in)
    cq_hg, cff, cfb, ci_hg, cog, cq_na, ck_na, cv_na, cg_a, cg_b = split_cols(hc @ w_in)
    hh = lambda t: to_heads(t, HG_HEADS).astype(jnp.float32)
    s0 = jnp.zeros((b, HG_HEADS, HG_DK, HG_DV), jnp.float32)
    o_c, s_cf, s_cb = hgrn2_bidir(hh(cq_hg), hh(ci_hg), hh(cff), hh(cfb), lb_fwd, lb_bwd, s0, s0, ctx_out)
    o, _, _ = hgrn2_bidir(hh(q_hg), hh(i_hg), hh(ff), hh(fb), lb_fwd, lb_bwd, s_cf, s_cb, True)
    y_hg = hgrn2_readout(o, og, hg_norm_g)
    na = lambda t: to_heads(t, NA_HEADS)
    qn, kn, vn = na(q_na), na(k_na), na(v_na)
    kc, vc = na(ck_na), na(cv_na)
    y_na = from_heads(neighborhood_attention(axial_rope(qn), axial_rope(kn), vn, qn, kc, vc, rpb))
    y = (jax.nn.sigmoid(g_a) * (y_hg @ w_branch_a) + jax.nn.sigmoid(g_b) * (y_na @ w_branch_b)) @ w_out
    if not ctx_out:
        return y, None
    yc_hg = hgrn2_readout(o_c, cog, hg_norm_g)
    yc_na = from_heads(context_attention(na(cq_na), kc, vc))
    yc = (jax.nn.sigmoid(cg_a) * (yc_hg @ w_branch_a) + jax.nn.sigmoid(cg_b) * (yc_na @ w_branch_b)) @ w_out
    return y, yc


def moe_ffn(h, w_router, router_bias, w_e_gate, w_e_up, w_e_down, w_sh_gate, w_sh_up, w_sh_down):
    tok = h.reshape(-1, MOE_BLOCK, h.shape[-1])

    def block(xb):
        scores = jax.nn.sigmoid(jnp.matmul(xb, w_router, preferred_element_type=jnp.float32))
        sel = scores + router_bias.astype(jnp.float32)
        grp = sel.reshape(-1, N_GROUPS, N_EXPERTS // N_GROUPS)
        grp_score = jnp.sum(lax.top_k(grp, 2)[0], axis=-1)
        _, top_g = lax.top_k(grp_score, TOPK_GROUPS)
        gmask = jnp.sum(jax.nn.one_hot(top_g, N_GROUPS, dtype=jnp.float32), axis=1)
        emask = jnp.repeat(gmask, N_EXPERTS // N_GROUPS, axis=-1)
        sel = jnp.where(emask > 0, sel, -jnp.inf)
        _, top_e = lax.top_k(sel, TOP_K)
        w = jnp.take_along_axis(scores, top_e, axis=-1)
        w = w / jnp.sum(w, axis=-1, keepdims=True) * ROUTED_SCALE
        gates = jnp.sum(jax.nn.one_hot(top_e, N_EXPERTS, dtype=jnp.float32) * w[..., None], axis=1)
        a = jnp.einsum('td,edf->tef', xb, w_e_gate)
        u = jnp.einsum('td,edf->tef', xb, w_e_up)
        act = jax.nn.silu(a) * u * gates[..., None].astype(xb.dtype)
        routed = jnp.einsum('tef,efd->td', act, w_e_down)
        shared = (jax.nn.silu(xb @ w_sh_gate) * (xb @ w_sh_up)) @ w_sh_down
        return routed + shared

    return lax.map(block, tok).reshape(h.shape)


def setup_inputs(seed: int = 0) -> dict:
    key = jax.random.key(seed)
    ks = jax.random.split(key, 26)
    f32 = jnp.float32
    beta = (8.0 * DEPTH) ** -0.25
    nrm = lambda k, shape, s: jax.random.normal(k, shape, f32) * s
    col_scale = jnp.ones((IN_WIDTH,), f32)
    col_scale = col_scale.at[3 * HG_WIDTH:4 * HG_WIDTH].set(beta)
    col_scale = col_scale.at[5 * HG_WIDTH + 2 * NA_WIDTH:5 * HG_WIDTH + 3 * NA_WIDTH].set(beta)
    return {
        "x": nrm(ks[0], (BATCH, SEQ, D_MODEL), 1.0),
        "c": nrm(ks[1], (BATCH, D_MODEL), 1.0),
        "ctx": nrm(ks[2], (BATCH, CTX_LEN, D_MODEL), 1.0),
        "c_ctx": nrm(ks[3], (D_MODEL,), 1.0),
        "w_ada": nrm(ks[4], (DEPTH, D_MODEL, 6 * D_MODEL), 0.5 * D_MODEL ** -0.5),
        "b_ada": nrm(ks[5], (DEPTH, 6 * D_MODEL), 0.02),
        "w_in": nrm(ks[6], (DEPTH, D_MODEL, IN_WIDTH), D_MODEL ** -0.5) * col_scale,
        "hg_lb_fwd": nrm(ks[7], (DEPTH + 1, HG_WIDTH), 0.1),
        "hg_lb_bwd": nrm(ks[8], (DEPTH + 1, HG_WIDTH), 0.1),
        "hg_norm_g": 1.0 + nrm(ks[9], (DEPTH, HG_WIDTH), 0.02),
        "na_rpb": nrm(ks[10], (DEPTH, NA_HEADS, 2 * NA_WIN_R - 1, 2 * NA_WIN_C - 1), 0.02),
        "w_branch_a": nrm(ks[11], (DEPTH, HG_WIDTH, D_MODEL), HG_WIDTH ** -0.5),
        "w_branch_b": nrm(ks[12], (DEPTH, NA_WIDTH, D_MODEL), NA_WIDTH ** -0.5),
        "w_out": nrm(ks[13], (DEPTH, D_MODEL, D_MODEL), beta * D_MODEL ** -0.5),
        "ln1_g": 1.0 + nrm(ks[14], (DEPTH, D_MODEL), 0.02),
        "ln1_b": nrm(ks[15], (DEPTH, D_MODEL), 0.02),
        "w_router": nrm(ks[16], (DEPTH, D_MODEL, N_EXPERTS), D_MODEL ** -0.5),
        "router_bias": nrm(ks[17], (DEPTH, N_EXPERTS), 0.01),
        "w_e_gate": nrm(ks[18], (DEPTH, N_EXPERTS, D_MODEL, EXPERT_DIM), D_MODEL ** -0.5),
        "w_e_up": nrm(ks[19], (DEPTH, N_EXPERTS, D_MODEL, EXPERT_DIM), D_MODEL ** -0.5),
        "w_e_down": nrm(ks[20], (DEPTH, N_EXPERTS, EXPERT_DIM, D_MODEL), beta * EXPERT_DIM ** -0.5),
        "w_sh_gate": nrm(ks[21], (DEPTH, D_MODEL, SHARED_DIM), D_MODEL ** -0.5),
        "w_sh_up": nrm(ks[22], (DEPTH, D_MODEL, SHARED_DIM), D_MODEL ** -0.5),
        "w_sh_down": nrm(ks[23], (DEPTH, SHARED_DIM, D_MODEL), beta * SHARED_DIM ** -0.5),
        "ln2_g": 1.0 + nrm(ks[24], (DEPTH, D_MODEL), 0.02),
        "ln2_b": nrm(ks[25], (DEPTH, D_MODEL), 0.02),
    }


def reference(x, c, ctx, c_ctx, w_ada, b_ada, w_in, hg_lb_fwd, hg_lb_bwd, hg_norm_g, na_rpb,
              w_branch_a, w_branch_b, w_out, ln1_g, ln1_b, w_router, router_bias, w_e_gate, w_e_up,
              w_e_down, w_sh_gate, w_sh_up, w_sh_down, ln2_g, ln2_b):
    alpha = (2.0 * DEPTH) ** 0.25
    lb_fwd = jnp.cumsum(jax.nn.softmax(hg_lb_fwd.astype(jnp.float32), axis=0), axis=0)
    lb_bwd = jnp.cumsum(jax.nn.softmax(hg_lb_bwd.astype(jnp.float32), axis=0), axis=0)
    cond = jax.nn.silu(c)
    cond_ctx = jax.nn.silu(c_ctx)
    for l in range(DEPTH):
        ctx_needed = l < DEPTH - 1
        mod = (cond @ w_ada[l] + b_ada[l])[:, None, :]
        mod_c = cond_ctx @ w_ada[l] + b_ada[l]
        sh1, sc1, g1, sh2, sc2, g2 = jnp.split(mod, 6, axis=-1)
        csh1, csc1, cg1, csh2, csc2, cg2 = jnp.split(mod_c, 6, axis=-1)
        h = modulate(x, sh1, sc1)
        hc = modulate(ctx, csh1, csc1)
        y, yc = token_mixer(h, hc, w_in[l], lb_fwd[l].reshape(HG_HEADS, HG_DK),
                            lb_bwd[l].reshape(HG_HEADS, HG_DK), hg_norm_g[l], na_rpb[l],
                            w_branch_a[l], w_branch_b[l], w_out[l], ctx_needed)
        x = layer_norm(alpha * x + g1 * y, ln1_g[l], ln1_b[l])
        y2 = moe_ffn(modulate(x, sh2, sc2), w_router[l], router_bias[l], w_e_gate[l], w_e_up[l],
                     w_e_down[l], w_sh_gate[l], w_sh_up[l], w_sh_down[l])
        x = layer_norm(alpha * x + g2 * y2, ln2_g[l], ln2_b[l])
        if ctx_needed:
            ctx = layer_norm(alpha * ctx + cg1 * yc, ln1_g[l], ln1_b[l])
            yc2 = moe_ffn(modulate(ctx, csh2, csc2), w_router[l], router_bias[l], w_e_gate[l], w_e_up[l],
                          w_e_down[l], w_sh_gate[l], w_sh_up[l], w_sh_down[l])
            ctx = layer_norm(alpha * ctx + cg2 * yc2, ln2_g[l], ln2_b[l])
    return x
```

```python
import contextlib
import numpy as np
import ml_dtypes
import concourse.bass as bass
import concourse.mybir as mybir
from concourse.bass_utils import run_bass_kernel_spmd

F32 = mybir.dt.float32
BF16 = mybir.dt.bfloat16
AF = mybir.ActivationFunctionType
ALU = mybir.AluOpType
AX = mybir.AxisListType


class Tr:
    def __init__(self, name):
        self.name = name
        self.w = None
        self.r = {}
        self.dsem = None


class Buf(Tr):
    def __init__(self, name, handle):
        super().__init__(name)
        self.h = handle
        self.ap = handle[:] if not hasattr(handle, "ap") or not callable(getattr(handle, "ap")) else handle.ap()


class Sched:
    ENG = ("pe", "act", "dve", "pool", "sp")

    def __init__(self, nc):
        self.nc = nc
        self.gstack = contextlib.ExitStack()
        self.stack = self.gstack
        self.prog = {e: [] for e in self.ENG}
        self.sem = {e: self.gstack.enter_context(nc.semaphore("s_" + e)) for e in self.ENG}
        self.cnt = {e: 0 for e in self.ENG}
        self.seen = {e: {} for e in self.ENG}
        self.dsems = []
        self.dfree = []
        self.scope_trs = [[]]
        self.ninst = 0

    def sb(self, name, shape, dtype):
        h = self.stack.enter_context(self.nc.sbuf_tensor(name, list(shape), dtype))
        return Buf(name, h)

    def ps(self, name, shape, dtype):
        h = self.stack.enter_context(self.nc.psum_tensor(name, list(shape), dtype))
        return Buf(name, h)

    def tr(self, name="t"):
        return Tr(name)

    def _need(self, eng, reads, writes, skip_pe_waw=False):
        need = {}

        def add(key, semh, val):
            if key not in need or need[key][1] < val:
                need[key] = (semh, val)

        for t in reads:
            if t.w is not None:
                add(*t.w)
        for t in writes:
            if t.w is not None:
                if not (skip_pe_waw and t.w[0] == "pe"):
                    add(*t.w)
            for key, (semh, val) in t.r.items():
                add(key, semh, val)
        out = []
        for key, (semh, val) in need.items():
            if self.seen[eng].get(key, 0) < val:
                self.seen[eng][key] = val
                out.append((semh, val))
        return out

    def _emit_waits(self, eng, waits):
        for semh, val in waits:
            self.prog[eng].append(lambda e, semh=semh, val=val: e.wait_ge(semh, val))

    def op(self, eng, fn, reads=(), writes=()):
        waits = self._need(eng, reads, writes, skip_pe_waw=(eng == "pe"))
        self._emit_waits(eng, waits)
        self.cnt[eng] += 1
        self.ninst += 1
        val = self.cnt[eng]
        semh = self.sem[eng]
        self.prog[eng].append(lambda e, fn=fn, semh=semh: fn(e).then_inc(semh, 1))
        rec = (eng, semh, val)
        for t in reads:
            t.r[eng] = (semh, val)
        for t in writes:
            t.w = rec
            t.r = {}

    def dma(self, eng, out_ap, in_ap, reads=(), writes=()):
        waits = self._need(eng, reads, writes)
        self._emit_waits(eng, waits)
        t0 = writes[0]
        if t0.dsem is None:
            if self.dfree:
                t0.dsem = self.dfree.pop()
            else:
                h = self.gstack.enter_context(self.nc.semaphore("d%d" % len(self.dsems)))
                self.dsems.append([h, 0])
                t0.dsem = len(self.dsems) - 1
            self.scope_trs[-1].append(t0)
        rec = self.dsems[t0.dsem]
        rec[1] += 16
        semh, val = rec[0], rec[1]
        key = ("d", t0.dsem)
        self.prog[eng].append(lambda e, o=out_ap, i=in_ap, semh=semh: e.dma_start(out=o, in_=i).then_inc(semh, 16))
        for t in reads:
            t.r[key] = (semh, val)
        for t in writes:
            t.w = (key, semh, val)
            t.r = {}

    def barrier(self):
        for eng in self.ENG:
            for e2 in ("pe", "act", "dve", "pool"):
                if self.cnt[e2] and self.seen[eng].get(e2, 0) < self.cnt[e2]:
                    self.seen[eng][e2] = self.cnt[e2]
                    self.prog[eng].append(lambda e, semh=self.sem[e2], val=self.cnt[e2]: e.wait_ge(semh, val))
            for i, (h, c) in enumerate(self.dsems):
                key = ("d", i)
                if c and self.seen[eng].get(key, 0) < c:
                    self.seen[eng][key] = c
                    self.prog[eng].append(lambda e, semh=h, val=c: e.wait_ge(semh, val))

    def flush(self):
        nc = self.nc
        progs = self.prog
        self.prog = {e: [] for e in self.ENG}
        self.nblk = getattr(self, "nblk", 0) + 1
        with nc.named_scope("blk%d" % self.nblk), nc.Block() as block:
            @block.tensor
            def _(e):
                for f in progs["pe"]:
                    f(e)

            @block.scalar
            def _(e):
                for f in progs["act"]:
                    f(e)

            @block.vector
            def _(e):
                for f in progs["dve"]:
                    f(e)

            @block.gpsimd
            def _(e):
                for f in progs["pool"]:
                    f(e)

            @block.sync
            def _(e):
                for f in progs["sp"]:
                    f(e)

    @contextlib.contextmanager
    def scope(self):
        old = self.stack
        self.stack = contextlib.ExitStack()
        self.scope_trs.append([])
        try:
            yield
        finally:
            self.barrier()
            self.flush()
            for t in self.scope_trs.pop():
                self.dfree.append(t.dsem)
                t.dsem = None
            self.stack.close()
            self.stack = old

    def finish(self):
        self.barrier()
        self.flush()
        self.gstack.close()


D = 1024
NOWN = 2048
NKB = 2560
ALPHA = 2.0 ** 0.25
EPS = 1e-6


def interleave(lists):
    n = max(len(l) for l in lists)
    for i in range(n):
        for l in lists:
            if i < len(l):
                l[i]()


def pipeline(n, stages):
    ns = len(stages)
    for step in range(n + ns - 1):
        for s in range(ns - 1, -1, -1):
            i = step - s
            if 0 <= i < n:
                stages[s](i)


class PsumPool:
    def __init__(self, S):
        self.banks = [S.ps("psb%d" % i, [128, 512], F32) for i in range(8)]
        for b in self.banks:
            b.bf = b.ap.bitcast(BF16)
        self.i = 0
        self.reserved = set()

    def get(self):
        while True:
            b = self.banks[self.i]
            self.i = (self.i + 1) % 8
            if id(b) not in self.reserved:
                return b

    def reserve(self):
        b = self.get()
        self.reserved.add(id(b))
        return b

    def release(self, b):
        self.reserved.discard(id(b))


class Stage:
    def __init__(self, S, n=4, size=2048, tag=""):
        self.S = S
        self.slots = [S.sb("stg%s_%d" % (tag, i), [128, size], F32) for i in range(n)]
        self.i = 0
        self.size = size

    def load(self, src_ap, nfree):
        s = self.slots[self.i]
        self.i = (self.i + 1) % len(self.slots)
        p = src_ap.shape[0]
        view = s.ap[0:p, 0:nfree]
        if len(src_ap.shape) == 3:
            view = view.rearrange("p (a b) -> p a b", a=src_ap.shape[1])
        self.S.dma("sp", view, src_ap, writes=[s])
        return s, view

    def load_cast(self, src_ap, nfree, dst_ap, dst_tr, eng="act"):
        s, view = self.load(src_ap, nfree)
        if eng == "act":
            self.S.op("act", lambda e, o=dst_ap, i=view: e.activation(o, i, AF.Copy), reads=[s], writes=[dst_tr])
        else:
            self.S.op(eng, lambda e, o=dst_ap, i=view: e.tensor_copy(o, i), reads=[s], writes=[dst_tr])


def ln_tokens(S, PS, ident_bf, src, T, dst_fn, dst_trs, sc_ap_fn, sh_ap_fn, col_tr, xts, work):
    nt = T // 128

    def stA(t):
        st, mv, rstd, nmr, xb = work[t % len(work)]
        xt = xts[t % len(xts)]
        S.dma("sp", xt.ap[:], src[t * 128:(t + 1) * 128, :], writes=[xt])
        for i in range(2):
            S.op("dve", lambda e, i=i: e.bn_stats(st.ap[:, i, :], xt.ap[:, i * 512:(i + 1) * 512]), reads=[xt], writes=[st])
        S.op("dve", lambda e: e.bn_aggr(mv.ap[:], st.ap[:].rearrange("p a b -> p (a b)")), reads=[st], writes=[mv])
        S.op("act", lambda e: e.activation(rstd.ap[:], mv.ap[:, 1:2], AF.Sqrt, bias=EPS, scale=1.0), reads=[mv], writes=[rstd])
        S.op("dve", lambda e: e.reciprocal(rstd.ap[:], rstd.ap[:]), reads=[rstd], writes=[rstd])
        S.op("dve", lambda e: e.scalar_tensor_tensor(nmr.ap[:], mv.ap[:, 0:1], -1.0, rstd.ap[:], ALU.mult, ALU.mult),
             reads=[mv, rstd], writes=[nmr])
        S.op("act", lambda e: e.activation(xb.ap[:], xt.ap[:], AF.Identity, bias=nmr.ap[:], scale=rstd.ap[:]),
             reads=[xt, nmr, rstd], writes=[xb])

    def stB(t):
        st, mv, rstd, nmr, xb = work[t % len(work)]
        ps = PS.get()

        def tr(e):
            for k in range(8):
                ins = e.transpose(ps.bf[:, k * 128:(k + 1) * 128], xb.ap[:, k * 128:(k + 1) * 128], ident_bf.ap[:])
            return ins
        S.op("pe", tr, reads=[xb, ident_bf], writes=[ps])
        for k in range(8):
            if k % 2 == 0:
                S.op("act", lambda e, k=k: e.activation(
                    dst_fn(k, t), ps.bf[:, k * 128:(k + 1) * 128], AF.Identity, bias=sh_ap_fn(k), scale=sc_ap_fn(k)),
                    reads=[ps, col_tr], writes=[dst_trs[t]])
            else:
                S.op("dve", lambda e, k=k: e.tensor_scalar(
                    dst_fn(k, t), ps.bf[:, k * 128:(k + 1) * 128], sc_ap_fn(k), sh_ap_fn(k), ALU.mult, ALU.add),
                    reads=[ps, col_tr], writes=[dst_trs[t]])
    for step in range(nt + 1):
        if step < nt:
            stA(step)
        if step >= 1:
            stB(step - 1)


def build_program(stage=99):
    nc = bass.Bass("TRN2", target_bir_lowering=False)

    def din(name, shape, dt=F32):
        return nc.dram_tensor(name, list(shape), dt, kind="ExternalInput").ap()

    def dscr(name, shape, dt=F32):
        return nc.dram_tensor(name, list(shape), dt, kind="Internal").ap()

    def dout(name, shape, dt=F32):
        return nc.dram_tensor(name, list(shape), dt, kind="ExternalOutput").ap()

    x_own = din("x_own", [2048, D])
    x_halo = din("x_halo", [512, D])
    ctx2 = din("ctx2", [512, D])
    x_slots = din("x_slots", [6144, D])
    cvecT = din("cvecT", [128, 16])
    w_ada = din("w_ada", [D, 6 * D])
    b_row = din("b_row", [1, 6 * D])
    b_col = din("b_col", [128, 48])
    w_in = din("w_in", [D, 10 * D])
    wf_slots = din("wf_slots", [3, D, D])
    lbT = din("lbT", [128, 80])
    flags = din("flags", [128, 6])
    hgT = din("hgT", [128, 8])
    consts_bf = din("consts_bf", [128, 768], BF16)
    ident_f_d = din("ident_f", [128, 128])
    mod_d = dscr("mod_d", [2, 6 * D])
    w_a = din("w_a", [D, D])
    w_b = din("w_b", [D, D])
    w_o = din("w_o", [D, D])
    lnp = din("lnp", [4, D])
    w_r = din("w_r", [D, 64])
    rbias = din("rbias", [1, 64])
    w_eg = din("w_eg", [65, D, 256])
    w_eu = din("w_eu", [65, D, 256])
    w_ed = din("w_ed", [65, 256, D])
    x1_d = dscr("x1_d", [NOWN, D])
    g_d = dscr("g_d", [65, NOWN])
    out_d = dout("out", [NOWN, D])
    cosT_d = din("cosT", [128, NKB])
    sinT_d = din("sinT", [128, NKB])
    Bint_d = din("Bint", [8, 128, 512])
    Bspec_d = din("Bspec", [8, 128, 7 * 768])

    S = Sched(nc)
    PS = PsumPool(S)
    dbg = {}

    cb = S.sb("cb", [128, 768], BF16)
    S.dma("sp", cb.ap[:], consts_bf, writes=[cb])
    ident_bf = Buf.__new__(Buf)
    Tr.__init__(ident_bf, "ident_bf")
    ident_bf.ap = cb.ap[:, 0:128]
    ident_bf.w = cb.w
    ident_f = S.sb("ident_f_sb", [128, 128], F32)
    S.dma("sp", ident_f.ap[:], ident_f_d, writes=[ident_f])
    cols = S.sb("cols", [128, 6, 8], F32)
    lbv = S.sb("lbv", [128, 5, 8], F32)
    oml = S.sb("oml", [128, 5, 8], F32)
    flg = S.sb("flg", [128, 6], F32)
    S.dma("sp", flg.ap[:], flags, writes=[flg])
    hgc = S.sb("hgc", [128, 8], F32)
    S.dma("sp", hgc.ap[:], hgT, writes=[hgc])
    hT = S.sb("hT", [128, 8, NKB], BF16)
    hT_t = [S.tr("hT%d" % i) for i in range(20)]
    xts = [S.sb("gxt%d" % i, [128, 1024], F32) for i in range(2)]
    work = [(S.sb("gst%d" % i, [128, 2, 6], F32), S.sb("gmv%d" % i, [128, 2], F32), S.sb("grstd%d" % i, [128, 1], F32),
             S.sb("gnmr%d" % i, [128, 1], F32), S.sb("gxb%d" % i, [128, 1024], BF16)) for i in range(2)]
    hgs = S.scope()
    hgs.__enter__()
    ones_bf = S.sb("ones_bf", [128, 2048], BF16)
    S.op("pool", lambda e: e.memset(ones_bf.ap[:], 1.0), writes=[ones_bf])
    rmask = S.sb("rmask", [128, 2048], BF16)
    S.op("pool", lambda e: e.memset(rmask.ap[:], 1.0), writes=[rmask])
    S.op("pool", lambda e: e.memset(rmask.ap[:].rearrange("p (c t) -> p c t", t=64)[:, :, 0:1], 0.0), writes=[rmask])
    Sf0 = S.sb("Sf0", [128, 8, 128], F32)
    Sb0 = S.sb("Sb0", [128, 8, 128], F32)
    S.op("pool", lambda e: e.memset(Sf0.ap[:], 0.0), writes=[Sf0])
    S.op("pool", lambda e: e.memset(Sb0.ap[:], 0.0), writes=[Sb0])

    with S.scope():
        STG = Stage(S, 4, 2048, "p0")
        condT = S.sb("condT", [128, 16], F32)
        S.dma("sp", condT.ap[:], cvecT, writes=[condT])
        S.op("act", lambda e: e.activation(condT.ap[:], condT.ap[:], AF.Silu), reads=[condT], writes=[condT])
        bcol = S.sb("bcol", [128, 48], F32)
        S.dma("sp", bcol.ap[:], b_col, writes=[bcol])
        brow = S.sb("brow", [2, 6 * D], F32)
        brow_t = [S.tr("brow0"), S.tr("brow1")]
        for r in range(2):
            S.dma("sp", brow.ap[r:r + 1, :], b_row, writes=[brow_t[r]])
        modrow = S.sb("modrow", [2, 6 * D], F32)
        modcol = S.sb("modcol", [128, 48, 2], F32)
        lbraw = S.sb("lbraw", [128, 80], F32)
        S.dma("sp", lbraw.ap[:], lbT, writes=[lbraw])
        lbd = S.sb("lbd", [128, 5, 8], F32)
        lb4 = lbraw.ap[:].rearrange("p (a two h) -> p a two h", two=2, h=8)
        S.op("dve", lambda e: e.tensor_tensor(lbd.ap[:], lb4[:, :, 0, :], lb4[:, :, 1, :], ALU.subtract),
             reads=[lbraw], writes=[lbd])
        S.op("act", lambda e: e.activation(lbv.ap[:], lbd.ap[:], AF.Sigmoid), reads=[lbd], writes=[lbv])
        S.op("dve", lambda e: e.tensor_scalar(oml.ap[:], lbv.ap[:], -1.0, 1.0, ALU.mult, ALU.add),
             reads=[lbv], writes=[oml])
        psC = PS.reserve()
        w_ada_v = w_ada.rearrange("(k p) n -> p k n", p=128)
        for blk in range(24):
            stg, view = STG.load(w_ada_v[:, :, blk * 256:(blk + 1) * 256], 2048)

            def mmc(e, view=view, blk=blk):
                for cc in range(2):
                    c0 = (2 * blk + cc) * 2
                    for k in range(8):
                        ins = e.matmul(psC.ap[:, c0:c0 + 2], view[:, k, cc * 128:(cc + 1) * 128],
                                       condT.ap[:, 2 * k:2 * k + 2], start=(k == 0), stop=(k == 7))
                return ins
            S.op("pe", mmc, reads=[stg, condT], writes=[psC])
            psR = PS.get()

            def mmr(e, view=view, psR=psR):
                for k in range(8):
                    ins = e.matmul(psR.ap[0:2, 0:256], condT.ap[:, 2 * k:2 * k + 2], view[:, k, :],
                                   start=(k == 0), stop=(k == 7))
                return ins
            S.op("pe", mmr, reads=[stg, condT], writes=[psR])
            S.op("dve", lambda e, psR=psR, blk=blk: e.tensor_tensor(
                modrow.ap[0:2, blk * 256:(blk + 1) * 256], psR.ap[0:2, 0:256],
                brow.ap[0:2, blk * 256:(blk + 1) * 256], ALU.add),
                reads=[psR] + brow_t, writes=[modrow])
        S.dma("sp", mod_d, modrow.ap[0:2, :], reads=[modrow], writes=[S.tr("mod_d")])
        pc3 = psC.ap[:, 0:96].rearrange("p (c r) -> p c r", r=2)
        for r in range(2):
            S.op("dve", lambda e, r=r: e.tensor_tensor(modcol.ap[:, :, r], pc3[:, :, r], bcol.ap[:], ALU.add),
                 reads=[psC, bcol], writes=[modcol])
        for ci, (c0, r, addone) in enumerate([(8, 0, 1.0), (0, 0, 0.0), (8, 1, 1.0), (0, 1, 0.0), (32, 0, 1.0), (24, 0, 0.0)]):
            S.op("dve", lambda e, ci=ci, c0=c0, r=r, addone=addone: e.tensor_scalar(
                cols.ap[:, ci, :], modcol.ap[:, c0:c0 + 8, r], addone, None, ALU.add),
                reads=[modcol], writes=[cols])
        PS.release(psC)
    if stage == 0:
        o_cols = dout("o_cols", [128, 48])
        o_lb = dout("o_lb", [128, 40])
        S.dma("sp", o_cols, cols.ap[:].rearrange("p a b -> p (a b)"), reads=[cols], writes=[S.tr("o1")])
        S.dma("sp", o_lb, lbv.ap[:].rearrange("p a b -> p (a b)"), reads=[lbv], writes=[S.tr("o2")])
        o_mod = dout("o_mod", [2, 6 * D])
        S.dma("sp", o_mod, mod_d, reads=[], writes=[S.tr("o3")])
        S.finish()
        return nc

    with S.scope():
        STG = Stage(S, 4, 1024, "p2")
        hTs = hT
        hTs_t = hT_t[0:16]
        WfB = S.sb("WfB", [128, 8, 1024], BF16)
        WiB = S.sb("WiB", [128, 8, 1024], BF16)
        T1 = S.sb("T1", [128, 2048], F32)
        T2 = S.sb("T2", [128, 2048], F32)
        T3 = S.sb("T3", [128, 2048], F32)
        T4 = S.sb("T4", [128, 2048], F32)
        kd = S.sb("kd", [128, 2048], BF16)
        kd_tm = S.sb("kd_tm", [128, 16, 128], BF16)
        v_tm = S.sb("v_tm", [128, 16, 128], BF16)
        tmpS = S.sb("tmpS", [128, 128], F32)
        tmpD = S.sb("tmpD", [128, 128], F32)
        Dcol = S.sb("Dcol", [128, 1], F32)
        w_in_v = w_in.rearrange("(k p) n -> p k n", p=128)
        for q8 in range(8):
            STG.load_cast(w_in_v[:, :, 3072 + q8 * 128:3072 + (q8 + 1) * 128], 1024,
                          WiB.ap[:, :, q8 * 128:(q8 + 1) * 128], WiB)
        slots = [
            (ctx2[0:256, :], 256, 2, 3, w_in_v[:, :, 1024:2048], 0, "f"),
            (ctx2[256:512, :], 256, 2, 3, w_in_v[:, :, 2048:3072], 1, "b"),
        ]
        for s in range(3):
            slots.append((x_slots[s * 2048:(s + 1) * 2048, :], 2048, 0, 1,
                          wf_slots[s].rearrange("(k p) n -> p k n", p=128), 2 + s, s))
        def slot_body(src, T, ci_sc, ci_sh, wf_src, lbi, mode):
            nt = T // 128
            TB = min(512, T)
            ln_tokens(S, PS, ident_bf, src, T,
                      lambda k, t: hTs.ap[:, k, t * 128:(t + 1) * 128], hTs_t,
                      lambda k, ci=ci_sc: cols.ap[:, ci, k:k + 1], lambda k, ci=ci_sh: cols.ap[:, ci, k:k + 1],
                      cols, xts, work)
            for q8 in range(8):
                STG.load_cast(wf_src[:, :, q8 * 128:(q8 + 1) * 128], 1024, WfB.ap[:, :, q8 * 128:(q8 + 1) * 128], WfB)
            vctx = {}

            def pA1(h):
                hs = slice(h * 128, (h + 1) * 128)
                for tb in range(T // TB):
                    ps = PS.get()
                    ts_ = slice(tb * TB, (tb + 1) * TB)
                    def mmf(e, ps=ps, ts_=ts_, hs=hs):
                        for k in range(8):
                            ins = e.matmul(ps.ap[:, 0:TB], WfB.ap[:, k, hs], hTs.ap[:, k, ts_], start=(k == 0), stop=(k == 7))
                        return ins
                    S.op("pe", mmf, reads=[WfB] + hTs_t[tb * TB // 128:(tb + 1) * TB // 128], writes=[ps])
                    S.op("act", lambda e, ps=ps, ts_=ts_: e.activation(T1.ap[:, ts_], ps.ap[:, 0:TB], AF.Sigmoid),
                         reads=[ps], writes=[T1])

            def pA2(h):
                hs = slice(h * 128, (h + 1) * 128)
                vps = []
                vctx[h] = vps
                for g in range((nt + 3) // 4):
                    n = min(4, nt - g * 4)
                    ps = PS.reserve()
                    vps.append((ps, g, n))
                    def mmv(e, ps=ps, g=g, n=n, hs=hs):
                        for j in range(n):
                            t = g * 4 + j
                            for k in range(8):
                                ins = e.matmul(ps.ap[:, j * 128:(j + 1) * 128], hTs.ap[:, k, t * 128:(t + 1) * 128],
                                               WiB.ap[:, k, hs], start=(k == 0), stop=(k == 7))
                        return ins
                    S.op("pe", mmv, reads=[WiB] + hTs_t[g * 4:g * 4 + n], writes=[ps])

            def pB(h):
                S.op("dve", lambda e, h=h, lbi=lbi: e.tensor_scalar(
                    T1.ap[:, 0:T], T1.ap[:, 0:T], oml.ap[:, lbi, h:h + 1], lbv.ap[:, lbi, h:h + 1], ALU.mult, ALU.add),
                    reads=[T1, oml, lbv], writes=[T1])
                S.op("act", lambda e: e.activation(T2.ap[:, 0:T], T1.ap[:, 0:T], AF.Ln), reads=[T1], writes=[T2])
                S.op("act", lambda e: e.activation(T1.ap[:, 0:T], T1.ap[:, 0:T], AF.Identity, bias=1.0, scale=-1.0),
                     reads=[T1], writes=[T1])
                S.op("dve", lambda e: e.tensor_tensor_scan(T3.ap[:, 0:T], ones_bf.ap[:, 0:T], T2.ap[:, 0:T], 0.0, ALU.mult, ALU.add),
                     reads=[ones_bf, T2], writes=[T3])
                S.op("act", lambda e: e.activation(T4.ap[:, 0:T], T3.ap[:, 0:T], AF.Exp, bias=T3.ap[:, T - 1:T], scale=-1.0),
                     reads=[T3], writes=[T4])
                S.op("act", lambda e: e.activation(Dcol.ap[:], T3.ap[:, T - 1:T], AF.Exp), reads=[T3], writes=[Dcol])
                S.op("dve", lambda e: e.tensor_tensor(kd.ap[:, 0:T], T1.ap[:, 0:T], T4.ap[:, 0:T], ALU.mult),
                     reads=[T1, T4], writes=[kd])

            def pC1(h):
                vps = vctx.pop(h)
                for (ps, g, n) in vps:
                    S.op("dve", lambda e, ps=ps, g=g, n=n: e.tensor_copy(
                        v_tm.ap[:, g * 4:g * 4 + n, :], ps.ap[:, 0:n * 128].rearrange("p (a b) -> p a b", b=128)),
                        reads=[ps], writes=[v_tm])
                    PS.release(ps)

            def pC2(h):
                for g in range((nt + 7) // 8):
                    n = min(8, nt - g * 8)
                    ps = PS.get()
                    def trk(e, ps=ps, g=g, n=n):
                        for j in range(n):
                            t = g * 8 + j
                            ins = e.transpose(ps.bf[:, j * 128:(j + 1) * 128], kd.ap[:, t * 128:(t + 1) * 128], ident_bf.ap[:])
                        return ins
                    S.op("pe", trk, reads=[kd, ident_bf], writes=[ps])
                    S.op("act", lambda e, ps=ps, g=g, n=n: e.activation(
                        kd_tm.ap[:, g * 8:g * 8 + n, :], ps.bf[:, 0:n * 128].rearrange("p (a b) -> p a b", b=128), AF.Copy),
                        reads=[ps], writes=[kd_tm])
                psU = PS.get()
                def mmu(e, psU=psU, nt=nt):
                    for t in range(nt):
                        ins = e.matmul(psU.ap[:, 0:128], kd_tm.ap[:, t, :], v_tm.ap[:, t, :], start=(t == 0), stop=(t == nt - 1))
                    return ins
                S.op("pe", mmu, reads=[kd_tm, v_tm], writes=[psU])
                if mode == "f":
                    S.op("dve", lambda e, psU=psU, h=h: e.tensor_copy(Sf0.ap[:, h, :], psU.ap[:, 0:128]), reads=[psU], writes=[Sf0])
                elif mode == "b":
                    S.op("dve", lambda e, psU=psU, h=h: e.tensor_copy(Sb0.ap[:, h, :], psU.ap[:, 0:128]), reads=[psU], writes=[Sb0])
                else:
                    for (SX, fc) in ((Sf0, mode), (Sb0, 3 + mode)):
                        S.op("dve", lambda e, SX=SX, psU=psU, h=h: e.scalar_tensor_tensor(
                            tmpS.ap[:], SX.ap[:, h, :], Dcol.ap[:, 0:1], psU.ap[:, 0:128], ALU.mult, ALU.add),
                            reads=[SX, Dcol, psU], writes=[tmpS])
                        S.op("dve", lambda e, SX=SX, h=h: e.tensor_tensor(tmpD.ap[:], tmpS.ap[:], SX.ap[:, h, :], ALU.subtract),
                             reads=[tmpS, SX], writes=[tmpD])
                        S.op("dve", lambda e, SX=SX, h=h, fc=fc: e.scalar_tensor_tensor(
                            SX.ap[:, h, :], tmpD.ap[:], flg.ap[:, fc:fc + 1], SX.ap[:, h, :], ALU.mult, ALU.add),
                            reads=[tmpD, flg, SX], writes=[SX])
            pA1(0)
            pA2(0)
            pB(0)
            for h in range(1, 8):
                pA1(h)
                pC1(h - 1)
                pA2(h)
                pC2(h - 1)
                pB(h)
            pC1(7)
            pC2(7)
        for sl in slots:
            slot_body(*sl)

    if stage == 2:
        o_sf = dout("o_sf", [128, 1024])
        o_sb = dout("o_sb", [128, 1024])
        S.dma("sp", o_sf, Sf0.ap[:].rearrange("p a b -> p (a b)"), reads=[Sf0], writes=[S.tr("o1")])
        S.dma("sp", o_sb, Sb0.ap[:].rearrange("p a b -> p (a b)"), reads=[Sb0], writes=[S.tr("o2")])
        S.finish()
        return nc

    y_hg_d = dscr("y_hg_d", [8, 128, NOWN], BF16)
    y_hg_t = [S.tr("yhg%d" % i) for i in range(8)]
    w_in_v = w_in.rearrange("(k p) n -> p k n", p=128)

    def ln_main(src, T, tile0):
        ln_tokens(S, PS, ident_bf, src, T,
                  lambda k, t: hT.ap[:, k, (tile0 + t) * 128:(tile0 + t + 1) * 128], hT_t[tile0:tile0 + T // 128],
                  lambda k: cols.ap[:, 0, k:k + 1], lambda k: cols.ap[:, 1, k:k + 1], cols, xts, work)
    ln_main(x_own, 2048, 2)
    ln_main(x_halo[0:256, :], 256, 0)
    ln_main(x_halo[256:512, :], 256, 18)

    with S.scope():
        STG = Stage(S, 4, 1024, "p3")
        W5s = [S.sb("W5_%d" % i, [128, 8, 5, 128], BF16) for i in range(2)]
        sq = S.sb("sq", [128, NOWN], BF16)
        sog = S.sb("sog", [128, NOWN], BF16)
        G1 = S.sb("G1", [128, 1024], F32)
        G2 = S.sb("G2", [128, 1024], F32)
        G3 = S.sb("G3", [128, 1024], F32)
        G4 = S.sb("G4", [128, 1024], F32)
        qa = [S.sb("qa%d" % d_, [128, NOWN], BF16) for d_ in range(2)]
        kb = [S.sb("kb%d" % d_, [128, NOWN], BF16) for d_ in range(2)]
        kdT = S.sb("kdT", [128, NOWN], BF16)
        kdm = [S.sb("kdm%d" % d_, [64, 32, 128], BF16) for d_ in range(2)]
        dec = [S.sb("dec%d" % d_, [128, 32], F32) for d_ in range(2)]
        Sbf = [S.sb("Sbf%d" % d_, [128, 32, 128], BF16) for d_ in range(2)]
        vtm = S.sb("vtm", [64, 32, 128], BF16)
        scm = [S.sb("scm%d" % i, [64, 512], BF16) for i in range(2)]
        osq = S.sb("osq", [128, 512], BF16)
        lnv = S.sb("lnv", [128, 512], F32)
        rs = S.sb("rs", [128, 512], F32)
        t1 = S.sb("t1", [128, 512], F32)
        yh = kdT
        mask8 = cb.ap[0:64, 256:768]

        def load_w5(h):
            for g in range(5):
                STG.load_cast(w_in_v[:, :, g * 1024 + h * 128:g * 1024 + (h + 1) * 128], 1024, W5s[h % 2].ap[:, :, g, :], W5s[h % 2])

        def hg_head(h):
            W5 = W5s[h % 2]
            if h == 0:
                load_w5(0)

            def proj_fm(g, tok0, n, ps):
                def f(e):
                    for k in range(8):
                        ins = e.matmul(ps.ap[:, 0:n], W5.ap[:, k, g, :], hT.ap[:, k, 256 + tok0:256 + tok0 + n],
                                       start=(k == 0), stop=(k == 7))
                    return ins
                S.op("pe", f, reads=[W5] + hT_t[2 + tok0 // 128:2 + (tok0 + n) // 128], writes=[ps])
            for (g, dst) in ((0, sq), (4, sog)):
                for tb in range(4):
                    ps = PS.get()
                    proj_fm(g, tb * 512, 512, ps)
                    S.op("act", lambda e, ps=ps, tb=tb, dst=dst: e.activation(dst.ap[:, tb * 512:(tb + 1) * 512], ps.ap[:], AF.Silu),
                         reads=[ps], writes=[dst])
            for g8 in range(8):
                ps = PS.get()

                def mmv(e, ps=ps, g8=g8):
                    for j in range(4):
                        c = g8 * 4 + j
                        for k in range(8):
                            ins = e.matmul(ps.ap[0:64, j * 128:(j + 1) * 128], hT.ap[:, k, 256 + c * 64:256 + (c + 1) * 64],
                                           W5.ap[:, k, 3, :], start=(k == 0), stop=(k == 7))
                    return ins
                S.op("pe", mmv, reads=[W5] + hT_t[2 + g8 * 2:2 + g8 * 2 + 2], writes=[ps])
                S.op("dve", lambda e, ps=ps, g8=g8: e.tensor_copy(
                    vtm.ap[:, g8 * 4:g8 * 4 + 4, :], ps.ap[0:64, :].rearrange("p (a b) -> p a b", b=128)), reads=[ps], writes=[vtm])

            def gate_ops(d_, hf):
                ts_ = slice(hf * 1024, (hf + 1) * 1024)
                ops = []

                def add(*a_, **k_):
                    ops.append(lambda: S.op(*a_, **k_))
                for tb in range(2):
                    def fpro(tb=tb):
                        ps = PS.get()
                        proj_fm(1 + d_, hf * 1024 + tb * 512, 512, ps)
                        S.op("act", lambda e: e.activation(G1.ap[:, tb * 512:(tb + 1) * 512], ps.ap[:], AF.Sigmoid),
                             reads=[ps], writes=[G1])
                    ops.append(fpro)
                add("dve", lambda e: e.tensor_scalar(G1.ap[:], G1.ap[:], oml.ap[:, d_, h:h + 1], lbv.ap[:, d_, h:h + 1], ALU.mult, ALU.add),
                     reads=[G1, oml, lbv], writes=[G1])
                add("act", lambda e: e.activation(G2.ap[:], G1.ap[:], AF.Ln), reads=[G1], writes=[G2])
                add("act", lambda e: e.activation(G1.ap[:], G1.ap[:], AF.Identity, bias=1.0, scale=-1.0), reads=[G1], writes=[G1])
                add("dve", lambda e: e.tensor_tensor_scan(G3.ap[:], rmask.ap[:, 0:1024], G2.ap[:], 0.0, ALU.mult, ALU.add),
                     reads=[rmask, G2], writes=[G3])
                g3v = G3.ap[:].rearrange("p (c t) -> p c t", t=64)
                g4v = G4.ap[:].rearrange("p (c t) -> p c t", t=64)
                if d_ == 1:
                    add("dve", lambda e: e.tensor_tensor(g4v, g3v[:, :, 63:64].broadcast_to([128, 16, 64]), g3v, ALU.subtract),
                         reads=[G3], writes=[G4])
                    add("dve", lambda e: e.tensor_tensor(G3.ap[:], G4.ap[:], G2.ap[:], ALU.add), reads=[G4, G2], writes=[G3])
                add("act", lambda e: e.activation(G4.ap[:], G3.ap[:], AF.Exp), reads=[G3], writes=[G4])
                add("act", lambda e: e.activation(G2.ap[:], G3.ap[:], AF.Exp, scale=-1.0), reads=[G3], writes=[G2])
                add("dve", lambda e: e.tensor_tensor(qa[d_].ap[:, ts_], sq.ap[:, ts_], G4.ap[:], ALU.mult),
                     reads=[sq, G4], writes=[qa[d_]])
                add("dve", lambda e: e.tensor_tensor(kb[d_].ap[:, ts_], G1.ap[:], G2.ap[:], ALU.mult),
                     reads=[G1, G2], writes=[kb[d_]])
                ecol = 63 if d_ == 0 else 0
                add("act", lambda e: e.activation(dec[d_].ap[:, hf * 16:(hf + 1) * 16], g4v[:, :, ecol], AF.Copy),
                     reads=[G4], writes=[dec[d_]])
                add("dve", lambda e: e.tensor_tensor(
                    kdT.ap[:, ts_].rearrange("p (c t) -> p c t", t=64), kb[d_].ap[:, ts_].rearrange("p (c t) -> p c t", t=64),
                    g4v[:, :, ecol:ecol + 1].broadcast_to([128, 16, 64]), ALU.mult),
                    reads=[kb[d_], G4], writes=[kdT])

                return ops

            def rest_ops(d_):
                ops = []
                for g in range(4):
                    def trg(g=g):
                        ps = PS.get()

                        def trk(e):
                            for j in range(8):
                                c = g * 8 + j
                                ins = e.transpose(ps.bf[0:64, j * 128:(j + 1) * 128], kdT.ap[:, c * 64:(c + 1) * 64], ident_bf.ap[:])
                            return ins
                        S.op("pe", trk, reads=[kdT, ident_bf], writes=[ps])
                        S.op("act", lambda e: e.activation(
                            kdm[d_].ap[:, g * 8:(g + 1) * 8, :], ps.bf[0:64, :].rearrange("p (a b) -> p a b", b=128), AF.Copy),
                            reads=[ps], writes=[kdm[d_]])
                    ops.append(trg)
                if d_ == 0:
                    ops.append(lambda: S.op("act", lambda e: e.activation(Sbf[0].ap[:, 0, :], Sf0.ap[:, h, :], AF.Copy), reads=[Sf0], writes=[Sbf[0]]))
                    chunks = list(range(0, 31))
                else:
                    ops.append(lambda: S.op("act", lambda e: e.activation(Sbf[1].ap[:, 31, :], Sb0.ap[:, h, :], AF.Copy), reads=[Sb0], writes=[Sbf[1]]))
                    chunks = list(range(31, 0, -1))
                for g0 in range(0, len(chunks), 4):
                    def ug(grp=chunks[g0:g0 + 4]):
                        ps = PS.get()

                        def mmu(e):
                            for j, c in enumerate(grp):
                                ins = e.matmul(ps.ap[:, j * 128:(j + 1) * 128], kdm[d_].ap[:, c, :], vtm.ap[:, c, :], start=True, stop=True)
                            return ins
                        S.op("pe", mmu, reads=[kdm[d_], vtm], writes=[ps])
                        for j, c in enumerate(grp):
                            cn = c + 1 if d_ == 0 else c - 1
                            S.op("dve", lambda e, j=j, c=c, cn=cn: e.scalar_tensor_tensor(
                                Sbf[d_].ap[:, cn, :], Sbf[d_].ap[:, c, :], dec[d_].ap[:, c:c + 1], ps.ap[:, j * 128:(j + 1) * 128],
                                ALU.mult, ALU.add), reads=[Sbf[d_], dec[d_], ps], writes=[Sbf[d_]])
                    ops.append(ug)
                return ops

            for op_ in gate_ops(0, 0) + gate_ops(0, 1):
                op_()
            interleave([rest_ops(0), gate_ops(1, 0) + gate_ops(1, 1)])
            for op_ in rest_ops(1):
                op_()

            if h + 1 < 8:
                load_w5(h + 1)
            psos = {}

            def stX(tb):
                pso = PS.reserve()
                psos[tb] = pso
                pssl = []
                for g2 in range(2):
                    c0 = tb * 8 + g2 * 4
                    pss = PS.reserve()
                    pssl.append(pss)

                    def mms(e, pss=pss, c0=c0):
                        for j in range(4):
                            csl = slice((c0 + j) * 64, (c0 + j + 1) * 64)
                            for dd in range(2):
                                o_ = (j * 2 + dd) * 64
                                ins = e.matmul(pss.ap[0:64, o_:o_ + 64], kb[dd].ap[:, csl], qa[dd].ap[:, csl], start=True, stop=True)
                        return ins
                    S.op("pe", mms, reads=[kb[0], kb[1], qa[0], qa[1]], writes=[pss])
                for g2 in range(2):
                    pss = pssl[g2]
                    sc_ = scm[g2]
                    S.op("dve", lambda e, pss=pss, sc_=sc_: e.tensor_tensor(sc_.ap[:], pss.ap[0:64, :], mask8, ALU.mult),
                         reads=[pss, cb], writes=[sc_])
                    PS.release(pss)
                for g2 in range(2):
                    c0 = tb * 8 + g2 * 4
                    sc_ = scm[g2]

                    def mmo(e, c0=c0, g2=g2, sc_=sc_):
                        for j in range(4):
                            c = c0 + j
                            o64 = slice((g2 * 4 + j) * 64, (g2 * 4 + j + 1) * 64)
                            csl = slice(c * 64, (c + 1) * 64)
                            e.matmul(pso.ap[:, o64], vtm.ap[:, c, :], sc_.ap[:, (j * 2) * 64:(j * 2 + 1) * 64], start=True, stop=False)
                            e.matmul(pso.ap[:, o64], vtm.ap[:, c, :], sc_.ap[:, (j * 2 + 1) * 64:(j * 2 + 2) * 64], start=False, stop=False)
                            e.matmul(pso.ap[:, o64], Sbf[0].ap[:, c, :], qa[0].ap[:, csl], start=False, stop=False)
                            ins = e.matmul(pso.ap[:, o64], Sbf[1].ap[:, c, :], qa[1].ap[:, csl], start=False, stop=True)
                        return ins
                    S.op("pe", mmo, reads=[vtm, sc_, Sbf[0], Sbf[1], qa[0], qa[1]], writes=[pso])

            def stY(tb):
                pso = psos.pop(tb)
                S.op("act", lambda e: e.activation(osq.ap[:], pso.ap[:], AF.Square), reads=[pso], writes=[osq])
                pq = PS.get()
                S.op("pe", lambda e: e.matmul(pq.ap[:], ones_bf.ap[:, 0:128], osq.ap[:], start=True, stop=True),
                     reads=[ones_bf, osq], writes=[pq])
                S.op("act", lambda e: e.activation(lnv.ap[:], pq.ap[:], AF.Ln, bias=EPS, scale=1.0 / 128.0), reads=[pq], writes=[lnv])
                S.op("act", lambda e: e.activation(rs.ap[:], lnv.ap[:], AF.Exp, scale=-0.5), reads=[lnv], writes=[rs])
                S.op("dve", lambda e: e.tensor_tensor(t1.ap[:], pso.ap[:], rs.ap[:], ALU.mult), reads=[pso, rs], writes=[t1])
                S.op("dve", lambda e: e.scalar_tensor_tensor(
                    yh.ap[:, tb * 512:(tb + 1) * 512], t1.ap[:], hgc.ap[:, h:h + 1], sog.ap[:, tb * 512:(tb + 1) * 512],
                    ALU.mult, ALU.mult), reads=[t1, hgc, sog], writes=[yh])
                PS.release(pso)
            for step in range(5):
                if step < 4:
                    stX(step)
                if step >= 1:
                    stY(step - 1)
            S.dma("sp", y_hg_d[h], yh.ap[:], reads=[yh], writes=[y_hg_t[h]])

        for h in range(8):
            hg_head(h)
    hgs.__exit__(None, None, None)
    if stage == 3:
        o_yhg = dout("o_yhg", [8, 128, NOWN], BF16)
        S.dma("sp", o_yhg, y_hg_d, reads=y_hg_t, writes=[S.tr("o1")])
        S.finish()
        return nc

    y_na_d = dscr("y_na_d", [8, 128, NOWN], BF16)
    y_na_t = [S.tr("yna%d" % i) for i in range(8)]
    with S.scope():
        STG = Stage(S, 4, 1024, "p4")
        hcT = S.sb("hcT", [128, 8, 256], BF16)
        hcT_t = [S.tr("hcT0"), S.tr("hcT1")]
        ln_tokens(S, PS, ident_bf, ctx2[0:256, :], 256, lambda k, t: hcT.ap[:, k, t * 128:(t + 1) * 128], hcT_t,
                  lambda k: cols.ap[:, 2, k:k + 1], lambda k: cols.ap[:, 3, k:k + 1], cols, xts, work)
        cosT = S.sb("cosT_sb", [128, NKB], F32)
        sinT = S.sb("sinT_sb", [128, NKB], F32)
        S.dma("sp", cosT.ap[:], cosT_d, writes=[cosT])
        S.dma("sp", sinT.ap[:], sinT_d, writes=[sinT])
        W3 = S.sb("W3", [128, 8, 3, 128], BF16)
        qblk = S.sb("qblk", [128, 32, 128], BF16)
        qpblk = S.sb("qpblk", [128, 32, 128], BF16)
        S.op("pool", lambda e: e.memset(qblk.ap[:], 0.0), writes=[qblk])
        S.op("pool", lambda e: e.memset(qpblk.ap[:], 0.0), writes=[qpblk])
        krT = S.sb("krT", [128, NKB], BF16)
        kcT = S.sb("kcT", [128, 256], BF16)
        pl = S.sb("pl", [128, 512], BF16)
        tqa = S.sb("tqa", [128, 512], F32)
        tqb = S.sb("tqb", [128, 512], F32)
        v_ev = S.sb("v_ev", [128, 20, 2, 65], BF16)
        v_od = S.sb("v_od", [128, 19, 2, 65], BF16)
        v_cx = S.sb("v_cx", [128, 2, 2, 65], BF16)
        for vb in (v_ev, v_od, v_cx):
            S.op("pool", lambda e, vb=vb: e.memset(vb.ap[:], 1.0), writes=[vb])
        Bi = S.sb("Bi", [128, 512], BF16)
        Bs = S.sb("Bs", [128, 7, 768], BF16)
        Pm = [S.sb("Pm%d" % i, [128, 1024], BF16) for i in range(4)]
        PT = [S.sb("PT%d" % i, [128, 8, 128], BF16) for i in range(3)]
        mxs = [S.sb("mx%d" % i, [128, 2], F32) for i in range(3)]
        nmxs = [S.sb("nmx%d" % i, [128, 1], F32) for i in range(3)]
        rcs = [S.sb("rc%d" % i, [64, 2], F32) for i in range(3)]
        ytms = [S.sb("ytm%d" % i, [64, 8, 128], BF16) for i in range(2)]
        ynaT = S.sb("ynaT", [128, NOWN], BF16)

        def na_pair(pr):
            for g in range(3):
                c0 = 5120 + g * 1024 + pr * 128
                STG.load_cast(w_in_v[:, :, c0:c0 + 128], 1024, W3.ap[:, :, g, :], W3)
            STG.load_cast(Bint_d[pr], 512, Bi.ap[:], Bi)
            for sr in range(7):
                STG.load_cast(Bspec_d[pr][:, sr * 768:(sr + 1) * 768], 768, Bs.ap[:, sr, :], Bs)

            def proj(g, src_t, src_trs, tok0, n, ps):
                def f(e):
                    for k in range(8):
                        ins = e.matmul(ps.ap[:, 0:n], W3.ap[:, k, g, :], src_t.ap[:, k, tok0:tok0 + n], start=(k == 0), stop=(k == 7))
                    return ins
                S.op("pe", f, reads=[W3] + src_trs, writes=[ps])

            def rope(ps, tok0, n, scale, out_fn, out_tr, plain_fn=None, plain_tr=None):
                S.op("act", lambda e: e.activation(pl.ap[:, 0:n], ps.ap[:, 0:n], AF.Identity, scale=scale), reads=[ps], writes=[pl])
                ps2 = PS.get()
                S.op("pe", lambda e: e.matmul(ps2.ap[:, 0:n], cb.ap[:, 128:256], pl.ap[:, 0:n], start=True, stop=True),
                     reads=[cb, pl], writes=[ps2])
                S.op("dve", lambda e: e.tensor_tensor(tqa.ap[:, 0:n], pl.ap[:, 0:n], cosT.ap[:, tok0:tok0 + n], ALU.mult),
                     reads=[pl, cosT], writes=[tqa])
                S.op("dve", lambda e: e.tensor_tensor(tqb.ap[:, 0:n], ps2.ap[:, 0:n], sinT.ap[:, tok0:tok0 + n], ALU.mult),
                     reads=[ps2, sinT], writes=[tqb])
                out_fn(tqa, tqb)
                if plain_fn is not None:
                    plain_fn(pl)

            for tb in range(4):
                ps = PS.get()
                proj(0, hT, hT_t[2 + tb * 4:2 + tb * 4 + 4], 256 + tb * 512, 512, ps)

                def qout(a_, b_, tb=tb):
                    for hh in range(2):
                        psl = slice(hh * 64, (hh + 1) * 64)
                        S.op("dve", lambda e, psl=psl: e.tensor_tensor(
                            qblk.ap[psl, tb * 8:(tb + 1) * 8, psl], a_.ap[psl, 0:512].rearrange("p (r q) -> p r q", q=64),
                            b_.ap[psl, 0:512].rearrange("p (r q) -> p r q", q=64), ALU.add), reads=[a_, b_], writes=[qblk])

                def qplain(pl_, tb=tb):
                    for hh in range(2):
                        psl = slice(hh * 64, (hh + 1) * 64)
                        S.op("act", lambda e, psl=psl: e.activation(
                            qpblk.ap[psl, tb * 8:(tb + 1) * 8, psl], pl_.ap[psl, 0:512].rearrange("p (r q) -> p r q", q=64), AF.Copy),
                            reads=[pl_], writes=[qpblk])
                rope(ps, 256 + tb * 512, 512, 0.125, qout, qblk, qplain, qpblk)
            for tb in range(5):
                ps = PS.get()
                proj(1, hT, hT_t[tb * 4:tb * 4 + 4], tb * 512, 512, ps)

                def kout(a_, b_, tb=tb):
                    S.op("dve", lambda e: e.tensor_tensor(krT.ap[:, tb * 512:(tb + 1) * 512], a_.ap[:, 0:512], b_.ap[:, 0:512], ALU.add),
                         reads=[a_, b_], writes=[krT])
                rope(ps, tb * 512, 512, 1.0, kout, krT)
            ps = PS.get()
            proj(1, hcT, hcT_t, 0, 256, ps)
            S.op("act", lambda e, ps=ps: e.activation(kcT.ap[:], ps.ap[:, 0:256], AF.Copy), reads=[ps], writes=[kcT])

            def vproj(src_t, src_trs_fn, tiles, dst):
                for g0 in range(0, len(tiles), 4):
                    grp = tiles[g0:g0 + 4]
                    ps = PS.get()
                    trs = []
                    for (ti, tok0) in grp:
                        trs += src_trs_fn(tok0)

                    def f(e, ps=ps, grp=grp):
                        for j, (ti, tok0) in enumerate(grp):
                            for k in range(8):
                                ins = e.matmul(ps.ap[:, j * 128:(j + 1) * 128], src_t.ap[:, k, tok0:tok0 + 128], W3.ap[:, k, 2, :],
                                               start=(k == 0), stop=(k == 7))
                        return ins
                    S.op("pe", f, reads=[W3] + trs, writes=[ps])
                    n = len(grp)
                    t0 = grp[0][0]
                    S.op("dve", lambda e, ps=ps, n=n, t0=t0: e.tensor_copy(
                        dst.ap[:, t0:t0 + n, :, 0:64], ps.ap[:, 0:n * 128].rearrange("p (a h d) -> p a h d", h=2, d=64)),
                        reads=[ps], writes=[dst])
            vproj(hT, lambda tok0: [hT_t[tok0 // 128]], [(i, i * 128) for i in range(20)], v_ev)
            vproj(hT, lambda tok0: [hT_t[tok0 // 128], hT_t[tok0 // 128 + 1]], [(i, 64 + i * 128) for i in range(19)], v_od)
            vproj(hcT, lambda tok0: [hcT_t[tok0 // 128]], [(i, i * 128) for i in range(2)], v_cx)

            ctx_ = {}

            def rowcfg(lr):
                if lr < 4:
                    return 0, 12, Bs.ap[:, lr, :], Bs
                if lr >= 29:
                    return 27, 12, Bs.ap[:, 4 + lr - 29, :], Bs
                return lr, 8, Bi.ap[:], Bi

            def st0(lr):
                rel0, nkr, Bap, Btr = rowcfg(lr)
                nk = nkr * 64
                psA = PS.get()
                psB = PS.get()
                k0 = rel0 * 64
                ctx_[lr] = dict(psA=psA, psB=psB, nk=nk, nB=nk - 512 + 256, rel0=rel0)

                def qk(e):
                    e.matmul(psA.ap[:, 0:512], qblk.ap[:, lr, :], krT.ap[:, k0:k0 + 512], start=True, stop=False)
                    e.matmul(psA.ap[:, 0:512], ident_bf.ap[:], Bap[:, 0:512], start=False, stop=True)
                    o = 0
                    if nk > 512:
                        e.matmul(psB.ap[:, 0:256], qblk.ap[:, lr, :], krT.ap[:, k0 + 512:k0 + 768], start=True, stop=False)
                        e.matmul(psB.ap[:, 0:256], ident_bf.ap[:], Bap[:, 512:768], start=False, stop=True)
                        o = 256
                    return e.matmul(psB.ap[:, o:o + 256], qpblk.ap[:, lr, :], kcT.ap[:], start=True, stop=True)
                S.op("pe", qk, reads=[qblk, qpblk, krT, kcT, Btr, ident_bf], writes=[psA, psB])

            def st1(lr):
                c = ctx_[lr]
                psA, psB, nB = c["psA"], c["psB"], c["nB"]
                mx_ = mxs[lr % 3]
                nm_ = nmxs[lr % 3]
                P_ = Pm[lr % 4]
                S.op("dve", lambda e: e.tensor_reduce(mx_.ap[:, 0:1], psA.ap[:, 0:512], AX.X, ALU.max), reads=[psA], writes=[mx_])
                S.op("dve", lambda e: e.tensor_reduce(mx_.ap[:, 1:2], psB.ap[:, 0:nB], AX.X, ALU.max), reads=[psB], writes=[mx_])
                S.op("dve", lambda e: e.tensor_scalar(nm_.ap[:], mx_.ap[:, 0:1], mx_.ap[:, 1:2], -1.0, ALU.max, ALU.mult),
                     reads=[mx_], writes=[nm_])
                S.op("act", lambda e: e.activation(P_.ap[:, 0:512], psA.ap[:, 0:512], AF.Exp, bias=nm_.ap[:], scale=1.0),
                     reads=[psA, nm_], writes=[P_])
                S.op("act", lambda e: e.activation(P_.ap[:, 512:512 + nB], psB.ap[:, 0:nB], AF.Exp, bias=nm_.ap[:], scale=1.0),
                     reads=[psB, nm_], writes=[P_])

            def st2(lr):
                c = ctx_[lr]
                nkt = (512 + c["nB"]) // 128
                P_ = Pm[lr % 4]
                PT_ = PT[lr % 3]
                pst = PS.get()

                def trp(e):
                    for jt in range(nkt):
                        ins = e.transpose(pst.bf[:, jt * 128:(jt + 1) * 128], P_.ap[:, jt * 128:(jt + 1) * 128], ident_bf.ap[:])
                    return ins
                S.op("pe", trp, reads=[P_, ident_bf], writes=[pst])
                S.op("act", lambda e: e.activation(
                    PT_.ap[:, 0:nkt, :], pst.bf[:, 0:nkt * 128].rearrange("p (a b) -> p a b", b=128), AF.Copy), reads=[pst], writes=[PT_])

            def st3(lr):
                c = ctx_.pop(lr)
                rel0 = c["rel0"]
                nwt = c["nk"] // 128
                PT_ = PT[lr % 3]
                pso = PS.get()
                if rel0 % 2 == 0:
                    vt, vi0 = v_ev, rel0 // 2
                else:
                    vt, vi0 = v_od, (rel0 - 1) // 2

                def pv(e):
                    for hh in range(2):
                        for jt in range(nwt + 2):
                            if jt < nwt:
                                rhs = vt.ap[:, vi0 + jt, hh, :]
                            else:
                                rhs = v_cx.ap[:, jt - nwt, hh, :]
                            ins = e.matmul(pso.ap[0:64, hh * 65:(hh + 1) * 65], PT_.ap[:, jt, hh * 64:(hh + 1) * 64], rhs,
                                           start=(jt == 0), stop=(jt == nwt + 1))
                    return ins
                S.op("pe", pv, reads=[PT_, v_ev, v_od, v_cx], writes=[pso])
                pv3 = pso.ap[0:64, 0:130].rearrange("p (h d) -> p h d", d=65)
                rc_ = rcs[lr % 3]
                yt_ = ytms[(lr // 8) % 2]
                rr = lr % 8
                S.op("dve", lambda e: e.reciprocal(rc_.ap[:], pv3[:, :, 64]), reads=[pso], writes=[rc_])
                S.op("dve", lambda e: e.tensor_tensor(
                    yt_.ap[:, rr, :].rearrange("p (h d) -> p h d", d=64), pv3[:, :, 0:64],
                    rc_.ap[:].rearrange("p (h o) -> p h o", o=1).broadcast_to([64, 2, 64]), ALU.mult),
                    reads=[pso, rc_], writes=[yt_])
                if rr == 7:
                    r8 = lr // 8
                    psy = PS.get()

                    def try_(e):
                        for q_ in range(8):
                            ins = e.transpose(psy.bf[:, q_ * 64:(q_ + 1) * 64], yt_.ap[:, q_, :], ident_bf.ap[0:64, 0:64])
                        return ins
                    S.op("pe", try_, reads=[yt_, ident_bf], writes=[psy])
                    S.op("act", lambda e: e.activation(ynaT.ap[:, r8 * 512:(r8 + 1) * 512], psy.bf[:, 0:512], AF.Copy),
                         reads=[psy], writes=[ynaT])
            for step in range(32 + 4):
                if 0 <= step - 1 < 32:
                    st1(step - 1)
                if 0 <= step - 4 < 32:
                    st3(step - 4)
                if 0 <= step - 3 < 32:
                    st2(step - 3)
                if step < 32:
                    st0(step)
            S.dma("sp", y_na_d[pr], ynaT.ap[:], reads=[ynaT], writes=[y_na_t[pr]])

        for pr in range(8):
            na_pair(pr)
    if stage == 4:
        o_yna = dout("o_yna", [8, 128, NOWN], BF16)
        S.dma("sp", o_yna, y_na_d, reads=y_na_t, writes=[S.tr("o1")])
        S.finish()
        return nc

    x1_t = [S.tr("x1_%d" % i) for i in range(16)]

    def bcast_load(dst_ap, src_row_ap, tr):
        S.dma("sp", dst_ap, src_row_ap.partition_broadcast(128), writes=[tr])

    def ln_stats(src_buf, st, mv, rstd, nmr):
        for i in range(2):
            S.op("dve", lambda e, i=i: e.bn_stats(st.ap[:, i, :], src_buf.ap[:, i * 512:(i + 1) * 512]), reads=[src_buf], writes=[st])
        S.op("dve", lambda e: e.bn_aggr(mv.ap[:], st.ap[:].rearrange("p a b -> p (a b)")), reads=[st], writes=[mv])
        S.op("act", lambda e: e.activation(rstd.ap[:], mv.ap[:, 1:2], AF.Sqrt, bias=EPS, scale=1.0), reads=[mv], writes=[rstd])
        S.op("dve", lambda e: e.reciprocal(rstd.ap[:], rstd.ap[:]), reads=[rstd], writes=[rstd])
        S.op("dve", lambda e: e.scalar_tensor_tensor(nmr.ap[:], mv.ap[:, 0:1], -1.0, rstd.ap[:], ALU.mult, ALU.mult),
             reads=[mv, rstd], writes=[nmr])
    st_, mv_, rstd_, nmr_, _xb = work[0]
    gtm = S.sb("gtm", [128, 16, 65], F32)
    gtm_t = [S.tr("gtm%d" % i) for i in range(16)]
    S.op("pool", lambda e: e.memset(gtm.ap[:], 1.0), writes=gtm_t)
    with S.scope():
        STG = Stage(S, 4, 1024, "p5")
        bc = S.sb("bc5", [128, 3, D], F32)
        bc_t = S.tr("bc5t")
        S.dma("sp", bc.ap[:, 0, :], mod_d[0:1, 2048:3072].partition_broadcast(128), reads=[], writes=[bc_t])
        S.dma("sp", bc.ap[:, 1, :], lnp[0:1, :].partition_broadcast(128), writes=[bc_t])
        S.dma("sp", bc.ap[:, 2, :], lnp[1:2, :].partition_broadcast(128), writes=[bc_t])
        rb = S.sb("rb", [128, 64], F32)
        S.dma("sp", rb.ap[:], rbias.partition_broadcast(128), writes=[rb])
        Wr = S.sb("Wr", [128, 8, 64], F32)
        S.dma("sp", Wr.ap[:], w_r.rearrange("(k p) n -> p k n", p=128), writes=[Wr])
        WoB = S.sb("WoB", [128, 8, D], BF16)
        w_o_v = w_o.rearrange("(k p) n -> p k n", p=128)
        for q8 in range(8):
            STG.load_cast(w_o_v[:, :, q8 * 128:(q8 + 1) * 128], 1024, WoB.ap[:, :, q8 * 128:(q8 + 1) * 128], WoB)
        yhT = S.sb("yhT", [128, 8, 512], BF16)
        ynT = S.sb("ynT", [128, 8, 512], BF16)
        mT = S.sb("mT", [128, 8, 512], BF16)
        Wms = [S.sb("Wm%d" % i, [128, 8, 4, 128], BF16) for i in range(2)]
        sga = S.sb("sga", [128, 512], F32)
        sgb = S.sb("sgb", [128, 512], F32)
        tma = S.sb("tma", [128, 512], F32)
        tmb = S.sb("tmb", [128, 512], F32)
        Ab = [S.sb("Ab%d" % i, [128, D], F32) for i in range(2)]
        Bb = [S.sb("Bb%d" % i, [128, D], F32) for i in range(2)]
        h2fs = [S.sb("h2f%d" % i, [128, 8, 128], F32) for i in range(2)]
        rts = [(S.sb("scr%d" % i, [128, 64], F32), S.sb("sel%d" % i, [128, 64], F32), S.sb("selm%d" % i, [128, 64], F32),
                S.sb("m8_%d" % i, [128, 8, 8], F32), S.sb("gs%d" % i, [128, 8], F32), S.sb("gm8_%d" % i, [128, 8], F32),
                S.sb("pen%d" % i, [128, 8], F32), S.sb("e8_%d" % i, [128, 8], F32), S.sb("wsel%d" % i, [128, 64], F32),
                S.sb("wsum%d" % i, [128, 1], F32), S.sb("gts%d" % i, [128, 64], F32)) for i in range(2)]
        w_a_v = w_a.rearrange("(k p) n -> p k n", p=128)
        w_b_v = w_b.rearrange("(k p) n -> p k n", p=128)
        BIG = 1.0e9

        def quarter(qt):
            tok0 = qt * 512
            S.dma("sp", yhT.ap[:], y_hg_d[:, :, tok0:tok0 + 512].rearrange("h p t -> p h t"), reads=y_hg_t, writes=[yhT])
            S.dma("sp", ynT.ap[:], y_na_d[:, :, tok0:tok0 + 512].rearrange("h p t -> p h t"), reads=y_na_t, writes=[ynT])
            for c in range(8):
                Wm = Wms[c % 2]
                for g, srcv in enumerate((w_in_v[:, :, 8192 + c * 128:8192 + (c + 1) * 128], w_in_v[:, :, 9216 + c * 128:9216 + (c + 1) * 128],
                                          w_a_v[:, :, c * 128:(c + 1) * 128], w_b_v[:, :, c * 128:(c + 1) * 128])):
                    STG.load_cast(srcv, 1024, Wm.ap[:, :, g, :], Wm)
                pss = [PS.get() for _ in range(4)]

                def mm4(e, pss=pss, Wm=Wm):
                    for g in range(4):
                        for k in range(8):
                            if g < 2:
                                rhs = hT.ap[:, k, 256 + tok0:256 + tok0 + 512]
                            else:
                                rhs = (yhT if g == 2 else ynT).ap[:, k, :]
                            ins = e.matmul(pss[g].ap[:], Wm.ap[:, k, g, :], rhs, start=(k == 0), stop=(k == 7))
                    return ins
                S.op("pe", mm4, reads=[Wm, yhT, ynT] + hT_t[2 + qt * 4:2 + qt * 4 + 4], writes=pss)
                S.op("act", lambda e, pss=pss: e.activation(sga.ap[:], pss[0].ap[:], AF.Sigmoid), reads=[pss[0]], writes=[sga])
                S.op("act", lambda e, pss=pss: e.activation(sgb.ap[:], pss[1].ap[:], AF.Sigmoid), reads=[pss[1]], writes=[sgb])
                S.op("dve", lambda e, pss=pss: e.tensor_tensor(tma.ap[:], sga.ap[:], pss[2].ap[:], ALU.mult), reads=[sga, pss[2]], writes=[tma])
                S.op("dve", lambda e, pss=pss: e.tensor_tensor(tmb.ap[:], sgb.ap[:], pss[3].ap[:], ALU.mult), reads=[sgb, pss[3]], writes=[tmb])
                S.op("dve", lambda e, c=c: e.tensor_tensor(mT.ap[:, c, :], tma.ap[:], tmb.ap[:], ALU.add), reads=[tma, tmb], writes=[mT])
            def tile_ops(t4):
                tile_ = qt * 4 + t4
                bs = tile_ % 2
                xt = xts[bs]
                A_ = Ab[bs]
                B_ = Bb[bs]
                h2f = h2fs[bs]
                st_, mv_, rstd_, nmr_, _x = work[bs]
                scr, sel, selm, m8, gs, gm8, pen, e8, wsel, wsum, gts = rts[bs]
                c = {}
                ops = []
                ops.append(lambda: S.dma("sp", xt.ap[:], x_own[tile_ * 128:(tile_ + 1) * 128, :], writes=[xt]))

                def o_mmy():
                    c["psy"] = [PS.reserve(), PS.reserve()]
                    psy = c["psy"]

                    def mmy(e):
                        for hh in range(2):
                            for k in range(8):
                                ins = e.matmul(psy[hh].ap[:], mT.ap[:, k, t4 * 128:(t4 + 1) * 128], WoB.ap[:, k, hh * 512:(hh + 1) * 512],
                                               start=(k == 0), stop=(k == 7))
                        return ins
                    S.op("pe", mmy, reads=[mT, WoB], writes=psy)
                ops.append(o_mmy)
                for hh in range(2):
                    def o_g1(hh=hh):
                        psy = c["psy"]
                        S.op("dve", lambda e: e.tensor_tensor(
                            A_.ap[:, hh * 512:(hh + 1) * 512], psy[hh].ap[:], bc.ap[:, 0, hh * 512:(hh + 1) * 512], ALU.mult),
                            reads=[psy[hh], bc_t], writes=[A_])
                        PS.release(psy[hh])
                    ops.append(o_g1)
                ops.append(lambda: S.op("dve", lambda e: e.scalar_tensor_tensor(A_.ap[:], xt.ap[:], ALPHA, A_.ap[:], ALU.mult, ALU.add),
                                        reads=[xt, A_], writes=[A_]))

                def stats_ops(buf):
                    for i in range(2):
                        ops.append(lambda i=i: S.op("dve", lambda e: e.bn_stats(st_.ap[:, i, :], buf.ap[:, i * 512:(i + 1) * 512]),
                                                    reads=[buf], writes=[st_]))
                    ops.append(lambda: S.op("dve", lambda e: e.bn_aggr(mv_.ap[:], st_.ap[:].rearrange("p a b -> p (a b)")), reads=[st_], writes=[mv_]))
                    ops.append(lambda: S.op("act", lambda e: e.activation(rstd_.ap[:], mv_.ap[:, 1:2], AF.Sqrt, bias=EPS, scale=1.0),
                                            reads=[mv_], writes=[rstd_]))
                    ops.append(lambda: S.op("dve", lambda e: e.reciprocal(rstd_.ap[:], rstd_.ap[:]), reads=[rstd_], writes=[rstd_]))
                    ops.append(lambda: S.op("dve", lambda e: e.scalar_tensor_tensor(nmr_.ap[:], mv_.ap[:, 0:1], -1.0, rstd_.ap[:], ALU.mult, ALU.mult),
                                            reads=[mv_, rstd_], writes=[nmr_]))
                stats_ops(A_)
                ops.append(lambda: S.op("act", lambda e: e.activation(A_.ap[:], A_.ap[:], AF.Identity, bias=nmr_.ap[:], scale=rstd_.ap[:]),
                                        reads=[A_, nmr_, rstd_], writes=[A_]))
                ops.append(lambda: S.op("dve", lambda e: e.tensor_tensor(A_.ap[:], A_.ap[:], bc.ap[:, 1, :], ALU.mult), reads=[A_, bc_t], writes=[A_]))
                ops.append(lambda: S.op("dve", lambda e: e.tensor_tensor(A_.ap[:], A_.ap[:], bc.ap[:, 2, :], ALU.add), reads=[A_, bc_t], writes=[A_]))
                ops.append(lambda: S.dma("sp", x1_d[tile_ * 128:(tile_ + 1) * 128, :], A_.ap[:], reads=[A_], writes=[x1_t[tile_]]))
                stats_ops(A_)
                ops.append(lambda: S.op("act", lambda e: e.activation(B_.ap[:], A_.ap[:], AF.Identity, bias=nmr_.ap[:], scale=rstd_.ap[:]),
                                        reads=[A_, nmr_, rstd_], writes=[B_]))

                def o_trf():
                    c["pst"] = [PS.reserve(), PS.reserve()]
                    pst = c["pst"]

                    def trf(e):
                        for k in range(8):
                            ins = e.transpose(pst[k // 4].ap[:, (k % 4) * 128:(k % 4 + 1) * 128], B_.ap[:, k * 128:(k + 1) * 128], ident_f.ap[:])
                        return ins
                    S.op("pe", trf, reads=[B_, ident_f], writes=pst)
                ops.append(o_trf)
                for k in range(8):
                    def o_ev(k=k):
                        pst = c["pst"]
                        src_ap = pst[k // 4].ap[:, (k % 4) * 128:(k % 4 + 1) * 128]
                        if k % 2 == 0:
                            S.op("act", lambda e: e.activation(
                                h2f.ap[:, k, :], src_ap, AF.Identity, bias=cols.ap[:, 5, k:k + 1], scale=cols.ap[:, 4, k:k + 1]),
                                reads=[pst[k // 4], cols], writes=[h2f])
                        else:
                            S.op("dve", lambda e: e.tensor_scalar(
                                h2f.ap[:, k, :], src_ap, cols.ap[:, 4, k:k + 1], cols.ap[:, 5, k:k + 1], ALU.mult, ALU.add),
                                reads=[pst[k // 4], cols], writes=[h2f])
                        if k == 3:
                            PS.release(pst[0])
                        if k == 7:
                            PS.release(pst[1])
                    ops.append(o_ev)
                ops.append(lambda: S.op("act", lambda e: e.activation(hT.ap[:, :, 256 + tile_ * 128:256 + (tile_ + 1) * 128], h2f.ap[:], AF.Copy),
                                        reads=[h2f], writes=[hT_t[2 + tile_]]))

                def o_mml():
                    c["psl"] = PS.reserve()
                    psl = c["psl"]

                    def mml(e):
                        for k in range(8):
                            ins = e.matmul(psl.ap[:, 0:64], h2f.ap[:, k, :], Wr.ap[:, k, :], start=(k == 0), stop=(k == 7))
                        return ins
                    S.op("pe", mml, reads=[h2f, Wr], writes=[psl])
                ops.append(o_mml)

                def o_sig():
                    psl = c["psl"]
                    S.op("act", lambda e: e.activation(scr.ap[:], psl.ap[:, 0:64], AF.Sigmoid), reads=[psl], writes=[scr])
                    PS.release(psl)
                ops.append(o_sig)
                ops.append(lambda: S.op("dve", lambda e: e.tensor_tensor(sel.ap[:], scr.ap[:], rb.ap[:], ALU.add), reads=[scr, rb], writes=[sel]))
                for g in range(8):
                    ops.append(lambda g=g: S.op("dve", lambda e: e.max(m8.ap[:, g, :], sel.ap[:, g * 8:(g + 1) * 8]), reads=[sel], writes=[m8]))
                ops.append(lambda: S.op("dve", lambda e: e.tensor_tensor(gs.ap[:], m8.ap[:, :, 0], m8.ap[:, :, 1], ALU.add), reads=[m8], writes=[gs]))
                ops.append(lambda: S.op("dve", lambda e: e.max(gm8.ap[:], gs.ap[:]), reads=[gs], writes=[gm8]))
                ops.append(lambda: S.op("dve", lambda e: e.tensor_scalar(pen.ap[:], gs.ap[:], gm8.ap[:, 3:4], None, ALU.is_ge), reads=[gs, gm8], writes=[pen]))
                ops.append(lambda: S.op("dve", lambda e: e.tensor_scalar(pen.ap[:], pen.ap[:], BIG, -BIG, ALU.mult, ALU.add), reads=[pen], writes=[pen]))
                ops.append(lambda: S.op("dve", lambda e: e.tensor_tensor(
                    selm.ap[:].rearrange("p (g j) -> p g j", j=8), sel.ap[:].rearrange("p (g j) -> p g j", j=8),
                    pen.ap[:].rearrange("p (g o) -> p g o", o=1).broadcast_to([128, 8, 8]), ALU.add), reads=[sel, pen], writes=[selm]))
                ops.append(lambda: S.op("dve", lambda e: e.max(e8.ap[:], selm.ap[:]), reads=[selm], writes=[e8]))
                ops.append(lambda: S.op("dve", lambda e: e.tensor_scalar(wsel.ap[:], selm.ap[:], e8.ap[:, 7:8], None, ALU.is_ge), reads=[selm, e8], writes=[wsel]))
                ops.append(lambda: S.op("dve", lambda e: e.tensor_tensor(wsel.ap[:], wsel.ap[:], scr.ap[:], ALU.mult), reads=[wsel, scr], writes=[wsel]))
                ops.append(lambda: S.op("dve", lambda e: e.tensor_reduce(wsum.ap[:], wsel.ap[:], AX.X, ALU.add), reads=[wsel], writes=[wsum]))
                ops.append(lambda: S.op("dve", lambda e: e.reciprocal(wsum.ap[:], wsum.ap[:]), reads=[wsum], writes=[wsum]))
                ops.append(lambda: S.op("dve", lambda e: e.tensor_scalar(gtm.ap[:, tile_, 0:64], wsel.ap[:], wsum.ap[:, 0:1], 2.5, ALU.mult, ALU.mult),
                                        reads=[wsel, wsum], writes=[gtm_t[tile_]]))
                return ops
            for pair in range(2):
                interleave([tile_ops(pair * 2), tile_ops(pair * 2 + 1)])
        for qt in range(4):
            quarter(qt)
    if stage == 5:
        o_x1 = dout("o_x1", [NOWN, D])
        o_g = dout("o_g", [128, 16 * 65])
        S.dma("sp", o_x1, x1_d, reads=x1_t, writes=[S.tr("o1")])
        S.dma("sp", o_g, gtm.ap[:].rearrange("p a b -> p (a b)"), reads=gtm_t, writes=[S.tr("o2")])
        o_h2 = dout("o_h2", [128, 8 * NOWN], BF16)
        S.dma("sp", o_h2.rearrange("p (k t) -> p k t", k=8), hT.ap[:, :, 256:256 + NOWN], reads=hT_t, writes=[S.tr("o3")])
        S.finish()
        return nc

    outer = S.scope()
    outer.__enter__()
    y2 = S.sb("y2", [128, 16, D], F32)
    y2_t = [S.tr("y2_%d" % i) for i in range(16)]
    with S.scope():
        STG = Stage(S, 4, 2048, "p6")
        Wg = [S.sb("Wg%d" % i, [128, 8, 256], BF16) for i in range(2)]
        Wu = [S.sb("Wu%d" % i, [128, 8, 256], BF16) for i in range(2)]
        Wd = [S.sb("Wd%d" % i, [128, 2, D], BF16) for i in range(2)]
        sa = [S.sb("sa%d" % i, [128, 512], F32) for i in range(4)]
        actT = [S.sb("actT%d" % i, [128, 2, 512], BF16) for i in range(3)]

        def load_expert(e):
            b = e % 2
            STG.load_cast(w_eg[e].rearrange("(k p) n -> p k n", p=128), 2048, Wg[b].ap[:], Wg[b])
            STG.load_cast(w_eu[e].rearrange("(k p) n -> p k n", p=128), 2048, Wu[b].ap[:], Wu[b])
            STG.load_cast(w_ed[e].rearrange("(k p) n -> p k n", p=128), 2048, Wd[b].ap[:], Wd[b])

        NE = 65
        uctx = {}

        def u0_ops(u):
            e, tb = u // 4, u % 4
            b = e % 2
            pp = []
            uctx[u] = pp
            ops = []
            for i in range(8):
                def chunk(i=i):
                    fc, sub = i // 4, i % 4
                    if sub == 0:
                        pp.append((PS.reserve(), PS.reserve()))
                    psa, psu = pp[fc]
                    dstp = psa if sub < 2 else psu
                    W = Wg[b] if sub < 2 else Wu[b]
                    k0 = (sub % 2) * 4

                    def mm(en):
                        for k in range(k0, k0 + 4):
                            ins = en.matmul(dstp.ap[:], W.ap[:, k, fc * 128:(fc + 1) * 128], hT.ap[:, k, 256 + tb * 512:256 + (tb + 1) * 512],
                                            start=(k == 0), stop=(k == 7))
                        return ins
                    S.op("pe", mm, reads=[W] + hT_t[2 + tb * 4:2 + tb * 4 + 4], writes=[dstp])
                ops.append(chunk)
            return ops

        def u1(u):
            pp = uctx.pop(u)
            A = actT[u % 3]
            for fc in range(2):
                psa, psu = pp[fc]
                s_ = sa[(u % 2) * 2 + fc]
                S.op("act", lambda en, psa=psa, s_=s_: en.activation(s_.ap[:], psa.ap[:], AF.Silu), reads=[psa], writes=[s_])
                S.op("dve", lambda en, psu=psu, s_=s_, fc=fc: en.tensor_tensor(A.ap[:, fc, :], s_.ap[:], psu.ap[:], ALU.mult),
                     reads=[s_, psu], writes=[A])
                PS.release(psa)
                PS.release(psu)

        def u2_ops(u):
            e, tb = u // 4, u % 4
            b = e % 2
            A = actT[u % 3]
            ops = []
            for tt in range(4):
                for dh in range(2):
                    def dn(tt=tt, dh=dh):
                        tile_ = tb * 4 + tt
                        psd = PS.get()

                        def mmd(en):
                            for fc in range(2):
                                ins = en.matmul(psd.ap[:], A.ap[:, fc, tt * 128:(tt + 1) * 128], Wd[b].ap[:, fc, dh * 512:(dh + 1) * 512],
                                                start=(fc == 0), stop=(fc == 1))
                            return ins
                        S.op("pe", mmd, reads=[A, Wd[b]], writes=[psd])
                        dst = y2.ap[:, tile_, dh * 512:(dh + 1) * 512]
                        gcol = gtm.ap[:, tile_, e:e + 1]
                        if e == 0:
                            S.op("act", lambda en: en.activation(dst, psd.ap[:], AF.Identity, scale=gcol),
                                 reads=[psd, gtm_t[tile_]], writes=[y2_t[tile_]])
                        else:
                            S.op("dve", lambda en: en.scalar_tensor_tensor(dst, psd.ap[:], gcol, dst, ALU.mult, ALU.add),
                                 reads=[psd, gtm_t[tile_], y2_t[tile_]], writes=[y2_t[tile_]])
                    ops.append(dn)
            return ops
        load_expert(0)
        NU = NE * 4
        for step in range(NU + 2):
            if 0 <= step - 1 < NU:
                u1(step - 1)
            la = u2_ops(step - 2) if 0 <= step - 2 < NU else []
            lb = u0_ops(step) if step < NU else []
            interleave([la, lb])
            if step < NU and step % 4 == 1 and step // 4 + 1 < NE:
                load_expert(step // 4 + 1)

    with S.scope():
        bc7 = S.sb("bc7", [128, 3, D], F32)
        bc7_t = S.tr("bc7t")
        S.dma("sp", bc7.ap[:, 0, :], mod_d[0:1, 5120:6144].partition_broadcast(128), writes=[bc7_t])
        S.dma("sp", bc7.ap[:, 1, :], lnp[2:3, :].partition_broadcast(128), writes=[bc7_t])
        S.dma("sp", bc7.ap[:, 2, :], lnp[3:4, :].partition_broadcast(128), writes=[bc7_t])
        tmp7s = [S.sb("tmp7_%d" % i, [128, D], F32) for i in range(2)]
        r7s = [S.sb("r7_%d" % i, [128, D], F32) for i in range(2)]
        o7 = [S.sb("o7_%d" % i, [128, D], F32) for i in range(2)]
        out_t = S.tr("out")
        def fin_ops(tile_):
            bs = tile_ % 2
            xt = xts[bs]
            tmp7 = tmp7s[bs]
            r7 = r7s[bs]
            ob = o7[bs]
            st_, mv_, rstd_, nmr_, _x = work[bs]
            ops = []

            def add(*a_, **k_):
                ops.append(lambda: S.op(*a_, **k_))
            ops.append(lambda: S.dma("sp", xt.ap[:], x1_d[tile_ * 128:(tile_ + 1) * 128, :], reads=[x1_t[tile_]], writes=[xt]))
            add("dve", lambda e: e.tensor_tensor(tmp7.ap[:], y2.ap[:, tile_, :], bc7.ap[:, 0, :], ALU.mult),
                reads=[y2_t[tile_], bc7_t], writes=[tmp7])
            add("dve", lambda e: e.scalar_tensor_tensor(r7.ap[:], xt.ap[:], ALPHA, tmp7.ap[:], ALU.mult, ALU.add),
                reads=[xt, tmp7], writes=[r7])
            for i in range(2):
                add("dve", lambda e, i=i: e.bn_stats(st_.ap[:, i, :], r7.ap[:, i * 512:(i + 1) * 512]), reads=[r7], writes=[st_])
            add("dve", lambda e: e.bn_aggr(mv_.ap[:], st_.ap[:].rearrange("p a b -> p (a b)")), reads=[st_], writes=[mv_])
            add("act", lambda e: e.activation(rstd_.ap[:], mv_.ap[:, 1:2], AF.Sqrt, bias=EPS, scale=1.0), reads=[mv_], writes=[rstd_])
            add("dve", lambda e: e.reciprocal(rstd_.ap[:], rstd_.ap[:]), reads=[rstd_], writes=[rstd_])
            add("dve", lambda e: e.scalar_tensor_tensor(nmr_.ap[:], mv_.ap[:, 0:1], -1.0, rstd_.ap[:], ALU.mult, ALU.mult),
                reads=[mv_, rstd_], writes=[nmr_])
            add("act", lambda e: e.activation(r7.ap[:], r7.ap[:], AF.Identity, bias=nmr_.ap[:], scale=rstd_.ap[:]),
                reads=[r7, nmr_, rstd_], writes=[r7])
            add("dve", lambda e: e.tensor_tensor(tmp7.ap[:], r7.ap[:], bc7.ap[:, 1, :], ALU.mult), reads=[r7, bc7_t], writes=[tmp7])
            add("dve", lambda e: e.tensor_tensor(ob.ap[:], tmp7.ap[:], bc7.ap[:, 2, :], ALU.add), reads=[tmp7, bc7_t], writes=[ob])
            ops.append(lambda: S.dma("sp", out_d[tile_ * 128:(tile_ + 1) * 128, :], ob.ap[:], reads=[ob], writes=[out_t]))
            return ops
        for pair in range(8):
            interleave([fin_ops(2 * pair), fin_ops(2 * pair + 1)])
    outer.__exit__(None, None, None)
    S.finish()
    return nc


def _consts():
    ident = np.eye(128, dtype=np.float32)
    Rm = np.zeros((128, 128), np.float32)
    for m in range(128):
        w = m % 32
        if w < 16:
            Rm[m + 16, m] = -1.0
        else:
            Rm[m - 16, m] = 1.0
    j = np.arange(128)[:, None]
    i = np.arange(128)[None, :]
    valid = (j < 64) & (i < 64)
    mF = (valid & (j <= i)).astype(np.float32)[:, 0:64]
    mB = (valid & (j >= i)).astype(np.float32)[:, 0:64]
    cb = np.concatenate([ident, Rm] + [mF, mB] * 4, axis=1).astype(ml_dtypes.bfloat16)
    return cb, ident


def col_layout(v):
    return np.ascontiguousarray(v.reshape(-1, 128).T)


def prep_inputs(inp, cid):
    b, j = cid // 4, cid % 4
    x = inp["x"][b]
    tok0 = 2048 * j
    m = {}
    m["x_own"] = np.ascontiguousarray(x[tok0:tok0 + 2048])
    halo = np.zeros((512, D), np.float32)
    if j > 0:
        halo[0:256] = x[tok0 - 256:tok0]
    if j < 3:
        halo[256:512] = x[tok0 + 2048:tok0 + 2048 + 256]
    m["x_halo"] = halo
    ctx = inp["ctx"][b]
    m["ctx2"] = np.ascontiguousarray(np.concatenate([ctx, ctx[::-1]], axis=0))
    segs = []
    wfs = []
    lbrows = [inp["hg_lb_fwd"][0], inp["hg_lb_fwd"][1], inp["hg_lb_bwd"][0], inp["hg_lb_bwd"][1]]
    fl = np.zeros((128, 6), np.float32)
    w_in = inp["w_in"][0]
    order = [("f", s) for s in range(0, j)] + [("b", s) for s in range(3, j, -1)]
    for si, (d_, s) in enumerate(order):
        seg = x[2048 * s:2048 * (s + 1)]
        if d_ == "f":
            segs.append(seg)
            wfs.append(w_in[:, 1024:2048])
            lbrows += [inp["hg_lb_fwd"][0], inp["hg_lb_fwd"][1]]
            fl[:, si] = 1.0
        else:
            segs.append(seg[::-1])
            wfs.append(w_in[:, 2048:3072])
            lbrows += [inp["hg_lb_bwd"][0], inp["hg_lb_bwd"][1]]
            fl[:, 3 + si] = 1.0
    m["x_slots"] = np.ascontiguousarray(np.concatenate(segs, axis=0))
    m["wf_slots"] = np.ascontiguousarray(np.stack(wfs))
    lbr = np.stack(lbrows)
    m["lbT"] = np.ascontiguousarray(lbr.reshape(10, 8, 128).transpose(2, 0, 1).reshape(128, 80))
    m["flags"] = fl
    cv = np.stack([inp["c"][b], inp["c_ctx"]])
    m["cvecT"] = np.ascontiguousarray(cv.reshape(2, 8, 128).transpose(2, 1, 0).reshape(128, 16))
    m["w_ada"] = inp["w_ada"][0]
    m["b_row"] = inp["b_ada"][0:1]
    m["b_col"] = col_layout(inp["b_ada"][0])
    m["w_in"] = w_in
    m["hgT"] = col_layout(inp["hg_norm_g"][0])
    m["w_a"] = inp["w_branch_a"][0]
    m["w_b"] = inp["w_branch_b"][0]
    m["w_o"] = inp["w_out"][0]
    m["lnp"] = np.ascontiguousarray(np.stack([inp["ln1_g"][0], inp["ln1_b"][0], inp["ln2_g"][0], inp["ln2_b"][0]]))
    m["w_r"] = inp["w_router"][0]
    m["rbias"] = inp["router_bias"][0:1]
    m["w_eg"] = inp["_w_eg"]
    m["w_eu"] = inp["_w_eu"]
    m["w_ed"] = inp["_w_ed"]
    t = np.arange(NKB)
    g = 2048 * j - 256 + t
    row = np.floor_divide(g, 64).astype(np.float32)
    col = np.mod(g, 64).astype(np.float32)
    inv = np.power(np.float32(10000.0), -np.arange(0, 32, 2, dtype=np.float32) / np.float32(32)).astype(np.float32)
    dd = np.arange(64)
    pos = np.where((dd < 32)[:, None], row[None, :], col[None, :]).astype(np.float32)
    ang = (pos * inv[dd % 16][:, None]).astype(np.float32)
    m["cosT"] = np.ascontiguousarray(np.tile(np.cos(ang).astype(np.float32), (2, 1)))
    m["sinT"] = np.ascontiguousarray(np.tile(np.sin(ang).astype(np.float32), (2, 1)))
    rpb = inp["na_rpb"][0]
    qq = np.arange(64)[:, None]
    kc = np.arange(64)[None, :]
    cs = np.clip(qq - 8, 0, 48)
    inwin = (kc >= cs) & (kc < cs + 16)
    dc = np.clip(kc - qq + 15, 0, 30)
    NEG = np.float32(-1e30)

    def Bfor(head, drs):
        out = np.full((64, len(drs), 64), NEG, np.float32)
        for s_, dr in enumerate(drs):
            if dr is not None and 0 <= dr <= 14:
                out[:, s_, :] = np.where(inwin, rpb[head, dr][dc], NEG)
        return out.reshape(64, -1)
    Bint = np.zeros((8, 128, 512), np.float32)
    Bspec = np.zeros((8, 128, 7, 768), np.float32)
    for pr in range(8):
        for hh in range(2):
            head = pr * 2 + hh
            Bint[pr, hh * 64:(hh + 1) * 64] = Bfor(head, list(range(3, 11)))
            for si, lr in enumerate([0, 1, 2, 3, 29, 30, 31]):
                r = 32 * j + lr
                rs_ = int(np.clip(r - 4, 0, 120))
                rel0 = 0 if lr < 4 else 27
                drs = []
                for s_ in range(12):
                    kg = 32 * j - 4 + rel0 + s_
                    drs.append(kg - r + 7 if rs_ <= kg < rs_ + 8 else None)
                Bspec[pr, hh * 64:(hh + 1) * 64, si] = Bfor(head, drs)
    m["Bint"] = Bint
    m["Bspec"] = np.ascontiguousarray(Bspec.reshape(8, 128, 7 * 768))
    cb, ident = _consts()
    m["consts_bf"] = cb
    m["ident_f"] = ident
    return m


_CACHE = {}


def kernel(**inputs):
    inp = {k: np.asarray(v) for k, v in inputs.items()}
    inp["_w_eg"] = np.ascontiguousarray(np.concatenate([inp["w_e_gate"][0], inp["w_sh_gate"]], axis=0))
    inp["_w_eu"] = np.ascontiguousarray(np.concatenate([inp["w_e_up"][0], inp["w_sh_up"]], axis=0))
    inp["_w_ed"] = np.ascontiguousarray(np.concatenate([inp["w_e_down"][0], inp["w_sh_down"]], axis=0))
    if "nc" not in _CACHE:
        _CACHE["nc"] = build_program(99)
    nc = _CACHE["nc"]
    maps = [prep_inputs(inp, c) for c in range(8)]
    res = run_bass_kernel_spmd(nc, maps, core_ids=list(range(8)))
    out = np.zeros((2, 8192, D), np.float32)
    for c in range(8):
        b, j = c // 4, c % 4
        out[b, 2048 * j:2048 * (j + 1)] = res.results[c]["out"]
    return out
```

```python
import contextlib
import numpy as np
import ml_dtypes
import concourse.bass as bass
import concourse.mybir as mybir
from concourse.bass_utils import run_bass_kernel_spmd

F32 = mybir.dt.float32
BF16 = mybir.dt.bfloat16
AF = mybir.ActivationFunctionType
ALU = mybir.AluOpType
AX = mybir.AxisListType


class Tr:
    def __init__(self, name):
        self.name = name
        self.w = None
        self.r = {}
        self.dsem = None


class Buf(Tr):
    def __init__(self, name, handle):
        super().__init__(name)
        self.h = handle
        self.ap = handle[:] if not hasattr(handle, "ap") or not callable(getattr(handle, "ap")) else handle.ap()


class Sched:
    ENG = ("pe", "act", "dve", "pool", "sp")

    def __init__(self, nc):
        self.nc = nc
        self.gstack = contextlib.ExitStack()
        self.stack = self.gstack
        self.prog = {e: [] for e in self.ENG}
        self.sem = {e: self.gstack.enter_context(nc.semaphore("s_" + e)) for e in self.ENG}
        self.cnt = {e: 0 for e in self.ENG}
        self.seen = {e: {} for e in self.ENG}
        self.dsems = []
        self.dfree = []
        self.scope_trs = [[]]
        self.ninst = 0

    def sb(self, name, shape, dtype):
        h = self.stack.enter_context(self.nc.sbuf_tensor(name, list(shape), dtype))
        return Buf(name, h)

    def ps(self, name, shape, dtype):
        h = self.stack.enter_context(self.nc.psum_tensor(name, list(shape), dtype))
        return Buf(name, h)

    def tr(self, name="t"):
        return Tr(name)

    def _need(self, eng, reads, writes, skip_pe_waw=False):
        need = {}

        def add(key, semh, val):
            if key not in need or need[key][1] < val:
                need[key] = (semh, val)

        for t in reads:
            if t.w is not None:
                add(*t.w)
        for t in writes:
            if t.w is not None:
                if not (skip_pe_waw and t.w[0] == "pe"):
                    add(*t.w)
            for key, (semh, val) in t.r.items():
                add(key, semh, val)
        out = []
        for key, (semh, val) in need.items():
            if self.seen[eng].get(key, 0) < val:
                self.seen[eng][key] = val
                out.append((semh, val))
        return out

    def _emit_waits(self, eng, waits):
        for semh, val in waits:
            self.prog[eng].append(lambda e, semh=semh, val=val: e.wait_ge(semh, val))

    def op(self, eng, fn, reads=(), writes=()):
        waits = self._need(eng, reads, writes, skip_pe_waw=(eng == "pe"))
        self._emit_waits(eng, waits)
        self.cnt[eng] += 1
        self.ninst += 1
        val = self.cnt[eng]
        semh = self.sem[eng]
        self.prog[eng].append(lambda e, fn=fn, semh=semh: fn(e).then_inc(semh, 1))
        rec = (eng, semh, val)
        for t in reads:
            t.r[eng] = (semh, val)
        for t in writes:
            t.w = rec
            t.r = {}

    def dma(self, eng, out_ap, in_ap, reads=(), writes=()):
        waits = self._need(eng, reads, writes)
        self._emit_waits(eng, waits)
        t0 = writes[0]
        if t0.dsem is None:
            if self.dfree:
                t0.dsem = self.dfree.pop()
            else:
                h = self.gstack.enter_context(self.nc.semaphore("d%d" % len(self.dsems)))
                self.dsems.append([h, 0])
                t0.dsem = len(self.dsems) - 1
            self.scope_trs[-1].append(t0)
        rec = self.dsems[t0.dsem]
        rec[1] += 16
        semh, val = rec[0], rec[1]
        key = ("d", t0.dsem)
        self.prog[eng].append(lambda e, o=out_ap, i=in_ap, semh=semh: e.dma_start(out=o, in_=i).then_inc(semh, 16))
        for t in reads:
            t.r[key] = (semh, val)
        for t in writes:
            t.w = (key, semh, val)
            t.r = {}

    def barrier(self):
        for eng in self.ENG:
            for e2 in ("pe", "act", "dve", "pool"):
                if self.cnt[e2] and self.seen[eng].get(e2, 0) < self.cnt[e2]:
                    self.seen[eng][e2] = self.cnt[e2]
                    self.prog[eng].append(lambda e, semh=self.sem[e2], val=self.cnt[e2]: e.wait_ge(semh, val))
            for i, (h, c) in enumerate(self.dsems):
                key = ("d", i)
                if c and self.seen[eng].get(key, 0) < c:
                    self.seen[eng][key] = c
                    self.prog[eng].append(lambda e, semh=h, val=c: e.wait_ge(semh, val))

    def flush(self):
        nc = self.nc
        progs = self.prog
        self.prog = {e: [] for e in self.ENG}
        self.nblk = getattr(self, "nblk", 0) + 1
        with nc.named_scope("blk%d" % self.nblk), nc.Block() as block:
            @block.tensor
            def _(e):
                for f in progs["pe"]:
                    f(e)

            @block.scalar
            def _(e):
                for f in progs["act"]:
                    f(e)

            @block.vector
            def _(e):
                for f in progs["dve"]:
                    f(e)

            @block.gpsimd
            def _(e):
                for f in progs["pool"]:
                    f(e)

            @block.sync
            def _(e):
                for f in progs["sp"]:
                    f(e)

    @contextlib.contextmanager
    def scope(self):
        old = self.stack
        self.stack = contextlib.ExitStack()
        self.scope_trs.append([])
        try:
            yield
        finally:
            self.barrier()
            self.flush()
            for t in self.scope_trs.pop():
                self.dfree.append(t.dsem)
                t.dsem = None
            self.stack.close()
            self.stack = old

    def finish(self):
        self.barrier()
        self.flush()
        self.gstack.close()


D = 1024
NOWN = 2048
NKB = 2560
ALPHA = 2.0 ** 0.25
EPS = 1e-6


def interleave(lists):
    n = max(len(l) for l in lists)
    for i in range(n):
        for l in lists:
            if i < len(l):
                l[i]()


def pipeline(n, stages):
    ns = len(stages)
    for step in range(n + ns - 1):
        for s in range(ns - 1, -1, -1):
            i = step - s
            if 0 <= i < n:
                stages[s](i)


class PsumPool:
    def __init__(self, S):
        self.banks = [S.ps("psb%d" % i, [128, 512], F32) for i in range(8)]
        for b in self.banks:
            b.bf = b.ap.bitcast(BF16)
        self.i = 0
        self.reserved = set()

    def get(self):
        while True:
            b = self.banks[self.i]
            self.i = (self.i + 1) % 8
            if id(b) not in self.reserved:
                return b

    def reserve(self):
        b = self.get()
        self.reserved.add(id(b))
        return b

    def release(self, b):
        self.reserved.discard(id(b))


class Stage:
    def __init__(self, S, n=4, size=2048, tag=""):
        self.S = S
        self.slots = [S.sb("stg%s_%d" % (tag, i), [128, size], F32) for i in range(n)]
        self.i = 0
        self.size = size

    def load(self, src_ap, nfree):
        s = self.slots[self.i]
        self.i = (self.i + 1) % len(self.slots)
        p = src_ap.shape[0]
        view = s.ap[0:p, 0:nfree]
        if len(src_ap.shape) == 3:
            view = view.rearrange("p (a b) -> p a b", a=src_ap.shape[1])
        self.S.dma("sp", view, src_ap, writes=[s])
        return s, view

    def load_cast(self, src_ap, nfree, dst_ap, dst_tr, eng="act"):
        s, view = self.load(src_ap, nfree)
        if eng == "act":
            self.S.op("act", lambda e, o=dst_ap, i=view: e.activation(o, i, AF.Copy), reads=[s], writes=[dst_tr])
        else:
            self.S.op(eng, lambda e, o=dst_ap, i=view: e.tensor_copy(o, i), reads=[s], writes=[dst_tr])


def ln_tokens(S, PS, ident_bf, src, T, dst_fn, dst_trs, sc_ap_fn, sh_ap_fn, col_tr, xts, work):
    nt = T // 128

    def stA(t):
        st, mv, rstd, nmr, xb = work[t % len(work)]
        xt = xts[t % len(xts)]
        S.dma("sp", xt.ap[:], src[t * 128:(t + 1) * 128, :], writes=[xt])
        for i in range(2):
            S.op("dve", lambda e, i=i: e.bn_stats(st.ap[:, i, :], xt.ap[:, i * 512:(i + 1) * 512]), reads=[xt], writes=[st])
        S.op("dve", lambda e: e.bn_aggr(mv.ap[:], st.ap[:].rearrange("p a b -> p (a b)")), reads=[st], writes=[mv])
        S.op("act", lambda e: e.activation(rstd.ap[:], mv.ap[:, 1:2], AF.Sqrt, bias=EPS, scale=1.0), reads=[mv], writes=[rstd])
        S.op("dve", lambda e: e.reciprocal(rstd.ap[:], rstd.ap[:]), reads=[rstd], writes=[rstd])
        S.op("dve", lambda e: e.scalar_tensor_tensor(nmr.ap[:], mv.ap[:, 0:1], -1.0, rstd.ap[:], ALU.mult, ALU.mult),
             reads=[mv, rstd], writes=[nmr])
        S.op("act", lambda e: e.activation(xb.ap[:], xt.ap[:], AF.Identity, bias=nmr.ap[:], scale=rstd.ap[:]),
             reads=[xt, nmr, rstd], writes=[xb])

    def stB(t):
        st, mv, rstd, nmr, xb = work[t % len(work)]
        ps = PS.get()

        def tr(e):
            for k in range(8):
                ins = e.transpose(ps.bf[:, k * 128:(k + 1) * 128], xb.ap[:, k * 128:(k + 1) * 128], ident_bf.ap[:])
            return ins
        S.op("pe", tr, reads=[xb, ident_bf], writes=[ps])
        for k in range(8):
            if k % 2 == 0:
                S.op("act", lambda e, k=k: e.activation(
                    dst_fn(k, t), ps.bf[:, k * 128:(k + 1) * 128], AF.Identity, bias=sh_ap_fn(k), scale=sc_ap_fn(k)),
                    reads=[ps, col_tr], writes=[dst_trs[t]])
            else:
                S.op("dve", lambda e, k=k: e.tensor_scalar(
                    dst_fn(k, t), ps.bf[:, k * 128:(k + 1) * 128], sc_ap_fn(k), sh_ap_fn(k), ALU.mult, ALU.add),
                    reads=[ps, col_tr], writes=[dst_trs[t]])
    for step in range(nt + 1):
        if step < nt:
            stA(step)
        if step >= 1:
            stB(step - 1)


def build_program(stage=99):
    nc = bass.Bass("TRN2", target_bir_lowering=False)

    def din(name, shape, dt=F32):
        return nc.dram_tensor(name, list(shape), dt, kind="ExternalInput").ap()

    def dscr(name, shape, dt=F32):
        return nc.dram_tensor(name, list(shape), dt, kind="Internal").ap()

    def dout(name, shape, dt=F32):
        return nc.dram_tensor(name, list(shape), dt, kind="ExternalOutput").ap()

    x_own = din("x_own", [2048, D])
    x_halo = din("x_halo", [512, D])
    ctx2 = din("ctx2", [512, D])
    x_slots = din("x_slots", [6144, D])
    cvecT = din("cvecT", [128, 16])
    w_ada = din("w_ada", [D, 6 * D])
    b_row = din("b_row", [1, 6 * D])
    b_col = din("b_col", [128, 48])
    w_in = din("w_in", [D, 10 * D])
    wf_slots = din("wf_slots", [3, D, D])
    lbT = din("lbT", [128, 80])
    flags = din("flags", [128, 6])
    hgT = din("hgT", [128, 8])
    consts_bf = din("consts_bf", [128, 768], BF16)
    ident_f_d = din("ident_f", [128, 128])
    mod_d = dscr("mod_d", [2, 6 * D])
    w_a = din("w_a", [D, D])
    w_b = din("w_b", [D, D])
    w_o = din("w_o", [D, D])
    lnp = din("lnp", [4, D])
    w_r = din("w_r", [D, 64])
    rbias = din("rbias", [1, 64])
    w_eg = din("w_eg", [65, D, 256])
    w_eu = din("w_eu", [65, D, 256])
    w_ed = din("w_ed", [65, 256, D])
    x1_d = dscr("x1_d", [NOWN, D])
    g_d = dscr("g_d", [65, NOWN])
    out_d = dout("out", [NOWN, D])
    cosT_d = din("cosT", [128, NKB])
    sinT_d = din("sinT", [128, NKB])
    Bint_d = din("Bint", [8, 128, 512])
    Bspec_d = din("Bspec", [8, 128, 7 * 768])

    S = Sched(nc)
    PS = PsumPool(S)
    dbg = {}

    cb = S.sb("cb", [128, 768], BF16)
    S.dma("sp", cb.ap[:], consts_bf, writes=[cb])
    ident_bf = Buf.__new__(Buf)
    Tr.__init__(ident_bf, "ident_bf")
    ident_bf.ap = cb.ap[:, 0:128]
    ident_bf.w = cb.w
    ident_f = S.sb("ident_f_sb", [128, 128], F32)
    S.dma("sp", ident_f.ap[:], ident_f_d, writes=[ident_f])
    cols = S.sb("cols", [128, 6, 8], F32)
    lbv = S.sb("lbv", [128, 5, 8], F32)
    oml = S.sb("oml", [128, 5, 8], F32)
    flg = S.sb("flg", [128, 6], F32)
    S.dma("sp", flg.ap[:], flags, writes=[flg])
    hgc = S.sb("hgc", [128, 8], F32)
    S.dma("sp", hgc.ap[:], hgT, writes=[hgc])
    hT = S.sb("hT", [128, 8, NKB], BF16)
    hT_t = [S.tr("hT%d" % i) for i in range(20)]
    xts = [S.sb("gxt%d" % i, [128, 1024], F32) for i in range(2)]
    work = [(S.sb("gst%d" % i, [128, 2, 6], F32), S.sb("gmv%d" % i, [128, 2], F32), S.sb("grstd%d" % i, [128, 1], F32),
             S.sb("gnmr%d" % i, [128, 1], F32), S.sb("gxb%d" % i, [128, 1024], BF16)) for i in range(2)]
    hgs = S.scope()
    hgs.__enter__()
    ones_bf = S.sb("ones_bf", [128, 2048], BF16)
    S.op("pool", lambda e: e.memset(ones_bf.ap[:], 1.0), writes=[ones_bf])
    rmask = S.sb("rmask", [128, 2048], BF16)
    S.op("pool", lambda e: e.memset(rmask.ap[:], 1.0), writes=[rmask])
    S.op("pool", lambda e: e.memset(rmask.ap[:].rearrange("p (c t) -> p c t", t=64)[:, :, 0:1], 0.0), writes=[rmask])
    Sf0 = S.sb("Sf0", [128, 8, 128], F32)
    Sb0 = S.sb("Sb0", [128, 8, 128], F32)
    S.op("pool", lambda e: e.memset(Sf0.ap[:], 0.0), writes=[Sf0])
    S.op("pool", lambda e: e.memset(Sb0.ap[:], 0.0), writes=[Sb0])

    with S.scope():
        STG = Stage(S, 4, 2048, "p0")
        condT = S.sb("condT", [128, 16], F32)
        S.dma("sp", condT.ap[:], cvecT, writes=[condT])
        S.op("act", lambda e: e.activation(condT.ap[:], condT.ap[:], AF.Silu), reads=[condT], writes=[condT])
        bcol = S.sb("bcol", [128, 48], F32)
        S.dma("sp", bcol.ap[:], b_col, writes=[bcol])
        brow = S.sb("brow", [2, 6 * D], F32)
        brow_t = [S.tr("brow0"), S.tr("brow1")]
        for r in range(2):
            S.dma("sp", brow.ap[r:r + 1, :], b_row, writes=[brow_t[r]])
        modrow = S.sb("modrow", [2, 6 * D], F32)
        modcol = S.sb("modcol", [128, 48, 2], F32)
        lbraw = S.sb("lbraw", [128, 80], F32)
        S.dma("sp", lbraw.ap[:], lbT, writes=[lbraw])
        lbd = S.sb("lbd", [128, 5, 8], F32)
        lb4 = lbraw.ap[:].rearrange("p (a two h) -> p a two h", two=2, h=8)
        S.op("dve", lambda e: e.tensor_tensor(lbd.ap[:], lb4[:, :, 0, :], lb4[:, :, 1, :], ALU.subtract),
             reads=[lbraw], writes=[lbd])
        S.op("act", lambda e: e.activation(lbv.ap[:], lbd.ap[:], AF.Sigmoid), reads=[lbd], writes=[lbv])
        S.op("dve", lambda e: e.tensor_scalar(oml.ap[:], lbv.ap[:], -1.0, 1.0, ALU.mult, ALU.add),
             reads=[lbv], writes=[oml])
        psC = PS.reserve()
        w_ada_v = w_ada.rearrange("(k p) n -> p k n", p=128)
        for blk in range(24):
            stg, view = STG.load(w_ada_v[:, :, blk * 256:(blk + 1) * 256], 2048)

            def mmc(e, view=view, blk=blk):
                for cc in range(2):
                    c0 = (2 * blk + cc) * 2
                    for k in range(8):
                        ins = e.matmul(psC.ap[:, c0:c0 + 2], view[:, k, cc * 128:(cc + 1) * 128],
                                       condT.ap[:, 2 * k:2 * k + 2], start=(k == 0), stop=(k == 7))
                return ins
            S.op("pe", mmc, reads=[stg, condT], writes=[psC])
            psR = PS.get()

            def mmr(e, view=view, psR=psR):
                for k in range(8):
                    ins = e.matmul(psR.ap[0:2, 0:256], condT.ap[:, 2 * k:2 * k + 2], view[:, k, :],
                                   start=(k == 0), stop=(k == 7))
                return ins
            S.op("pe", mmr, reads=[stg, condT], writes=[psR])
            S.op("dve", lambda e, psR=psR, blk=blk: e.tensor_tensor(
                modrow.ap[0:2, blk * 256:(blk + 1) * 256], psR.ap[0:2, 0:256],
                brow.ap[0:2, blk * 256:(blk + 1) * 256], ALU.add),
                reads=[psR] + brow_t, writes=[modrow])
        S.dma("sp", mod_d, modrow.ap[0:2, :], reads=[modrow], writes=[S.tr("mod_d")])
        pc3 = psC.ap[:, 0:96].rearrange("p (c r) -> p c r", r=2)
        for r in range(2):
            S.op("dve", lambda e, r=r: e.tensor_tensor(modcol.ap[:, :, r], pc3[:, :, r], bcol.ap[:], ALU.add),
                 reads=[psC, bcol], writes=[modcol])
        for ci, (c0, r, addone) in enumerate([(8, 0, 1.0), (0, 0, 0.0), (8, 1, 1.0), (0, 1, 0.0), (32, 0, 1.0), (24, 0, 0.0)]):
            S.op("dve", lambda e, ci=ci, c0=c0, r=r, addone=addone: e.tensor_scalar(
                cols.ap[:, ci, :], modcol.ap[:, c0:c0 + 8, r], addone, None, ALU.add),
                reads=[modcol], writes=[cols])
        PS.release(psC)
    if stage == 0:
        o_cols = dout("o_cols", [128, 48])
        o_lb = dout("o_lb", [128, 40])
        S.dma("sp", o_cols, cols.ap[:].rearrange("p a b -> p (a b)"), reads=[cols], writes=[S.tr("o1")])
        S.dma("sp", o_lb, lbv.ap[:].rearrange("p a b -> p (a b)"), reads=[lbv], writes=[S.tr("o2")])
        o_mod = dout("o_mod", [2, 6 * D])
        S.dma("sp", o_mod, mod_d, reads=[], writes=[S.tr("o3")])
        S.finish()
        return nc

    with S.scope():
        STG = Stage(S, 4, 1024, "p2")
        hTs = hT
        hTs_t = hT_t[0:16]
        WfB = S.sb("WfB", [128, 8, 1024], BF16)
        WiB = S.sb("WiB", [128, 8, 1024], BF16)
        T1 = S.sb("T1", [128, 2048], F32)
        T2 = S.sb("T2", [128, 2048], F32)
        T3 = S.sb("T3", [128, 2048], F32)
        T4 = S.sb("T4", [128, 2048], F32)
        kd = S.sb("kd", [128, 2048], BF16)
        kd_tm = S.sb("kd_tm", [128, 16, 128], BF16)
        v_tm = S.sb("v_tm", [128, 16, 128], BF16)
        tmpS = S.sb("tmpS", [128, 128], F32)
        tmpD = S.sb("tmpD", [128, 128], F32)
        Dcol = S.sb("Dcol", [128, 1], F32)
        w_in_v = w_in.rearrange("(k p) n -> p k n", p=128)
        for q8 in range(8):
            STG.load_cast(w_in_v[:, :, 3072 + q8 * 128:3072 + (q8 + 1) * 128], 1024,
                          WiB.ap[:, :, q8 * 128:(q8 + 1) * 128], WiB)
        slots = [
            (ctx2[0:256, :], 256, 2, 3, w_in_v[:, :, 1024:2048], 0, "f"),
            (ctx2[256:512, :], 256, 2, 3, w_in_v[:, :, 2048:3072], 1, "b"),
        ]
        for s in range(3):
            slots.append((x_slots[s * 2048:(s + 1) * 2048, :], 2048, 0, 1,
                          wf_slots[s].rearrange("(k p) n -> p k n", p=128), 2 + s, s))
        def slot_body(src, T, ci_sc, ci_sh, wf_src, lbi, mode):
            nt = T // 128
            TB = min(512, T)
            ln_tokens(S, PS, ident_bf, src, T,
                      lambda k, t: hTs.ap[:, k, t * 128:(t + 1) * 128], hTs_t,
                      lambda k, ci=ci_sc: cols.ap[:, ci, k:k + 1], lambda k, ci=ci_sh: cols.ap[:, ci, k:k + 1],
                      cols, xts, work)
            for q8 in range(8):
                STG.load_cast(wf_src[:, :, q8 * 128:(q8 + 1) * 128], 1024, WfB.ap[:, :, q8 * 128:(q8 + 1) * 128], WfB)
            vctx = {}

            def pA1(h):
                hs = slice(h * 128, (h + 1) * 128)
                for tb in range(T // TB):
                    ps = PS.get()
                    ts_ = slice(tb * TB, (tb + 1) * TB)
                    def mmf(e, ps=ps, ts_=ts_, hs=hs):
                        for k in range(8):
                            ins = e.matmul(ps.ap[:, 0:TB], WfB.ap[:, k, hs], hTs.ap[:, k, ts_], start=(k == 0), stop=(k == 7))
                        return ins
                    S.op("pe", mmf, reads=[WfB] + hTs_t[tb * TB // 128:(tb + 1) * TB // 128], writes=[ps])
                    S.op("act", lambda e, ps=ps, ts_=ts_: e.activation(T1.ap[:, ts_], ps.ap[:, 0:TB], AF.Sigmoid),
                         reads=[ps], writes=[T1])

            def pA2(h):
                hs = slice(h * 128, (h + 1) * 128)
                vps = []
                vctx[h] = vps
                for g in range((nt + 3) // 4):
                    n = min(4, nt - g * 4)
                    ps = PS.reserve()
                    vps.append((ps, g, n))
                    def mmv(e, ps=ps, g=g, n=n, hs=hs):
                        for j in range(n):
                            t = g * 4 + j
                            for k in range(8):
                                ins = e.matmul(ps.ap[:, j * 128:(j + 1) * 128], hTs.ap[:, k, t * 128:(t + 1) * 128],
                                               WiB.ap[:, k, hs], start=(k == 0), stop=(k == 7))
                        return ins
                    S.op("pe", mmv, reads=[WiB] + hTs_t[g * 4:g * 4 + n], writes=[ps])

            def pB(h):
                S.op("dve", lambda e, h=h, lbi=lbi: e.tensor_scalar(
                    T1.ap[:, 0:T], T1.ap[:, 0:T], oml.ap[:, lbi, h:h + 1], lbv.ap[:, lbi, h:h + 1], ALU.mult, ALU.add),
                    reads=[T1, oml, lbv], writes=[T1])
                S.op("act", lambda e: e.activation(T2.ap[:, 0:T], T1.ap[:, 0:T], AF.Ln), reads=[T1], writes=[T2])
                S.op("act", lambda e: e.activation(T1.ap[:, 0:T], T1.ap[:, 0:T], AF.Identity, bias=1.0, scale=-1.0),
                     reads=[T1], writes=[T1])
                S.op("dve", lambda e: e.tensor_tensor_scan(T3.ap[:, 0:T], ones_bf.ap[:, 0:T], T2.ap[:, 0:T], 0.0, ALU.mult, ALU.add),
                     reads=[ones_bf, T2], writes=[T3])
                S.op("act", lambda e: e.activation(T4.ap[:, 0:T], T3.ap[:, 0:T], AF.Exp, bias=T3.ap[:, T - 1:T], scale=-1.0),
                     reads=[T3], writes=[T4])
                S.op("act", lambda e: e.activation(Dcol.ap[:], T3.ap[:, T - 1:T], AF.Exp), reads=[T3], writes=[Dcol])
                S.op("dve", lambda e: e.tensor_tensor(kd.ap[:, 0:T], T1.ap[:, 0:T], T4.ap[:, 0:T], ALU.mult),
                     reads=[T1, T4], writes=[kd])

            def pC1(h):
                vps = vctx.pop(h)
                for (ps, g, n) in vps:
                    S.op("dve", lambda e, ps=ps, g=g, n=n: e.tensor_copy(
                        v_tm.ap[:, g * 4:g * 4 + n, :], ps.ap[:, 0:n * 128].rearrange("p (a b) -> p a b", b=128)),
                        reads=[ps], writes=[v_tm])
                    PS.release(ps)

            def pC2(h):
                for g in range((nt + 7) // 8):
                    n = min(8, nt - g * 8)
                    ps = PS.get()
                    def trk(e, ps=ps, g=g, n=n):
                        for j in range(n):
                            t = g * 8 + j
                            ins = e.transpose(ps.bf[:, j * 128:(j + 1) * 128], kd.ap[:, t * 128:(t + 1) * 128], ident_bf.ap[:])
                        return ins
                    S.op("pe", trk, reads=[kd, ident_bf], writes=[ps])
                    S.op("act", lambda e, ps=ps, g=g, n=n: e.activation(
                        kd_tm.ap[:, g * 8:g * 8 + n, :], ps.bf[:, 0:n * 128].rearrange("p (a b) -> p a b", b=128), AF.Copy),
                        reads=[ps], writes=[kd_tm])
                psU = PS.get()
                def mmu(e, psU=psU, nt=nt):
                    for t in range(nt):
                        ins = e.matmul(psU.ap[:, 0:128], kd_tm.ap[:, t, :], v_tm.ap[:, t, :], start=(t == 0), stop=(t == nt - 1))
                    return ins
                S.op("pe", mmu, reads=[kd_tm, v_tm], writes=[psU])
                if mode == "f":
                    S.op("dve", lambda e, psU=psU, h=h: e.tensor_copy(Sf0.ap[:, h, :], psU.ap[:, 0:128]), reads=[psU], writes=[Sf0])
                elif mode == "b":
                    S.op("dve", lambda e, psU=psU, h=h: e.tensor_copy(Sb0.ap[:, h, :], psU.ap[:, 0:128]), reads=[psU], writes=[Sb0])
                else:
                    for (SX, fc) in ((Sf0, mode), (Sb0, 3 + mode)):
                        S.op("dve", lambda e, SX=SX, psU=psU, h=h: e.scalar_tensor_tensor(
                            tmpS.ap[:], SX.ap[:, h, :], Dcol.ap[:, 0:1], psU.ap[:, 0:128], ALU.mult, ALU.add),
                            reads=[SX, Dcol, psU], writes=[tmpS])
                        S.op("dve", lambda e, SX=SX, h=h: e.tensor_tensor(tmpD.ap[:], tmpS.ap[:], SX.ap[:, h, :], ALU.subtract),
                             reads=[tmpS, SX], writes=[tmpD])
                        S.op("dve", lambda e, SX=SX, h=h, fc=fc: e.scalar_tensor_tensor(
                            SX.ap[:, h, :], tmpD.ap[:], flg.ap[:, fc:fc + 1], SX.ap[:, h, :], ALU.mult, ALU.add),
                            reads=[tmpD, flg, SX], writes=[SX])
            pA1(0)
            pA2(0)
            pB(0)
            for h in range(1, 8):
                pA1(h)
                pC1(h - 1)
                pA2(h)
                pC2(h - 1)
                pB(h)
            pC1(7)
            pC2(7)
        for sl in slots:
            slot_body(*sl)

    if stage == 2:
        o_sf = dout("o_sf", [128, 1024])
        o_sb = dout("o_sb", [128, 1024])
        S.dma("sp", o_sf, Sf0.ap[:].rearrange("p a b -> p (a b)"), reads=[Sf0], writes=[S.tr("o1")])
        S.dma("sp", o_sb, Sb0.ap[:].rearrange("p a b -> p (a b)"), reads=[Sb0], writes=[S.tr("o2")])
        S.finish()
        return nc

    y_hg_d = dscr("y_hg_d", [8, 128, NOWN], BF16)
    y_hg_t = [S.tr("yhg%d" % i) for i in range(8)]
    w_in_v = w_in.rearrange("(k p) n -> p k n", p=128)

    def ln_main(src, T, tile0):
        ln_tokens(S, PS, ident_bf, src, T,
                  lambda k, t: hT.ap[:, k, (tile0 + t) * 128:(tile0 + t + 1) * 128], hT_t[tile0:tile0 + T // 128],
                  lambda k: cols.ap[:, 0, k:k + 1], lambda k: cols.ap[:, 1, k:k + 1], cols, xts, work)
    ln_main(x_own, 2048, 2)
    ln_main(x_halo[0:256, :], 256, 0)
    ln_main(x_halo[256:512, :], 256, 18)

    with S.scope():
        STG = Stage(S, 4, 1024, "p3")
        W5s = [S.sb("W5_%d" % i, [128, 8, 5, 128], BF16) for i in range(2)]
        sq = S.sb("sq", [128, NOWN], BF16)
        sog = S.sb("sog", [128, NOWN], BF16)
        G1 = S.sb("G1", [128, 1024], F32)
        G2 = S.sb("G2", [128, 1024], F32)
        G3 = S.sb("G3", [128, 1024], F32)
        G4 = S.sb("G4", [128, 1024], F32)
        qa = [S.sb("qa%d" % d_, [128, NOWN], BF16) for d_ in range(2)]
        kb = [S.sb("kb%d" % d_, [128, NOWN], BF16) for d_ in range(2)]
        kdT = S.sb("kdT", [128, NOWN], BF16)
        kdm = [S.sb("kdm%d" % d_, [64, 32, 128], BF16) for d_ in range(2)]
        dec = [S.sb("dec%d" % d_, [128, 32], F32) for d_ in range(2)]
        Sbf = [S.sb("Sbf%d" % d_, [128, 32, 128], BF16) for d_ in range(2)]
        vtm = S.sb("vtm", [64, 32, 128], BF16)
        scm = [S.sb("scm%d" % i, [64, 512], BF16) for i in range(2)]
        osq = S.sb("osq", [128, 512], BF16)
        lnv = S.sb("lnv", [128, 512], F32)
        rs = S.sb("rs", [128, 512], F32)
        t1 = S.sb("t1", [128, 512], F32)
        yh = kdT
        mask8 = cb.ap[0:64, 256:768]

        def load_w5(h):
            for g in range(5):
                STG.load_cast(w_in_v[:, :, g * 1024 + h * 128:g * 1024 + (h + 1) * 128], 1024, W5s[h % 2].ap[:, :, g, :], W5s[h % 2])

        def hg_head(h):
            W5 = W5s[h % 2]
            if h == 0:
                load_w5(0)

            def proj_fm(g, tok0, n, ps):
                def f(e):
                    for k in range(8):
                        ins = e.matmul(ps.ap[:, 0:n], W5.ap[:, k, g, :], hT.ap[:, k, 256 + tok0:256 + tok0 + n],
                                       start=(k == 0), stop=(k == 7))
                    return ins
                S.op("pe", f, reads=[W5] + hT_t[2 + tok0 // 128:2 + (tok0 + n) // 128], writes=[ps])
            for (g, dst) in ((0, sq), (4, sog)):
                for tb in range(4):
                    ps = PS.get()
                    proj_fm(g, tb * 512, 512, ps)
                    S.op("act", lambda e, ps=ps, tb=tb, dst=dst: e.activation(dst.ap[:, tb * 512:(tb + 1) * 512], ps.ap[:], AF.Silu),
                         reads=[ps], writes=[dst])
            for g8 in range(8):
                ps = PS.get()

                def mmv(e, ps=ps, g8=g8):
                    for j in range(4):
                        c = g8 * 4 + j
                        for k in range(8):
                            ins = e.matmul(ps.ap[0:64, j * 128:(j + 1) * 128], hT.ap[:, k, 256 + c * 64:256 + (c + 1) * 64],
                                           W5.ap[:, k, 3, :], start=(k == 0), stop=(k == 7))
                    return ins
                S.op("pe", mmv, reads=[W5] + hT_t[2 + g8 * 2:2 + g8 * 2 + 2], writes=[ps])
                S.op("dve", lambda e, ps=ps, g8=g8: e.tensor_copy(
                    vtm.ap[:, g8 * 4:g8 * 4 + 4, :], ps.ap[0:64, :].rearrange("p (a b) -> p a b", b=128)), reads=[ps], writes=[vtm])

            def gate_ops(d_, hf):
                ts_ = slice(hf * 1024, (hf + 1) * 1024)
                ops = []

                def add(*a_, **k_):
                    ops.append(lambda: S.op(*a_, **k_))
                for tb in range(2):
                    def fpro(tb=tb):
                        ps = PS.get()
                        proj_fm(1 + d_, hf * 1024 + tb * 512, 512, ps)
                        S.op("act", lambda e: e.activation(G1.ap[:, tb * 512:(tb + 1) * 512], ps.ap[:], AF.Sigmoid),
                             reads=[ps], writes=[G1])
                    ops.append(fpro)
                add("dve", lambda e: e.tensor_scalar(G1.ap[:], G1.ap[:], oml.ap[:, d_, h:h + 1], lbv.ap[:, d_, h:h + 1], ALU.mult, ALU.add),
                     reads=[G1, oml, lbv], writes=[G1])
                add("act", lambda e: e.activation(G2.ap[:], G1.ap[:], AF.Ln), reads=[G1], writes=[G2])
                add("act", lambda e: e.activation(G1.ap[:], G1.ap[:], AF.Identity, bias=1.0, scale=-1.0), reads=[G1], writes=[G1])
                add("dve", lambda e: e.tensor_tensor_scan(G3.ap[:], rmask.ap[:, 0:1024], G2.ap[:], 0.0, ALU.mult, ALU.add),
                     reads=[rmask, G2], writes=[G3])
                g3v = G3.ap[:].rearrange("p (c t) -> p c t", t=64)
                g4v = G4.ap[:].rearrange("p (c t) -> p c t", t=64)
                if d_ == 1:
                    add("dve", lambda e: e.tensor_tensor(g4v, g3v[:, :, 63:64].broadcast_to([128, 16, 64]), g3v, ALU.subtract),
                         reads=[G3], writes=[G4])
                    add("dve", lambda e: e.tensor_tensor(G3.ap[:], G4.ap[:], G2.ap[:], ALU.add), reads=[G4, G2], writes=[G3])
                add("act", lambda e: e.activation(G4.ap[:], G3.ap[:], AF.Exp), reads=[G3], writes=[G4])
                add("act", lambda e: e.activation(G2.ap[:], G3.ap[:], AF.Exp, scale=-1.0), reads=[G3], writes=[G2])
                add("dve", lambda e: e.tensor_tensor(qa[d_].ap[:, ts_], sq.ap[:, ts_], G4.ap[:], ALU.mult),
                     reads=[sq, G4], writes=[qa[d_]])
                add("dve", lambda e: e.tensor_tensor(kb[d_].ap[:, ts_], G1.ap[:], G2.ap[:], ALU.mult),
                     reads=[G1, G2], writes=[kb[d_]])
                ecol = 63 if d_ == 0 else 0
                add("act", lambda e: e.activation(dec[d_].ap[:, hf * 16:(hf + 1) * 16], g4v[:, :, ecol], AF.Copy),
                     reads=[G4], writes=[dec[d_]])
                add("dve", lambda e: e.tensor_tensor(
                    kdT.ap[:, ts_].rearrange("p (c t) -> p c t", t=64), kb[d_].ap[:, ts_].rearrange("p (c t) -> p c t", t=64),
                    g4v[:, :, ecol:ecol + 1].broadcast_to([128, 16, 64]), ALU.mult),
                    reads=[kb[d_], G4], writes=[kdT])

                return ops

            def rest_ops(d_):
                ops = []
                for g in range(4):
                    def trg(g=g):
                        ps = PS.get()

                        def trk(e):
                            for j in range(8):
                                c = g * 8 + j
                                ins = e.transpose(ps.bf[0:64, j * 128:(j + 1) * 128], kdT.ap[:, c * 64:(c + 1) * 64], ident_bf.ap[:])
                            return ins
                        S.op("pe", trk, reads=[kdT, ident_bf], writes=[ps])
                        S.op("act", lambda e: e.activation(
                            kdm[d_].ap[:, g * 8:(g + 1) * 8, :], ps.bf[0:64, :].rearrange("p (a b) -> p a b", b=128), AF.Copy),
                            reads=[ps], writes=[kdm[d_]])
                    ops.append(trg)
                if d_ == 0:
                    ops.append(lambda: S.op("act", lambda e: e.activation(Sbf[0].ap[:, 0, :], Sf0.ap[:, h, :], AF.Copy), reads=[Sf0], writes=[Sbf[0]]))
                    chunks = list(range(0, 31))
                else:
                    ops.append(lambda: S.op("act", lambda e: e.activation(Sbf[1].ap[:, 31, :], Sb0.ap[:, h, :], AF.Copy), reads=[Sb0], writes=[Sbf[1]]))
                    chunks = list(range(31, 0, -1))
                for g0 in range(0, len(chunks), 4):
                    def ug(grp=chunks[g0:g0 + 4]):
                        ps = PS.get()

                        def mmu(e):
                            for j, c in enumerate(grp):
                                ins = e.matmul(ps.ap[:, j * 128:(j + 1) * 128], kdm[d_].ap[:, c, :], vtm.ap[:, c, :], start=True, stop=True)
                            return ins
                        S.op("pe", mmu, reads=[kdm[d_], vtm], writes=[ps])
                        for j, c in enumerate(grp):
                            cn = c + 1 if d_ == 0 else c - 1
                            S.op("dve", lambda e, j=j, c=c, cn=cn: e.scalar_tensor_tensor(
                                Sbf[d_].ap[:, cn, :], Sbf[d_].ap[:, c, :], dec[d_].ap[:, c:c + 1], ps.ap[:, j * 128:(j + 1) * 128],
                                ALU.mult, ALU.add), reads=[Sbf[d_], dec[d_], ps], writes=[Sbf[d_]])
                    ops.append(ug)
                return ops

            for op_ in gate_ops(0, 0) + gate_ops(0, 1):
                op_()
            interleave([rest_ops(0), gate_ops(1, 0) + gate_ops(1, 1)])
            for op_ in rest_ops(1):
                op_()

            if h + 1 < 8:
                load_w5(h + 1)
            psos = {}

            def stX(tb):
                pso = PS.reserve()
                psos[tb] = pso
                pssl = []
                for g2 in range(2):
                    c0 = tb * 8 + g2 * 4
                    pss = PS.reserve()
                    pssl.append(pss)

                    def mms(e, pss=pss, c0=c0):
                        for j in range(4):
                            csl = slice((c0 + j) * 64, (c0 + j + 1) * 64)
                            for dd in range(2):
                                o_ = (j * 2 + dd) * 64
                                ins = e.matmul(pss.ap[0:64, o_:o_ + 64], kb[dd].ap[:, csl], qa[dd].ap[:, csl], start=True, stop=True)
                        return ins
                    S.op("pe", mms, reads=[kb[0], kb[1], qa[0], qa[1]], writes=[pss])
                for g2 in range(2):
                    pss = pssl[g2]
                    sc_ = scm[g2]
                    S.op("dve", lambda e, pss=pss, sc_=sc_: e.tensor_tensor(sc_.ap[:], pss.ap[0:64, :], mask8, ALU.mult),
                         reads=[pss, cb], writes=[sc_])
                    PS.release(pss)
                for g2 in range(2):
                    c0 = tb * 8 + g2 * 4
                    sc_ = scm[g2]

                    def mmo(e, c0=c0, g2=g2, sc_=sc_):
                        for j in range(4):
                            c = c0 + j
                            o64 = slice((g2 * 4 + j) * 64, (g2 * 4 + j + 1) * 64)
                            csl = slice(c * 64, (c + 1) * 64)
                            e.matmul(pso.ap[:, o64], vtm.ap[:, c, :], sc_.ap[:, (j * 2) * 64:(j * 2 + 1) * 64], start=True, stop=False)
                            e.matmul(pso.ap[:, o64], vtm.ap[:, c, :], sc_.ap[:, (j * 2 + 1) * 64:(j * 2 + 2) * 64], start=False, stop=False)
                            e.matmul(pso.ap[:, o64], Sbf[0].ap[:, c, :], qa[0].ap[:, csl], start=False, stop=False)
                            ins = e.matmul(pso.ap[:, o64], Sbf[1].ap[:, c, :], qa[1].ap[:, csl], start=False, stop=True)
                        return ins
                    S.op("pe", mmo, reads=[vtm, sc_, Sbf[0], Sbf[1], qa[0], qa[1]], writes=[pso])

            def stY(tb):
                pso = psos.pop(tb)
                S.op("act", lambda e: e.activation(osq.ap[:], pso.ap[:], AF.Square), reads=[pso], writes=[osq])
                pq = PS.get()
                S.op("pe", lambda e: e.matmul(pq.ap[:], ones_bf.ap[:, 0:128], osq.ap[:], start=True, stop=True),
                     reads=[ones_bf, osq], writes=[pq])
                S.op("act", lambda e: e.activation(lnv.ap[:], pq.ap[:], AF.Ln, bias=EPS, scale=1.0 / 128.0), reads=[pq], writes=[lnv])
                S.op("act", lambda e: e.activation(rs.ap[:], lnv.ap[:], AF.Exp, scale=-0.5), reads=[lnv], writes=[rs])
                S.op("dve", lambda e: e.tensor_tensor(t1.ap[:], pso.ap[:], rs.ap[:], ALU.mult), reads=[pso, rs], writes=[t1])
                S.op("dve", lambda e: e.scalar_tensor_tensor(
                    yh.ap[:, tb * 512:(tb + 1) * 512], t1.ap[:], hgc.ap[:, h:h + 1], sog.ap[:, tb * 512:(tb + 1) * 512],
                    ALU.mult, ALU.mult), reads=[t1, hgc, sog], writes=[yh])
                PS.release(pso)
            for step in range(5):
                if step < 4:
                    stX(step)
                if step >= 1:
                    stY(step - 1)
            S.dma("sp", y_hg_d[h], yh.ap[:], reads=[yh], writes=[y_hg_t[h]])

        for h in range(8):
            hg_head(h)
    hgs.__exit__(None, None, None)
    if stage == 3:
        o_yhg = dout("o_yhg", [8, 128, NOWN], BF16)
        S.dma("sp", o_yhg, y_hg_d, reads=y_hg_t, writes=[S.tr("o1")])
        S.finish()
        return nc

    y_na_d = dscr("y_na_d", [8, 128, NOWN], BF16)
    y_na_t = [S.tr("yna%d" % i) for i in range(8)]
    with S.scope():
        STG = Stage(S, 4, 1024, "p4")
        hcT = S.sb("hcT", [128, 8, 256], BF16)
        hcT_t = [S.tr("hcT0"), S.tr("hcT1")]
        ln_tokens(S, PS, ident_bf, ctx2[0:256, :], 256, lambda k, t: hcT.ap[:, k, t * 128:(t + 1) * 128], hcT_t,
                  lambda k: cols.ap[:, 2, k:k + 1], lambda k: cols.ap[:, 3, k:k + 1], cols, xts, work)
        cosT = S.sb("cosT_sb", [128, NKB], F32)
        sinT = S.sb("sinT_sb", [128, NKB], F32)
        S.dma("sp", cosT.ap[:], cosT_d, writes=[cosT])
        S.dma("sp", sinT.ap[:], sinT_d, writes=[sinT])
        W3 = S.sb("W3", [128, 8, 3, 128], BF16)
        qblk = S.sb("qblk", [128, 32, 128], BF16)
        qpblk = S.sb("qpblk", [128, 32, 128], BF16)
        S.op("pool", lambda e: e.memset(qblk.ap[:], 0.0), writes=[qblk])
        S.op("pool", lambda e: e.memset(qpblk.ap[:], 0.0), writes=[qpblk])
        krT = S.sb("krT", [128, NKB], BF16)
        kcT = S.sb("kcT", [128, 256], BF16)
        pl = S.sb("pl", [128, 512], BF16)
        tqa = S.sb("tqa", [128, 512], F32)
        tqb = S.sb("tqb", [128, 512], F32)
        v_ev = S.sb("v_ev", [128, 20, 2, 65], BF16)
        v_od = S.sb("v_od", [128, 19, 2, 65], BF16)
        v_cx = S.sb("v_cx", [128, 2, 2, 65], BF16)
        for vb in (v_ev, v_od, v_cx):
            S.op("pool", lambda e, vb=vb: e.memset(vb.ap[:], 1.0), writes=[vb])
        Bi = S.sb("Bi", [128, 512], BF16)
        Bs = S.sb("Bs", [128, 7, 768], BF16)
        Pm = [S.sb("Pm%d" % i, [128, 1024], BF16) for i in range(4)]
        PT = [S.sb("PT%d" % i, [128, 8, 128], BF16) for i in range(3)]
        mxs = [S.sb("mx%d" % i, [128, 2], F32) for i in range(3)]
        nmxs = [S.sb("nmx%d" % i, [128, 1], F32) for i in range(3)]
        rcs = [S.sb("rc%d" % i, [64, 2], F32) for i in range(3)]
        ytms = [S.sb("ytm%d" % i, [64, 8, 128], BF16) for i in range(2)]
        ynaT = S.sb("ynaT", [128, NOWN], BF16)

        def na_pair(pr):
            for g in range(3):
                c0 = 5120 + g * 1024 + pr * 128
                STG.load_cast(w_in_v[:, :, c0:c0 + 128], 1024, W3.ap[:, :, g, :], W3)
            STG.load_cast(Bint_d[pr], 512, Bi.ap[:], Bi)
            for sr in range(7):
                STG.load_cast(Bspec_d[pr][:, sr * 768:(sr + 1) * 768], 768, Bs.ap[:, sr, :], Bs)

            def proj(g, src_t, src_trs, tok0, n, ps):
                def f(e):
                    for k in range(8):
                        ins = e.matmul(ps.ap[:, 0:n], W3.ap[:, k, g, :], src_t.ap[:, k, tok0:tok0 + n], start=(k == 0), stop=(k == 7))
                    return ins
                S.op("pe", f, reads=[W3] + src_trs, writes=[ps])

            def rope(ps, tok0, n, scale, out_fn, out_tr, plain_fn=None, plain_tr=None):
                S.op("act", lambda e: e.activation(pl.ap[:, 0:n], ps.ap[:, 0:n], AF.Identity, scale=scale), reads=[ps], writes=[pl])
                ps2 = PS.get()
                S.op("pe", lambda e: e.matmul(ps2.ap[:, 0:n], cb.ap[:, 128:256], pl.ap[:, 0:n], start=True, stop=True),
                     reads=[cb, pl], writes=[ps2])
                S.op("dve", lambda e: e.tensor_tensor(tqa.ap[:, 0:n], pl.ap[:, 0:n], cosT.ap[:, tok0:tok0 + n], ALU.mult),
                     reads=[pl, cosT], writes=[tqa])
                S.op("dve", lambda e: e.tensor_tensor(tqb.ap[:, 0:n], ps2.ap[:, 0:n], sinT.ap[:, tok0:tok0 + n], ALU.mult),
                     reads=[ps2, sinT], writes=[tqb])
                out_fn(tqa, tqb)
                if plain_fn is not None:
                    plain_fn(pl)

            for tb in range(4):
                ps = PS.get()
                proj(0, hT, hT_t[2 + tb * 4:2 + tb * 4 + 4], 256 + tb * 512, 512, ps)

                def qout(a_, b_, tb=tb):
                    for hh in range(2):
                        psl = slice(hh * 64, (hh + 1) * 64)
                        S.op("dve", lambda e, psl=psl: e.tensor_tensor(
                            qblk.ap[psl, tb * 8:(tb + 1) * 8, psl], a_.ap[psl, 0:512].rearrange("p (r q) -> p r q", q=64),
                            b_.ap[psl, 0:512].rearrange("p (r q) -> p r q", q=64), ALU.add), reads=[a_, b_], writes=[qblk])

                def qplain(pl_, tb=tb):
                    for hh in range(2):
                        psl = slice(hh * 64, (hh + 1) * 64)
                        S.op("act", lambda e, psl=psl: e.activation(
                            qpblk.ap[psl, tb * 8:(tb + 1) * 8, psl], pl_.ap[psl, 0:512].rearrange("p (r q) -> p r q", q=64), AF.Copy),
                            reads=[pl_], writes=[qpblk])
                rope(ps, 256 + tb * 512, 512, 0.125, qout, qblk, qplain, qpblk)
            for tb in range(5):
                ps = PS.get()
                proj(1, hT, hT_t[tb * 4:tb * 4 + 4], tb * 512, 512, ps)

                def kout(a_, b_, tb=tb):
                    S.op("dve", lambda e: e.tensor_tensor(krT.ap[:, tb * 512:(tb + 1) * 512], a_.ap[:, 0:512], b_.ap[:, 0:512], ALU.add),
                         reads=[a_, b_], writes=[krT])
                rope(ps, tb * 512, 512, 1.0, kout, krT)
            ps = PS.get()
            proj(1, hcT, hcT_t, 0, 256, ps)
            S.op("act", lambda e, ps=ps: e.activation(kcT.ap[:], ps.ap[:, 0:256], AF.Copy), reads=[ps], writes=[kcT])

            def vproj(src_t, src_trs_fn, tiles, dst):
                for g0 in range(0, len(tiles), 4):
                    grp = tiles[g0:g0 + 4]
                    ps = PS.get()
                    trs = []
                    for (ti, tok0) in grp:
                        trs += src_trs_fn(tok0)

                    def f(e, ps=ps, grp=grp):
                        for j, (ti, tok0) in enumerate(grp):
                            for k in range(8):
                                ins = e.matmul(ps.ap[:, j * 128:(j + 1) * 128], src_t.ap[:, k, tok0:tok0 + 128], W3.ap[:, k, 2, :],
                                               start=(k == 0), stop=(k == 7))
                        return ins
                    S.op("pe", f, reads=[W3] + trs, writes=[ps])
                    n = len(grp)
                    t0 = grp[0][0]
                    S.op("dve", lambda e, ps=ps, n=n, t0=t0: e.tensor_copy(
                        dst.ap[:, t0:t0 + n, :, 0:64], ps.ap[:, 0:n * 128].rearrange("p (a h d) -> p a h d", h=2, d=64)),
                        reads=[ps], writes=[dst])
            vproj(hT, lambda tok0: [hT_t[tok0 // 128]], [(i, i * 128) for i in range(20)], v_ev)
            vproj(hT, lambda tok0: [hT_t[tok0 // 128], hT_t[tok0 // 128 + 1]], [(i, 64 + i * 128) for i in range(19)], v_od)
            vproj(hcT, lambda tok0: [hcT_t[tok0 // 128]], [(i, i * 128) for i in range(2)], v_cx)

            ctx_ = {}

            def rowcfg(lr):
                if lr < 4:
                    return 0, 12, Bs.ap[:, lr, :], Bs
                if lr >= 29:
                    return 27, 12, Bs.ap[:, 4 + lr - 29, :], Bs
                return lr, 8, Bi.ap[:], Bi

            def st0(lr):
                rel0, nkr, Bap, Btr = rowcfg(lr)
                nk = nkr * 64
                psA = PS.get()
                psB = PS.get()
                k0 = rel0 * 64
                ctx_[lr] = dict(psA=psA, psB=psB, nk=nk, nB=nk - 512 + 256, rel0=rel0)

                def qk(e):
                    e.matmul(psA.ap[:, 0:512], qblk.ap[:, lr, :], krT.ap[:, k0:k0 + 512], start=True, stop=False)
                    e.matmul(psA.ap[:, 0:512], ident_bf.ap[:], Bap[:, 0:512], start=False, stop=True)
                    o = 0
                    if nk > 512:
                        e.matmul(psB.ap[:, 0:256], qblk.ap[:, lr, :], krT.ap[:, k0 + 512:k0 + 768], start=True, stop=False)
                        e.matmul(psB.ap[:, 0:256], ident_bf.ap[:], Bap[:, 512:768], start=False, stop=True)
                        o = 256
                    return e.matmul(psB.ap[:, o:o + 256], qpblk.ap[:, lr, :], kcT.ap[:], start=True, stop=True)
                S.op("pe", qk, reads=[qblk, qpblk, krT, kcT, Btr, ident_bf], writes=[psA, psB])

            def st1(lr):
                c = ctx_[lr]
                psA, psB, nB = c["psA"], c["psB"], c["nB"]
                mx_ = mxs[lr % 3]
                nm_ = nmxs[lr % 3]
                P_ = Pm[lr % 4]
                S.op("dve", lambda e: e.tensor_reduce(mx_.ap[:, 0:1], psA.ap[:, 0:512], AX.X, ALU.max), reads=[psA], writes=[mx_])
                S.op("dve", lambda e: e.tensor_reduce(mx_.ap[:, 1:2], psB.ap[:, 0:nB], AX.X, ALU.max), reads=[psB], writes=[mx_])
                S.op("dve", lambda e: e.tensor_scalar(nm_.ap[:], mx_.ap[:, 0:1], mx_.ap[:, 1:2], -1.0, ALU.max, ALU.mult),
                     reads=[mx_], writes=[nm_])
                S.op("act", lambda e: e.activation(P_.ap[:, 0:512], psA.ap[:, 0:512], AF.Exp, bias=nm_.ap[:], scale=1.0),
                     reads=[psA, nm_], writes=[P_])
                S.op("act", lambda e: e.activation(P_.ap[:, 512:512 + nB], psB.ap[:, 0:nB], AF.Exp, bias=nm_.ap[:], scale=1.0),
                     reads=[psB, nm_], writes=[P_])

            def st2(lr):
                c = ctx_[lr]
                nkt = (512 + c["nB"]) // 128
                P_ = Pm[lr % 4]
                PT_ = PT[lr % 3]
                pst = PS.get()

                def trp(e):
                    for jt in range(nkt):
                        ins = e.transpose(pst.bf[:, jt * 128:(jt + 1) * 128], P_.ap[:, jt * 128:(jt + 1) * 128], ident_bf.ap[:])
                    return ins
                S.op("pe", trp, reads=[P_, ident_bf], writes=[pst])
                S.op("dve", lambda e: e.tensor_copy(
                    PT_.ap[:, 0:nkt, :], pst.bf[:, 0:nkt * 128].rearrange("p (a b) -> p a b", b=128)), reads=[pst], writes=[PT_])

            def st3(lr):
                c = ctx_.pop(lr)
                rel0 = c["rel0"]
                nwt = c["nk"] // 128
                PT_ = PT[lr % 3]
                pso = PS.get()
                if rel0 % 2 == 0:
                    vt, vi0 = v_ev, rel0 // 2
                else:
                    vt, vi0 = v_od, (rel0 - 1) // 2

                def pv(e):
                    for hh in range(2):
                        for jt in range(nwt + 2):
                            if jt < nwt:
                                rhs = vt.ap[:, vi0 + jt, hh, :]
                            else:
                                rhs = v_cx.ap[:, jt - nwt, hh, :]
                            ins = e.matmul(pso.ap[0:64, hh * 65:(hh + 1) * 65], PT_.ap[:, jt, hh * 64:(hh + 1) * 64], rhs,
                                           start=(jt == 0), stop=(jt == nwt + 1))
                    return ins
                S.op("pe", pv, reads=[PT_, v_ev, v_od, v_cx], writes=[pso])
                pv3 = pso.ap[0:64, 0:130].rearrange("p (h d) -> p h d", d=65)
                rc_ = rcs[lr % 3]
                yt_ = ytms[(lr // 8) % 2]
                rr = lr % 8
                S.op("dve", lambda e: e.reciprocal(rc_.ap[:], pv3[:, :, 64]), reads=[pso], writes=[rc_])
                S.op("dve", lambda e: e.tensor_tensor(
                    yt_.ap[:, rr, :].rearrange("p (h d) -> p h d", d=64), pv3[:, :, 0:64],
                    rc_.ap[:].rearrange("p (h o) -> p h o", o=1).broadcast_to([64, 2, 64]), ALU.mult),
                    reads=[pso, rc_], writes=[yt_])
                if rr == 7:
                    r8 = lr // 8
                    psy = PS.get()

                    def try_(e):
                        for q_ in range(8):
                            ins = e.transpose(psy.bf[:, q_ * 64:(q_ + 1) * 64], yt_.ap[:, q_, :], ident_bf.ap[0:64, 0:64])
                        return ins
                    S.op("pe", try_, reads=[yt_, ident_bf], writes=[psy])
                    S.op("act", lambda e: e.activation(ynaT.ap[:, r8 * 512:(r8 + 1) * 512], psy.bf[:, 0:512], AF.Copy),
                         reads=[psy], writes=[ynaT])
            for step in range(32 + 4):
                if 0 <= step - 1 < 32:
                    st1(step - 1)
                if 0 <= step - 4 < 32:
                    st3(step - 4)
                if 0 <= step - 3 < 32:
                    st2(step - 3)
                if step < 32:
                    st0(step)
            S.dma("sp", y_na_d[pr], ynaT.ap[:], reads=[ynaT], writes=[y_na_t[pr]])

        for pr in range(8):
            na_pair(pr)
    if stage == 4:
        o_yna = dout("o_yna", [8, 128, NOWN], BF16)
        S.dma("sp", o_yna, y_na_d, reads=y_na_t, writes=[S.tr("o1")])
        S.finish()
        return nc

    x1_t = [S.tr("x1_%d" % i) for i in range(16)]

    def bcast_load(dst_ap, src_row_ap, tr):
        S.dma("sp", dst_ap, src_row_ap.partition_broadcast(128), writes=[tr])

    def ln_stats(src_buf, st, mv, rstd, nmr):
        for i in range(2):
            S.op("dve", lambda e, i=i: e.bn_stats(st.ap[:, i, :], src_buf.ap[:, i * 512:(i + 1) * 512]), reads=[src_buf], writes=[st])
        S.op("dve", lambda e: e.bn_aggr(mv.ap[:], st.ap[:].rearrange("p a b -> p (a b)")), reads=[st], writes=[mv])
        S.op("act", lambda e: e.activation(rstd.ap[:], mv.ap[:, 1:2], AF.Sqrt, bias=EPS, scale=1.0), reads=[mv], writes=[rstd])
        S.op("dve", lambda e: e.reciprocal(rstd.ap[:], rstd.ap[:]), reads=[rstd], writes=[rstd])
        S.op("dve", lambda e: e.scalar_tensor_tensor(nmr.ap[:], mv.ap[:, 0:1], -1.0, rstd.ap[:], ALU.mult, ALU.mult),
             reads=[mv, rstd], writes=[nmr])
    st_, mv_, rstd_, nmr_, _xb = work[0]
    gtm = S.sb("gtm", [128, 16, 65], F32)
    gtm_t = [S.tr("gtm%d" % i) for i in range(16)]
    S.op("pool", lambda e: e.memset(gtm.ap[:], 1.0), writes=gtm_t)
    with S.scope():
        STG = Stage(S, 4, 1024, "p5")
        bc = S.sb("bc5", [128, 3, D], F32)
        bc_t = S.tr("bc5t")
        S.dma("sp", bc.ap[:, 0, :], mod_d[0:1, 2048:3072].partition_broadcast(128), reads=[], writes=[bc_t])
        S.dma("sp", bc.ap[:, 1, :], lnp[0:1, :].partition_broadcast(128), writes=[bc_t])
        S.dma("sp", bc.ap[:, 2, :], lnp[1:2, :].partition_broadcast(128), writes=[bc_t])
        rb = S.sb("rb", [128, 64], F32)
        S.dma("sp", rb.ap[:], rbias.partition_broadcast(128), writes=[rb])
        Wr = S.sb("Wr", [128, 8, 64], F32)
        S.dma("sp", Wr.ap[:], w_r.rearrange("(k p) n -> p k n", p=128), writes=[Wr])
        WoB = S.sb("WoB", [128, 8, D], BF16)
        w_o_v = w_o.rearrange("(k p) n -> p k n", p=128)
        for q8 in range(8):
            STG.load_cast(w_o_v[:, :, q8 * 128:(q8 + 1) * 128], 1024, WoB.ap[:, :, q8 * 128:(q8 + 1) * 128], WoB)
        yhT = S.sb("yhT", [128, 8, 512], BF16)
        ynT = S.sb("ynT", [128, 8, 512], BF16)
        mT = S.sb("mT", [128, 8, 512], BF16)
        Wms = [S.sb("Wm%d" % i, [128, 8, 4, 128], BF16) for i in range(2)]
        sga = S.sb("sga", [128, 512], F32)
        sgb = S.sb("sgb", [128, 512], F32)
        tma = S.sb("tma", [128, 512], F32)
        tmb = S.sb("tmb", [128, 512], F32)
        Ab = [S.sb("Ab%d" % i, [128, D], F32) for i in range(2)]
        Bb = [S.sb("Bb%d" % i, [128, D], F32) for i in range(2)]
        h2fs = [S.sb("h2f%d" % i, [128, 8, 128], F32) for i in range(2)]
        rts = [(S.sb("scr%d" % i, [128, 64], F32), S.sb("sel%d" % i, [128, 64], F32), S.sb("selm%d" % i, [128, 64], F32),
                S.sb("m8_%d" % i, [128, 8, 8], F32), S.sb("gs%d" % i, [128, 8], F32), S.sb("gm8_%d" % i, [128, 8], F32),
                S.sb("pen%d" % i, [128, 8], F32), S.sb("e8_%d" % i, [128, 8], F32), S.sb("wsel%d" % i, [128, 64], F32),
                S.sb("wsum%d" % i, [128, 1], F32), S.sb("gts%d" % i, [128, 64], F32)) for i in range(2)]
        w_a_v = w_a.rearrange("(k p) n -> p k n", p=128)
        w_b_v = w_b.rearrange("(k p) n -> p k n", p=128)
        BIG = 1.0e9

        def quarter(qt):
            tok0 = qt * 512
            S.dma("sp", yhT.ap[:], y_hg_d[:, :, tok0:tok0 + 512].rearrange("h p t -> p h t"), reads=y_hg_t, writes=[yhT])
            S.dma("sp", ynT.ap[:], y_na_d[:, :, tok0:tok0 + 512].rearrange("h p t -> p h t"), reads=y_na_t, writes=[ynT])
            for c in range(8):
                Wm = Wms[c % 2]
                for g, srcv in enumerate((w_in_v[:, :, 8192 + c * 128:8192 + (c + 1) * 128], w_in_v[:, :, 9216 + c * 128:9216 + (c + 1) * 128],
                                          w_a_v[:, :, c * 128:(c + 1) * 128], w_b_v[:, :, c * 128:(c + 1) * 128])):
                    STG.load_cast(srcv, 1024, Wm.ap[:, :, g, :], Wm)
                pss = [PS.get() for _ in range(4)]

                def mm4(e, pss=pss, Wm=Wm):
                    for g in range(4):
                        for k in range(8):
                            if g < 2:
                                rhs = hT.ap[:, k, 256 + tok0:256 + tok0 + 512]
                            else:
                                rhs = (yhT if g == 2 else ynT).ap[:, k, :]
                            ins = e.matmul(pss[g].ap[:], Wm.ap[:, k, g, :], rhs, start=(k == 0), stop=(k == 7))
                    return ins
                S.op("pe", mm4, reads=[Wm, yhT, ynT] + hT_t[2 + qt * 4:2 + qt * 4 + 4], writes=pss)
                S.op("act", lambda e, pss=pss: e.activation(sga.ap[:], pss[0].ap[:], AF.Sigmoid), reads=[pss[0]], writes=[sga])
                S.op("act", lambda e, pss=pss: e.activation(sgb.ap[:], pss[1].ap[:], AF.Sigmoid), reads=[pss[1]], writes=[sgb])
                S.op("dve", lambda e, pss=pss: e.tensor_tensor(tma.ap[:], sga.ap[:], pss[2].ap[:], ALU.mult), reads=[sga, pss[2]], writes=[tma])
                S.op("dve", lambda e, pss=pss: e.tensor_tensor(tmb.ap[:], sgb.ap[:], pss[3].ap[:], ALU.mult), reads=[sgb, pss[3]], writes=[tmb])
                S.op("dve", lambda e, c=c: e.tensor_tensor(mT.ap[:, c, :], tma.ap[:], tmb.ap[:], ALU.add), reads=[tma, tmb], writes=[mT])
            def tile_ops(t4):
                tile_ = qt * 4 + t4
                bs = tile_ % 2
                xt = xts[bs]
                A_ = Ab[bs]
                B_ = Bb[bs]
                h2f = h2fs[bs]
                st_, mv_, rstd_, nmr_, _x = work[bs]
                scr, sel, selm, m8, gs, gm8, pen, e8, wsel, wsum, gts = rts[bs]
                c = {}
                ops = []
                ops.append(lambda: S.dma("sp", xt.ap[:], x_own[tile_ * 128:(tile_ + 1) * 128, :], writes=[xt]))

                def o_mmy():
                    c["psy"] = [PS.reserve(), PS.reserve()]
                    psy = c["psy"]

                    def mmy(e):
                        for hh in range(2):
                            for k in range(8):
                                ins = e.matmul(psy[hh].ap[:], mT.ap[:, k, t4 * 128:(t4 + 1) * 128], WoB.ap[:, k, hh * 512:(hh + 1) * 512],
                                               start=(k == 0), stop=(k == 7))
                        return ins
                    S.op("pe", mmy, reads=[mT, WoB], writes=psy)
                ops.append(o_mmy)
                for hh in range(2):
                    def o_g1(hh=hh):
                        psy = c["psy"]
                        S.op("dve", lambda e: e.tensor_tensor(
                            A_.ap[:, hh * 512:(hh + 1) * 512], psy[hh].ap[:], bc.ap[:, 0, hh * 512:(hh + 1) * 512], ALU.mult),
                            reads=[psy[hh], bc_t], writes=[A_])
                        PS.release(psy[hh])
                    ops.append(o_g1)
                ops.append(lambda: S.op("dve", lambda e: e.scalar_tensor_tensor(A_.ap[:], xt.ap[:], ALPHA, A_.ap[:], ALU.mult, ALU.add),
                                        reads=[xt, A_], writes=[A_]))

                def stats_ops(buf):
                    for i in range(2):
                        ops.append(lambda i=i: S.op("dve", lambda e: e.bn_stats(st_.ap[:, i, :], buf.ap[:, i * 512:(i + 1) * 512]),
                                                    reads=[buf], writes=[st_]))
                    ops.append(lambda: S.op("dve", lambda e: e.bn_aggr(mv_.ap[:], st_.ap[:].rearrange("p a b -> p (a b)")), reads=[st_], writes=[mv_]))
                    ops.append(lambda: S.op("act", lambda e: e.activation(rstd_.ap[:], mv_.ap[:, 1:2], AF.Sqrt, bias=EPS, scale=1.0),
                                            reads=[mv_], writes=[rstd_]))
                    ops.append(lambda: S.op("dve", lambda e: e.reciprocal(rstd_.ap[:], rstd_.ap[:]), reads=[rstd_], writes=[rstd_]))
                    ops.append(lambda: S.op("dve", lambda e: e.scalar_tensor_tensor(nmr_.ap[:], mv_.ap[:, 0:1], -1.0, rstd_.ap[:], ALU.mult, ALU.mult),
                                            reads=[mv_, rstd_], writes=[nmr_]))
                stats_ops(A_)
                ops.append(lambda: S.op("act", lambda e: e.activation(A_.ap[:], A_.ap[:], AF.Identity, bias=nmr_.ap[:], scale=rstd_.ap[:]),
                                        reads=[A_, nmr_, rstd_], writes=[A_]))
                ops.append(lambda: S.op("dve", lambda e: e.tensor_tensor(A_.ap[:], A_.ap[:], bc.ap[:, 1, :], ALU.mult), reads=[A_, bc_t], writes=[A_]))
                ops.append(lambda: S.op("dve", lambda e: e.tensor_tensor(A_.ap[:], A_.ap[:], bc.ap[:, 2, :], ALU.add), reads=[A_, bc_t], writes=[A_]))
                ops.append(lambda: S.dma("sp", x1_d[tile_ * 128:(tile_ + 1) * 128, :], A_.ap[:], reads=[A_], writes=[x1_t[tile_]]))
                stats_ops(A_)
                ops.append(lambda: S.op("act", lambda e: e.activation(B_.ap[:], A_.ap[:], AF.Identity, bias=nmr_.ap[:], scale=rstd_.ap[:]),
                                        reads=[A_, nmr_, rstd_], writes=[B_]))

                def o_trf():
                    c["pst"] = [PS.reserve(), PS.reserve()]
                    pst = c["pst"]

                    def trf(e):
                        for k in range(8):
                            ins = e.transpose(pst[k // 4].ap[:, (k % 4) * 128:(k % 4 + 1) * 128], B_.ap[:, k * 128:(k + 1) * 128], ident_f.ap[:])
                        return ins
                    S.op("pe", trf, reads=[B_, ident_f], writes=pst)
                ops.append(o_trf)
                for k in range(8):
                    def o_ev(k=k):
                        pst = c["pst"]
                        src_ap = pst[k // 4].ap[:, (k % 4) * 128:(k % 4 + 1) * 128]
                        if k % 2 == 0:
                            S.op("act", lambda e: e.activation(
                                h2f.ap[:, k, :], src_ap, AF.Identity, bias=cols.ap[:, 5, k:k + 1], scale=cols.ap[:, 4, k:k + 1]),
                                reads=[pst[k // 4], cols], writes=[h2f])
                        else:
                            S.op("dve", lambda e: e.tensor_scalar(
                                h2f.ap[:, k, :], src_ap, cols.ap[:, 4, k:k + 1], cols.ap[:, 5, k:k + 1], ALU.mult, ALU.add),
                                reads=[pst[k // 4], cols], writes=[h2f])
                        if k == 3:
                            PS.release(pst[0])
                        if k == 7:
                            PS.release(pst[1])
                    ops.append(o_ev)
                ops.append(lambda: S.op("act", lambda e: e.activation(hT.ap[:, :, 256 + tile_ * 128:256 + (tile_ + 1) * 128], h2f.ap[:], AF.Copy),
                                        reads=[h2f], writes=[hT_t[2 + tile_]]))

                def o_mml():
                    c["psl"] = PS.reserve()
                    psl = c["psl"]

                    def mml(e):
                        for k in range(8):
                            ins = e.matmul(psl.ap[:, 0:64], h2f.ap[:, k, :], Wr.ap[:, k, :], start=(k == 0), stop=(k == 7))
                        return ins
                    S.op("pe", mml, reads=[h2f, Wr], writes=[psl])
                ops.append(o_mml)

                def o_sig():
                    psl = c["psl"]
                    S.op("act", lambda e: e.activation(scr.ap[:], psl.ap[:, 0:64], AF.Sigmoid), reads=[psl], writes=[scr])
                    PS.release(psl)
                ops.append(o_sig)
                ops.append(lambda: S.op("dve", lambda e: e.tensor_tensor(sel.ap[:], scr.ap[:], rb.ap[:], ALU.add), reads=[scr, rb], writes=[sel]))
                for g in range(8):
                    ops.append(lambda g=g: S.op("dve", lambda e: e.max(m8.ap[:, g, :], sel.ap[:, g * 8:(g + 1) * 8]), reads=[sel], writes=[m8]))
                ops.append(lambda: S.op("dve", lambda e: e.tensor_tensor(gs.ap[:], m8.ap[:, :, 0], m8.ap[:, :, 1], ALU.add), reads=[m8], writes=[gs]))
                ops.append(lambda: S.op("dve", lambda e: e.max(gm8.ap[:], gs.ap[:]), reads=[gs], writes=[gm8]))
                ops.append(lambda: S.op("dve", lambda e: e.tensor_scalar(pen.ap[:], gs.ap[:], gm8.ap[:, 3:4], None, ALU.is_ge), reads=[gs, gm8], writes=[pen]))
                ops.append(lambda: S.op("dve", lambda e: e.tensor_scalar(pen.ap[:], pen.ap[:], BIG, -BIG, ALU.mult, ALU.add), reads=[pen], writes=[pen]))
                ops.append(lambda: S.op("dve", lambda e: e.tensor_tensor(
                    selm.ap[:].rearrange("p (g j) -> p g j", j=8), sel.ap[:].rearrange("p (g j) -> p g j", j=8),
                    pen.ap[:].rearrange("p (g o) -> p g o", o=1).broadcast_to([128, 8, 8]), ALU.add), reads=[sel, pen], writes=[selm]))
                ops.append(lambda: S.op("dve", lambda e: e.max(e8.ap[:], selm.ap[:]), reads=[selm], writes=[e8]))
                ops.append(lambda: S.op("dve", lambda e: e.tensor_scalar(wsel.ap[:], selm.ap[:], e8.ap[:, 7:8], None, ALU.is_ge), reads=[selm, e8], writes=[wsel]))
                ops.append(lambda: S.op("dve", lambda e: e.tensor_tensor(wsel.ap[:], wsel.ap[:], scr.ap[:], ALU.mult), reads=[wsel, scr], writes=[wsel]))
                ops.append(lambda: S.op("dve", lambda e: e.tensor_reduce(wsum.ap[:], wsel.ap[:], AX.X, ALU.add), reads=[wsel], writes=[wsum]))
                ops.append(lambda: S.op("dve", lambda e: e.reciprocal(wsum.ap[:], wsum.ap[:]), reads=[wsum], writes=[wsum]))
                ops.append(lambda: S.op("dve", lambda e: e.tensor_scalar(gtm.ap[:, tile_, 0:64], wsel.ap[:], wsum.ap[:, 0:1], 2.5, ALU.mult, ALU.mult),
                                        reads=[wsel, wsum], writes=[gtm_t[tile_]]))
                return ops
            for pair in range(2):
                interleave([tile_ops(pair * 2), tile_ops(pair * 2 + 1)])
        for qt in range(4):
            quarter(qt)
    if stage == 5:
        o_x1 = dout("o_x1", [NOWN, D])
        o_g = dout("o_g", [128, 16 * 65])
        S.dma("sp", o_x1, x1_d, reads=x1_t, writes=[S.tr("o1")])
        S.dma("sp", o_g, gtm.ap[:].rearrange("p a b -> p (a b)"), reads=gtm_t, writes=[S.tr("o2")])
        o_h2 = dout("o_h2", [128, 8 * NOWN], BF16)
        S.dma("sp", o_h2.rearrange("p (k t) -> p k t", k=8), hT.ap[:, :, 256:256 + NOWN], reads=hT_t, writes=[S.tr("o3")])
        S.finish()
        return nc

    outer = S.scope()
    outer.__enter__()
    y2 = S.sb("y2", [128, 16, D], F32)
    y2_t = [S.tr("y2_%d" % i) for i in range(16)]
    with S.scope():
        STG = Stage(S, 4, 2048, "p6")
        Wg = [S.sb("Wg%d" % i, [128, 8, 256], BF16) for i in range(2)]
        Wu = [S.sb("Wu%d" % i, [128, 8, 256], BF16) for i in range(2)]
        Wd = [S.sb("Wd%d" % i, [128, 2, D], BF16) for i in range(2)]
        sa = [S.sb("sa%d" % i, [128, 512], F32) for i in range(4)]
        actT = [S.sb("actT%d" % i, [128, 2, 512], BF16) for i in range(3)]

        def load_expert(e):
            b = e % 2
            STG.load_cast(w_eg[e].rearrange("(k p) n -> p k n", p=128), 2048, Wg[b].ap[:], Wg[b])
            STG.load_cast(w_eu[e].rearrange("(k p) n -> p k n", p=128), 2048, Wu[b].ap[:], Wu[b])
            STG.load_cast(w_ed[e].rearrange("(k p) n -> p k n", p=128), 2048, Wd[b].ap[:], Wd[b])

        NE = 65
        uctx = {}

        def u0_ops(u):
            e, tb = u // 4, u % 4
            b = e % 2
            pp = []
            uctx[u] = pp
            ops = []
            for i in range(8):
                def chunk(i=i):
                    fc, sub = i // 4, i % 4
                    if sub == 0:
                        pp.append((PS.reserve(), PS.reserve()))
                    psa, psu = pp[fc]
                    dstp = psa if sub < 2 else psu
                    W = Wg[b] if sub < 2 else Wu[b]
                    k0 = (sub % 2) * 4

                    def mm(en):
                        for k in range(k0, k0 + 4):
                            ins = en.matmul(dstp.ap[:], W.ap[:, k, fc * 128:(fc + 1) * 128], hT.ap[:, k, 256 + tb * 512:256 + (tb + 1) * 512],
                                            start=(k == 0), stop=(k == 7))
                        return ins
                    S.op("pe", mm, reads=[W] + hT_t[2 + tb * 4:2 + tb * 4 + 4], writes=[dstp])
                ops.append(chunk)
            return ops

        def u1(u):
            pp = uctx.pop(u)
            A = actT[u % 3]
            for fc in range(2):
                psa, psu = pp[fc]
                s_ = sa[(u % 2) * 2 + fc]
                S.op("act", lambda en, psa=psa, s_=s_: en.activation(s_.ap[:], psa.ap[:], AF.Silu), reads=[psa], writes=[s_])
                S.op("dve", lambda en, psu=psu, s_=s_, fc=fc: en.tensor_tensor(A.ap[:, fc, :], s_.ap[:], psu.ap[:], ALU.mult),
                     reads=[s_, psu], writes=[A])
                PS.release(psa)
                PS.release(psu)

        def u2_ops(u):
            e, tb = u // 4, u % 4
            b = e % 2
            A = actT[u % 3]
            ops = []
            for tt in range(4):
                for dh in range(2):
                    def dn(tt=tt, dh=dh):
                        tile_ = tb * 4 + tt
                        psd = PS.get()

                        def mmd(en):
                            for fc in range(2):
                                ins = en.matmul(psd.ap[:], A.ap[:, fc, tt * 128:(tt + 1) * 128], Wd[b].ap[:, fc, dh * 512:(dh + 1) * 512],
                                                start=(fc == 0), stop=(fc == 1))
                            return ins
                        S.op("pe", mmd, reads=[A, Wd[b]], writes=[psd])
                        dst = y2.ap[:, tile_, dh * 512:(dh + 1) * 512]
                        gcol = gtm.ap[:, tile_, e:e + 1]
                        if e == 0:
                            S.op("act", lambda en: en.activation(dst, psd.ap[:], AF.Identity, scale=gcol),
                                 reads=[psd, gtm_t[tile_]], writes=[y2_t[tile_]])
                        else:
                            S.op("dve", lambda en: en.scalar_tensor_tensor(dst, psd.ap[:], gcol, dst, ALU.mult, ALU.add),
                                 reads=[psd, gtm_t[tile_], y2_t[tile_]], writes=[y2_t[tile_]])
                    ops.append(dn)
            return ops
        load_expert(0)
        NU = NE * 4
        for step in range(NU + 2):
            if 0 <= step - 1 < NU:
                u1(step - 1)
            la = u2_ops(step - 2) if 0 <= step - 2 < NU else []
            lb = u0_ops(step) if step < NU else []
            interleave([la, lb])
            if step < NU and step % 4 == 1 and step // 4 + 1 < NE:
                load_expert(step // 4 + 1)

    with S.scope():
        bc7 = S.sb("bc7", [128, 3, D], F32)
        bc7_t = S.tr("bc7t")
        S.dma("sp", bc7.ap[:, 0, :], mod_d[0:1, 5120:6144].partition_broadcast(128), writes=[bc7_t])
        S.dma("sp", bc7.ap[:, 1, :], lnp[2:3, :].partition_broadcast(128), writes=[bc7_t])
        S.dma("sp", bc7.ap[:, 2, :], lnp[3:4, :].partition_broadcast(128), writes=[bc7_t])
        tmp7s = [S.sb("tmp7_%d" % i, [128, D], F32) for i in range(2)]
        r7s = [S.sb("r7_%d" % i, [128, D], F32) for i in range(2)]
        o7 = [S.sb("o7_%d" % i, [128, D], F32) for i in range(2)]
        out_t = S.tr("out")
        def fin_ops(tile_):
            bs = tile_ % 2
            xt = xts[bs]
            tmp7 = tmp7s[bs]
            r7 = r7s[bs]
            ob = o7[bs]
            st_, mv_, rstd_, nmr_, _x = work[bs]
            ops = []

            def add(*a_, **k_):
                ops.append(lambda: S.op(*a_, **k_))
            ops.append(lambda: S.dma("sp", xt.ap[:], x1_d[tile_ * 128:(tile_ + 1) * 128, :], reads=[x1_t[tile_]], writes=[xt]))
            add("dve", lambda e: e.tensor_tensor(tmp7.ap[:], y2.ap[:, tile_, :], bc7.ap[:, 0, :], ALU.mult),
                reads=[y2_t[tile_], bc7_t], writes=[tmp7])
            add("dve", lambda e: e.scalar_tensor_tensor(r7.ap[:], xt.ap[:], ALPHA, tmp7.ap[:], ALU.mult, ALU.add),
                reads=[xt, tmp7], writes=[r7])
            for i in range(2):
                add("dve", lambda e, i=i: e.bn_stats(st_.ap[:, i, :], r7.ap[:, i * 512:(i + 1) * 512]), reads=[r7], writes=[st_])
            add("dve", lambda e: e.bn_aggr(mv_.ap[:], st_.ap[:].rearrange("p a b -> p (a b)")), reads=[st_], writes=[mv_])
            add("act", lambda e: e.activation(rstd_.ap[:], mv_.ap[:, 1:2], AF.Sqrt, bias=EPS, scale=1.0), reads=[mv_], writes=[rstd_])
            add("dve", lambda e: e.reciprocal(rstd_.ap[:], rstd_.ap[:]), reads=[rstd_], writes=[rstd_])
            add("dve", lambda e: e.scalar_tensor_tensor(nmr_.ap[:], mv_.ap[:, 0:1], -1.0, rstd_.ap[:], ALU.mult, ALU.mult),
                reads=[mv_, rstd_], writes=[nmr_])
            add("act", lambda e: e.activation(r7.ap[:], r7.ap[:], AF.Identity, bias=nmr_.ap[:], scale=rstd_.ap[:]),
                reads=[r7, nmr_, rstd_], writes=[r7])
            add("dve", lambda e: e.tensor_tensor(tmp7.ap[:], r7.ap[:], bc7.ap[:, 1, :], ALU.mult), reads=[r7, bc7_t], writes=[tmp7])
            add("dve", lambda e: e.tensor_tensor(ob.ap[:], tmp7.ap[:], bc7.ap[:, 2, :], ALU.add), reads=[tmp7, bc7_t], writes=[ob])
            ops.append(lambda: S.dma("sp", out_d[tile_ * 128:(tile_ + 1) * 128, :], ob.ap[:], reads=[ob], writes=[out_t]))
            return ops
        for pair in range(8):
            interleave([fin_ops(2 * pair), fin_ops(2 * pair + 1)])
    outer.__exit__(None, None, None)
    S.finish()
    return nc


def _consts():
    ident = np.eye(128, dtype=np.float32)
    Rm = np.zeros((128, 128), np.float32)
    for m in range(128):
        w = m % 32
        if w < 16:
            Rm[m + 16, m] = -1.0
        else:
            Rm[m - 16, m] = 1.0
    j = np.arange(128)[:, None]
    i = np.arange(128)[None, :]
    valid = (j < 64) & (i < 64)
    mF = (valid & (j <= i)).astype(np.float32)[:, 0:64]
    mB = (valid & (j >= i)).astype(np.float32)[:, 0:64]
    cb = np.concatenate([ident, Rm] + [mF, mB] * 4, axis=1).astype(ml_dtypes.bfloat16)
    return cb, ident


def col_layout(v):
    return np.ascontiguousarray(v.reshape(-1, 128).T)


def prep_inputs(inp, cid):
    b, j = cid // 4, cid % 4
    x = inp["x"][b]
    tok0 = 2048 * j
    m = {}
    m["x_own"] = np.ascontiguousarray(x[tok0:tok0 + 2048])
    halo = np.zeros((512, D), np.float32)
    if j > 0:
        halo[0:256] = x[tok0 - 256:tok0]
    if j < 3:
        halo[256:512] = x[tok0 + 2048:tok0 + 2048 + 256]
    m["x_halo"] = halo
    ctx = inp["ctx"][b]
    m["ctx2"] = np.ascontiguousarray(np.concatenate([ctx, ctx[::-1]], axis=0))
    segs = []
    wfs = []
    lbrows = [inp["hg_lb_fwd"][0], inp["hg_lb_fwd"][1], inp["hg_lb_bwd"][0], inp["hg_lb_bwd"][1]]
    fl = np.zeros((128, 6), np.float32)
    w_in = inp["w_in"][0]
    order = [("f", s) for s in range(0, j)] + [("b", s) for s in range(3, j, -1)]
    for si, (d_, s) in enumerate(order):
        seg = x[2048 * s:2048 * (s + 1)]
        if d_ == "f":
            segs.append(seg)
            wfs.append(w_in[:, 1024:2048])
            lbrows += [inp["hg_lb_fwd"][0], inp["hg_lb_fwd"][1]]
            fl[:, si] = 1.0
        else:
            segs.append(seg[::-1])
            wfs.append(w_in[:, 2048:3072])
            lbrows += [inp["hg_lb_bwd"][0], inp["hg_lb_bwd"][1]]
            fl[:, 3 + si] = 1.0
    m["x_slots"] = np.ascontiguousarray(np.concatenate(segs, axis=0))
    m["wf_slots"] = np.ascontiguousarray(np.stack(wfs))
    lbr = np.stack(lbrows)
    m["lbT"] = np.ascontiguousarray(lbr.reshape(10, 8, 128).transpose(2, 0, 1).reshape(128, 80))
    m["flags"] = fl
    cv = np.stack([inp["c"][b], inp["c_ctx"]])
    m["cvecT"] = np.ascontiguousarray(cv.reshape(2, 8, 128).transpose(2, 1, 0).reshape(128, 16))
    m["w_ada"] = inp["w_ada"][0]
    m["b_row"] = inp["b_ada"][0:1]
    m["b_col"] = col_layout(inp["b_ada"][0])
    m["w_in"] = w_in
    m["hgT"] = col_layout(inp["hg_norm_g"][0])
    m["w_a"] = inp["w_branch_a"][0]
    m["w_b"] = inp["w_branch_b"][0]
    m["w_o"] = inp["w_out"][0]
    m["lnp"] = np.ascontiguousarray(np.stack([inp["ln1_g"][0], inp["ln1_b"][0], inp["ln2_g"][0], inp["ln2_b"][0]]))
    m["w_r"] = inp["w_router"][0]
    m["rbias"] = inp["router_bias"][0:1]
    m["w_eg"] = inp["_w_eg"]
    m["w_eu"] = inp["_w_eu"]
    m["w_ed"] = inp["_w_ed"]
    t = np.arange(NKB)
    g = 2048 * j - 256 + t
    row = np.floor_divide(g, 64).astype(np.float32)
    col = np.mod(g, 64).astype(np.float32)
    inv = np.power(np.float32(10000.0), -np.arange(0, 32, 2, dtype=np.float32) / np.float32(32)).astype(np.float32)
    dd = np.arange(64)
    pos = np.where((dd < 32)[:, None], row[None, :], col[None, :]).astype(np.float32)
    ang = (pos * inv[dd % 16][:, None]).astype(np.float32)
    m["cosT"] = np.ascontiguousarray(np.tile(np.cos(ang).astype(np.float32), (2, 1)))
    m["sinT"] = np.ascontiguousarray(np.tile(np.sin(ang).astype(np.float32), (2, 1)))
    rpb = inp["na_rpb"][0]
    qq = np.arange(64)[:, None]
    kc = np.arange(64)[None, :]
    cs = np.clip(qq - 8, 0, 48)
    inwin = (kc >= cs) & (kc < cs + 16)
    dc = np.clip(kc - qq + 15, 0, 30)
    NEG = np.float32(-1e30)

    def Bfor(head, drs):
        out = np.full((64, len(drs), 64), NEG, np.float32)
        for s_, dr in enumerate(drs):
            if dr is not None and 0 <= dr <= 14:
                out[:, s_, :] = np.where(inwin, rpb[head, dr][dc], NEG)
        return out.reshape(64, -1)
    Bint = np.zeros((8, 128, 512), np.float32)
    Bspec = np.zeros((8, 128, 7, 768), np.float32)
    for pr in range(8):
        for hh in range(2):
            head = pr * 2 + hh
            Bint[pr, hh * 64:(hh + 1) * 64] = Bfor(head, list(range(3, 11)))
            for si, lr in enumerate([0, 1, 2, 3, 29, 30, 31]):
                r = 32 * j + lr
                rs_ = int(np.clip(r - 4, 0, 120))
                rel0 = 0 if lr < 4 else 27
                drs = []
                for s_ in range(12):
                    kg = 32 * j - 4 + rel0 + s_
                    drs.append(kg - r + 7 if rs_ <= kg < rs_ + 8 else None)
                Bspec[pr, hh * 64:(hh + 1) * 64, si] = Bfor(head, drs)
    m["Bint"] = Bint
    m["Bspec"] = np.ascontiguousarray(Bspec.reshape(8, 128, 7 * 768))
    cb, ident = _consts()
    m["consts_bf"] = cb
    m["ident_f"] = ident
    return m


_CACHE = {}


def kernel(**inputs):
    inp = {k: np.asarray(v) for k, v in inputs.items()}
    inp["_w_eg"] = np.ascontiguousarray(np.concatenate([inp["w_e_gate"][0], inp["w_sh_gate"]], axis=0))
    inp["_w_eu"] = np.ascontiguousarray(np.concatenate([inp["w_e_up"][0], inp["w_sh_up"]], axis=0))
    inp["_w_ed"] = np.ascontiguousarray(np.concatenate([inp["w_e_down"][0], inp["w_sh_down"]], axis=0))
    if "nc" not in _CACHE:
        _CACHE["nc"] = build_program(99)
    nc = _CACHE["nc"]
    maps = [prep_inputs(inp, c) for c in range(8)]
    res = run_bass_kernel_spmd(nc, maps, core_ids=list(range(8)))
    out = np.zeros((2, 8192, D), np.float32)
    for c in range(8):
        b, j = c // 4, c % 4
        out[b, 2048 * j:2048 * (j + 1)] = res.results[c]["out"]
    return out
```

```python
import contextlib
import numpy as np
import ml_dtypes
import concourse.bass as bass
import concourse.mybir as mybir
from concourse.bass_utils import run_bass_kernel_spmd

F32 = mybir.dt.float32
BF16 = mybir.dt.bfloat16
AF = mybir.ActivationFunctionType
ALU = mybir.AluOpType
AX = mybir.AxisListType


class Tr:
    def __init__(self, name):
        self.name = name
        self.w = None
        self.r = {}
        self.dsem = None


class Buf(Tr):
    def __init__(self, name, handle):
        super().__init__(name)
        self.h = handle
        self.ap = handle[:] if not hasattr(handle, "ap") or not callable(getattr(handle, "ap")) else handle.ap()


class Sched:
    ENG = ("pe", "act", "dve", "pool", "sp")

    def __init__(self, nc):
        self.nc = nc
        self.gstack = contextlib.ExitStack()
        self.stack = self.gstack
        self.prog = {e: [] for e in self.ENG}
        self.sem = {e: self.gstack.enter_context(nc.semaphore("s_" + e)) for e in self.ENG}
        self.cnt = {e: 0 for e in self.ENG}
        self.seen = {e: {} for e in self.ENG}
        self.dsems = []
        self.dfree = []
        self.scope_trs = [[]]
        self.ninst = 0

    def sb(self, name, shape, dtype):
        h = self.stack.enter_context(self.nc.sbuf_tensor(name, list(shape), dtype))
        return Buf(name, h)

    def ps(self, name, shape, dtype):
        h = self.stack.enter_context(self.nc.psum_tensor(name, list(shape), dtype))
        return Buf(name, h)

    def tr(self, name="t"):
        return Tr(name)

    def _need(self, eng, reads, writes, skip_pe_waw=False):
        need = {}

        def add(key, semh, val):
            if key not in need or need[key][1] < val:
                need[key] = (semh, val)

        for t in reads:
            if t.w is not None:
                add(*t.w)
        for t in writes:
            if t.w is not None:
                if not (skip_pe_waw and t.w[0] == "pe"):
                    add(*t.w)
            for key, (semh, val) in t.r.items():
                add(key, semh, val)
        out = []
        for key, (semh, val) in need.items():
            if self.seen[eng].get(key, 0) < val:
                self.seen[eng][key] = val
                out.append((semh, val))
        return out

    def _emit_waits(self, eng, waits):
        for semh, val in waits:
            self.prog[eng].append(lambda e, semh=semh, val=val: e.wait_ge(semh, val))

    def op(self, eng, fn, reads=(), writes=()):
        waits = self._need(eng, reads, writes, skip_pe_waw=(eng == "pe"))
        self._emit_waits(eng, waits)
        self.cnt[eng] += 1
        self.ninst += 1
        val = self.cnt[eng]
        semh = self.sem[eng]
        self.prog[eng].append(lambda e, fn=fn, semh=semh: fn(e).then_inc(semh, 1))
        rec = (eng, semh, val)
        for t in reads:
            t.r[eng] = (semh, val)
        for t in writes:
            t.w = rec
            t.r = {}

    def dma(self, eng, out_ap, in_ap, reads=(), writes=()):
        waits = self._need(eng, reads, writes)
        self._emit_waits(eng, waits)
        t0 = writes[0]
        if t0.dsem is None:
            if self.dfree:
                t0.dsem = self.dfree.pop()
            else:
                h = self.gstack.enter_context(self.nc.semaphore("d%d" % len(self.dsems)))
                self.dsems.append([h, 0])
                t0.dsem = len(self.dsems) - 1
            self.scope_trs[-1].append(t0)
        rec = self.dsems[t0.dsem]
        rec[1] += 16
        semh, val = rec[0], rec[1]
        key = ("d", t0.dsem)
        self.prog[eng].append(lambda e, o=out_ap, i=in_ap, semh=semh: e.dma_start(out=o, in_=i).then_inc(semh, 16))
        for t in reads:
            t.r[key] = (semh, val)
        for t in writes:
            t.w = (key, semh, val)
            t.r = {}

    def barrier(self):
        for eng in self.ENG:
            for e2 in ("pe", "act", "dve", "pool"):
                if self.cnt[e2] and self.seen[eng].get(e2, 0) < self.cnt[e2]:
                    self.seen[eng][e2] = self.cnt[e2]
                    self.prog[eng].append(lambda e, semh=self.sem[e2], val=self.cnt[e2]: e.wait_ge(semh, val))
            for i, (h, c) in enumerate(self.dsems):
                key = ("d", i)
                if c and self.seen[eng].get(key, 0) < c:
                    self.seen[eng][key] = c
                    self.prog[eng].append(lambda e, semh=h, val=c: e.wait_ge(semh, val))

    def flush(self):
        nc = self.nc
        progs = self.prog
        self.prog = {e: [] for e in self.ENG}
        self.nblk = getattr(self, "nblk", 0) + 1
        with nc.named_scope("blk%d" % self.nblk), nc.Block() as block:
            @block.tensor
            def _(e):
                for f in progs["pe"]:
                    f(e)

            @block.scalar
            def _(e):
                for f in progs["act"]:
                    f(e)

            @block.vector
            def _(e):
                for f in progs["dve"]:
                    f(e)

            @block.gpsimd
            def _(e):
                for f in progs["pool"]:
                    f(e)

            @block.sync
            def _(e):
                for f in progs["sp"]:
                    f(e)

    @contextlib.contextmanager
    def scope(self):
        old = self.stack
        self.stack = contextlib.ExitStack()
        self.scope_trs.append([])
        try:
            yield
        finally:
            self.barrier()
            self.flush()
            for t in self.scope_trs.pop():
                self.dfree.append(t.dsem)
                t.dsem = None
            self.stack.close()
            self.stack = old

    def finish(self):
        self.barrier()
        self.flush()
        self.gstack.close()


D = 1024
NOWN = 2048
NKB = 2560
ALPHA = 2.0 ** 0.25
EPS = 1e-6


def interleave(lists):
    n = max(len(l) for l in lists)
    for i in range(n):
        for l in lists:
            if i < len(l):
                l[i]()


def pipeline(n, stages):
    ns = len(stages)
    for step in range(n + ns - 1):
        for s in range(ns - 1, -1, -1):
            i = step - s
            if 0 <= i < n:
                stages[s](i)


class PsumPool:
    def __init__(self, S):
        self.banks = [S.ps("psb%d" % i, [128, 512], F32) for i in range(8)]
        for b in self.banks:
            b.bf = b.ap.bitcast(BF16)
        self.i = 0
        self.reserved = set()

    def get(self):
        while True:
            b = self.banks[self.i]
            self.i = (self.i + 1) % 8
            if id(b) not in self.reserved:
                return b

    def reserve(self):
        b = self.get()
        self.reserved.add(id(b))
        return b

    def release(self, b):
        self.reserved.discard(id(b))


class Stage:
    def __init__(self, S, n=4, size=2048, tag=""):
        self.S = S
        self.slots = [S.sb("stg%s_%d" % (tag, i), [128, size], F32) for i in range(n)]
        self.i = 0
        self.size = size

    def load(self, src_ap, nfree):
        s = self.slots[self.i]
        self.i = (self.i + 1) % len(self.slots)
        p = src_ap.shape[0]
        view = s.ap[0:p, 0:nfree]
        if len(src_ap.shape) == 3:
            view = view.rearrange("p (a b) -> p a b", a=src_ap.shape[1])
        self.S.dma("sp", view, src_ap, writes=[s])
        return s, view

    def load_cast(self, src_ap, nfree, dst_ap, dst_tr, eng="act"):
        s, view = self.load(src_ap, nfree)
        if eng == "act":
            self.S.op("act", lambda e, o=dst_ap, i=view: e.activation(o, i, AF.Copy), reads=[s], writes=[dst_tr])
        else:
            self.S.op(eng, lambda e, o=dst_ap, i=view: e.tensor_copy(o, i), reads=[s], writes=[dst_tr])


def ln_tokens(S, PS, ident_bf, src, T, dst_fn, dst_trs, sc_ap_fn, sh_ap_fn, col_tr, xts, work):
    nt = T // 128

    def stA(t):
        st, mv, rstd, nmr, xb = work[t % len(work)]
        xt = xts[t % len(xts)]
        S.dma("sp", xt.ap[:], src[t * 128:(t + 1) * 128, :], writes=[xt])
        for i in range(2):
            S.op("dve", lambda e, i=i: e.bn_stats(st.ap[:, i, :], xt.ap[:, i * 512:(i + 1) * 512]), reads=[xt], writes=[st])
        S.op("dve", lambda e: e.bn_aggr(mv.ap[:], st.ap[:].rearrange("p a b -> p (a b)")), reads=[st], writes=[mv])
        S.op("act", lambda e: e.activation(rstd.ap[:], mv.ap[:, 1:2], AF.Sqrt, bias=EPS, scale=1.0), reads=[mv], writes=[rstd])
        S.op("dve", lambda e: e.reciprocal(rstd.ap[:], rstd.ap[:]), reads=[rstd], writes=[rstd])
        S.op("dve", lambda e: e.scalar_tensor_tensor(nmr.ap[:], mv.ap[:, 0:1], -1.0, rstd.ap[:], ALU.mult, ALU.mult),
             reads=[mv, rstd], writes=[nmr])
        S.op("act", lambda e: e.activation(xb.ap[:], xt.ap[:], AF.Identity, bias=nmr.ap[:], scale=rstd.ap[:]),
             reads=[xt, nmr, rstd], writes=[xb])

    def stB(t):
        st, mv, rstd, nmr, xb = work[t % len(work)]
        ps = PS.get()

        def tr(e):
            for k in range(8):
                ins = e.transpose(ps.bf[:, k * 128:(k + 1) * 128], xb.ap[:, k * 128:(k + 1) * 128], ident_bf.ap[:])
            return ins
        S.op("pe", tr, reads=[xb, ident_bf], writes=[ps])
        for k in range(8):
            if k % 2 == 0:
                S.op("act", lambda e, k=k: e.activation(
                    dst_fn(k, t), ps.bf[:, k * 128:(k + 1) * 128], AF.Identity, bias=sh_ap_fn(k), scale=sc_ap_fn(k)),
                    reads=[ps, col_tr], writes=[dst_trs[t]])
            else:
                S.op("dve", lambda e, k=k: e.tensor_scalar(
                    dst_fn(k, t), ps.bf[:, k * 128:(k + 1) * 128], sc_ap_fn(k), sh_ap_fn(k), ALU.mult, ALU.add),
                    reads=[ps, col_tr], writes=[dst_trs[t]])
    for step in range(nt + 1):
        if step < nt:
            stA(step)
        if step >= 1:
            stB(step - 1)


def build_program(stage=99):
    nc = bass.Bass("TRN2", target_bir_lowering=False)

    def din(name, shape, dt=F32):
        return nc.dram_tensor(name, list(shape), dt, kind="ExternalInput").ap()

    def dscr(name, shape, dt=F32):
        return nc.dram_tensor(name, list(shape), dt, kind="Internal").ap()

    def dout(name, shape, dt=F32):
        return nc.dram_tensor(name, list(shape), dt, kind="ExternalOutput").ap()

    x_own = din("x_own", [2048, D])
    x_halo = din("x_halo", [512, D])
    ctx2 = din("ctx2", [512, D])
    x_slots = din("x_slots", [6144, D])
    cvecT = din("cvecT", [128, 16])
    w_ada = din("w_ada", [D, 6 * D])
    b_row = din("b_row", [1, 6 * D])
    b_col = din("b_col", [128, 48])
    w_in = din("w_in", [D, 10 * D])
    wf_slots = din("wf_slots", [3, D, D])
    lbT = din("lbT", [128, 80])
    flags = din("flags", [128, 6])
    hgT = din("hgT", [128, 8])
    consts_bf = din("consts_bf", [128, 768], BF16)
    ident_f_d = din("ident_f", [128, 128])
    mod_d = dscr("mod_d", [2, 6 * D])
    w_a = din("w_a", [D, D])
    w_b = din("w_b", [D, D])
    w_o = din("w_o", [D, D])
    lnp = din("lnp", [4, D])
    w_r = din("w_r", [D, 64])
    rbias = din("rbias", [1, 64])
    w_eg = din("w_eg", [65, D, 256])
    w_eu = din("w_eu", [65, D, 256])
    w_ed = din("w_ed", [65, 256, D])
    x1_d = dscr("x1_d", [NOWN, D])
    g_d = dscr("g_d", [65, NOWN])
    out_d = dout("out", [NOWN, D])
    cosT_d = din("cosT", [128, NKB])
    sinT_d = din("sinT", [128, NKB])
    Bint_d = din("Bint", [8, 128, 512])
    Bspec_d = din("Bspec", [8, 128, 7 * 768])

    S = Sched(nc)
    PS = PsumPool(S)
    dbg = {}

    cb = S.sb("cb", [128, 768], BF16)
    S.dma("sp", cb.ap[:], consts_bf, writes=[cb])
    ident_bf = Buf.__new__(Buf)
    Tr.__init__(ident_bf, "ident_bf")
    ident_bf.ap = cb.ap[:, 0:128]
    ident_bf.w = cb.w
    ident_f = S.sb("ident_f_sb", [128, 128], F32)
    S.dma("sp", ident_f.ap[:], ident_f_d, writes=[ident_f])
    cols = S.sb("cols", [128, 6, 8], F32)
    lbv = S.sb("lbv", [128, 5, 8], F32)
    oml = S.sb("oml", [128, 5, 8], F32)
    flg = S.sb("flg", [128, 6], F32)
    S.dma("sp", flg.ap[:], flags, writes=[flg])
    hgc = S.sb("hgc", [128, 8], F32)
    S.dma("sp", hgc.ap[:], hgT, writes=[hgc])
    hT = S.sb("hT", [128, 8, NKB], BF16)
    hT_t = [S.tr("hT%d" % i) for i in range(20)]
    xts = [S.sb("gxt%d" % i, [128, 1024], F32) for i in range(2)]
    work = [(S.sb("gst%d" % i, [128, 2, 6], F32), S.sb("gmv%d" % i, [128, 2], F32), S.sb("grstd%d" % i, [128, 1], F32),
             S.sb("gnmr%d" % i, [128, 1], F32), S.sb("gxb%d" % i, [128, 1024], BF16)) for i in range(2)]
    hgs = S.scope()
    hgs.__enter__()
    ones_bf = S.sb("ones_bf", [128, 2048], BF16)
    S.op("pool", lambda e: e.memset(ones_bf.ap[:], 1.0), writes=[ones_bf])
    rmask = S.sb("rmask", [128, 2048], BF16)
    S.op("pool", lambda e: e.memset(rmask.ap[:], 1.0), writes=[rmask])
    S.op("pool", lambda e: e.memset(rmask.ap[:].rearrange("p (c t) -> p c t", t=64)[:, :, 0:1], 0.0), writes=[rmask])
    Sf0 = S.sb("Sf0", [128, 8, 128], F32)
    Sb0 = S.sb("Sb0", [128, 8, 128], F32)
    S.op("pool", lambda e: e.memset(Sf0.ap[:], 0.0), writes=[Sf0])
    S.op("pool", lambda e: e.memset(Sb0.ap[:], 0.0), writes=[Sb0])

    with S.scope():
        STG = Stage(S, 4, 2048, "p0")
        condT = S.sb("condT", [128, 16], F32)
        S.dma("sp", condT.ap[:], cvecT, writes=[condT])
        S.op("act", lambda e: e.activation(condT.ap[:], condT.ap[:], AF.Silu), reads=[condT], writes=[condT])
        bcol = S.sb("bcol", [128, 48], F32)
        S.dma("sp", bcol.ap[:], b_col, writes=[bcol])
        brow = S.sb("brow", [2, 6 * D], F32)
        brow_t = [S.tr("brow0"), S.tr("brow1")]
        for r in range(2):
            S.dma("sp", brow.ap[r:r + 1, :], b_row, writes=[brow_t[r]])
        modrow = S.sb("modrow", [2, 6 * D], F32)
        modcol = S.sb("modcol", [128, 48, 2], F32)
        lbraw = S.sb("lbraw", [128, 80], F32)
        S.dma("sp", lbraw.ap[:], lbT, writes=[lbraw])
        lbd = S.sb("lbd", [128, 5, 8], F32)
        lb4 = lbraw.ap[:].rearrange("p (a two h) -> p a two h", two=2, h=8)
        S.op("dve", lambda e: e.tensor_tensor(lbd.ap[:], lb4[:, :, 0, :], lb4[:, :, 1, :], ALU.subtract),
             reads=[lbraw], writes=[lbd])
        S.op("act", lambda e: e.activation(lbv.ap[:], lbd.ap[:], AF.Sigmoid), reads=[lbd], writes=[lbv])
        S.op("dve", lambda e: e.tensor_scalar(oml.ap[:], lbv.ap[:], -1.0, 1.0, ALU.mult, ALU.add),
             reads=[lbv], writes=[oml])
        psC = PS.reserve()
        w_ada_v = w_ada.rearrange("(k p) n -> p k n", p=128)
        for blk in range(24):
            stg, view = STG.load(w_ada_v[:, :, blk * 256:(blk + 1) * 256], 2048)

            psR = PS.get()

            def mmr(e, view=view, psR=psR):
                for k in range(8):
                    ins = e.matmul(psR.ap[0:2, 0:256], condT.ap[:, 2 * k:2 * k + 2], view[:, k, :],
                                   start=(k == 0), stop=(k == 7))
                return ins
            S.op("pe", mmr, reads=[stg, condT], writes=[psR])
            S.op("dve", lambda e, psR=psR, blk=blk: e.tensor_tensor(
                modrow.ap[0:2, blk * 256:(blk + 1) * 256], psR.ap[0:2, 0:256],
                brow.ap[0:2, blk * 256:(blk + 1) * 256], ALU.add),
                reads=[psR] + brow_t, writes=[modrow])
        S.dma("sp", mod_d, modrow.ap[0:2, :], reads=[modrow], writes=[S.tr("mod_d")])
        def trc(e):
            for c in range(48):
                ins = e.transpose(psC.ap[:, 2 * c:2 * c + 2], modrow.ap[0:2, c * 128:(c + 1) * 128], ident_f.ap[0:2, 0:2])
            return ins
        S.op("pe", trc, reads=[modrow, ident_f], writes=[psC])
        S.op("dve", lambda e: e.tensor_copy(modcol.ap[:].rearrange("p c r -> p (c r)"), psC.ap[:, 0:96]), reads=[psC], writes=[modcol])
        for ci, (c0, r, addone) in enumerate([(8, 0, 1.0), (0, 0, 0.0), (8, 1, 1.0), (0, 1, 0.0), (32, 0, 1.0), (24, 0, 0.0)]):
            S.op("dve", lambda e, ci=ci, c0=c0, r=r, addone=addone: e.tensor_scalar(
                cols.ap[:, ci, :], modcol.ap[:, c0:c0 + 8, r], addone, None, ALU.add),
                reads=[modcol], writes=[cols])
        PS.release(psC)
    if stage == 0:
        o_cols = dout("o_cols", [128, 48])
        o_lb = dout("o_lb", [128, 40])
        S.dma("sp", o_cols, cols.ap[:].rearrange("p a b -> p (a b)"), reads=[cols], writes=[S.tr("o1")])
        S.dma("sp", o_lb, lbv.ap[:].rearrange("p a b -> p (a b)"), reads=[lbv], writes=[S.tr("o2")])
        o_mod = dout("o_mod", [2, 6 * D])
        S.dma("sp", o_mod, mod_d, reads=[], writes=[S.tr("o3")])
        S.finish()
        return nc

    with S.scope():
        STG = Stage(S, 4, 1024, "p2")
        hTs = hT
        hTs_t = hT_t[0:16]
        WfB = S.sb("WfB", [128, 8, 1024], BF16)
        WiB = S.sb("WiB", [128, 8, 1024], BF16)
        T1 = S.sb("T1", [128, 2048], F32)
        T2 = S.sb("T2", [128, 2048], F32)
        T3 = S.sb("T3", [128, 2048], F32)
        T4 = S.sb("T4", [128, 2048], F32)
        kd = S.sb("kd", [128, 2048], BF16)
        kd_tm = S.sb("kd_tm", [128, 16, 128], BF16)
        v_tm = S.sb("v_tm", [128, 16, 128], BF16)
        tmpS = S.sb("tmpS", [128, 128], F32)
        tmpD = S.sb("tmpD", [128, 128], F32)
        Dcol = S.sb("Dcol", [128, 1], F32)
        w_in_v = w_in.rearrange("(k p) n -> p k n", p=128)
        for q8 in range(8):
            STG.load_cast(w_in_v[:, :, 3072 + q8 * 128:3072 + (q8 + 1) * 128], 1024,
                          WiB.ap[:, :, q8 * 128:(q8 + 1) * 128], WiB)
        slots = [
            (ctx2[0:256, :], 256, 2, 3, w_in_v[:, :, 1024:2048], 0, "f"),
            (ctx2[256:512, :], 256, 2, 3, w_in_v[:, :, 2048:3072], 1, "b"),
        ]
        for s in range(3):
            slots.append((x_slots[s * 2048:(s + 1) * 2048, :], 2048, 0, 1,
                          wf_slots[s].rearrange("(k p) n -> p k n", p=128), 2 + s, s))
        def slot_body(src, T, ci_sc, ci_sh, wf_src, lbi, mode):
            nt = T // 128
            TB = min(512, T)
            ln_tokens(S, PS, ident_bf, src, T,
                      lambda k, t: hTs.ap[:, k, t * 128:(t + 1) * 128], hTs_t,
                      lambda k, ci=ci_sc: cols.ap[:, ci, k:k + 1], lambda k, ci=ci_sh: cols.ap[:, ci, k:k + 1],
                      cols, xts, work)
            for q8 in range(8):
                STG.load_cast(wf_src[:, :, q8 * 128:(q8 + 1) * 128], 1024, WfB.ap[:, :, q8 * 128:(q8 + 1) * 128], WfB)
            vctx = {}

            def pA1(h):
                hs = slice(h * 128, (h + 1) * 128)
                for tb in range(T // TB):
                    ps = PS.get()
                    ts_ = slice(tb * TB, (tb + 1) * TB)
                    def mmf(e, ps=ps, ts_=ts_, hs=hs):
                        for k in range(8):
                            ins = e.matmul(ps.ap[:, 0:TB], WfB.ap[:, k, hs], hTs.ap[:, k, ts_], start=(k == 0), stop=(k == 7))
                        return ins
                    S.op("pe", mmf, reads=[WfB] + hTs_t[tb * TB // 128:(tb + 1) * TB // 128], writes=[ps])
                    S.op("act", lambda e, ps=ps, ts_=ts_: e.activation(T1.ap[:, ts_], ps.ap[:, 0:TB], AF.Sigmoid),
                         reads=[ps], writes=[T1])

            def pA2(h):
                hs = slice(h * 128, (h + 1) * 128)
                vps = []
                vctx[h] = vps
                for g in range((nt + 3) // 4):
                    n = min(4, nt - g * 4)
                    ps = PS.reserve()
                    vps.append((ps, g, n))
                    def mmv(e, ps=ps, g=g, n=n, hs=hs):
                        for j in range(n):
                            t = g * 4 + j
                            for k in range(8):
                                ins = e.matmul(ps.ap[:, j * 128:(j + 1) * 128], hTs.ap[:, k, t * 128:(t + 1) * 128],
                                               WiB.ap[:, k, hs], start=(k == 0), stop=(k == 7))
                        return ins
                    S.op("pe", mmv, reads=[WiB] + hTs_t[g * 4:g * 4 + n], writes=[ps])

            def pB(h):
                S.op("dve", lambda e, h=h, lbi=lbi: e.tensor_scalar(
                    T1.ap[:, 0:T], T1.ap[:, 0:T], oml.ap[:, lbi, h:h + 1], lbv.ap[:, lbi, h:h + 1], ALU.mult, ALU.add),
                    reads=[T1, oml, lbv], writes=[T1])
                S.op("act", lambda e: e.activation(T2.ap[:, 0:T], T1.ap[:, 0:T], AF.Ln), reads=[T1], writes=[T2])
                S.op("act", lambda e: e.activation(T1.ap[:, 0:T], T1.ap[:, 0:T], AF.Identity, bias=1.0, scale=-1.0),
                     reads=[T1], writes=[T1])
                S.op("dve", lambda e: e.tensor_tensor_scan(T3.ap[:, 0:T], ones_bf.ap[:, 0:T], T2.ap[:, 0:T], 0.0, ALU.mult, ALU.add),
                     reads=[ones_bf, T2], writes=[T3])
                S.op("act", lambda e: e.activation(T4.ap[:, 0:T], T3.ap[:, 0:T], AF.Exp, bias=T3.ap[:, T - 1:T], scale=-1.0),
                     reads=[T3], writes=[T4])
                S.op("act", lambda e: e.activation(Dcol.ap[:], T3.ap[:, T - 1:T], AF.Exp), reads=[T3], writes=[Dcol])
                S.op("dve", lambda e: e.tensor_tensor(kd.ap[:, 0:T], T1.ap[:, 0:T], T4.ap[:, 0:T], ALU.mult),
                     reads=[T1, T4], writes=[kd])

            def pC1(h):
                vps = vctx.pop(h)
                for (ps, g, n) in vps:
                    S.op("dve", lambda e, ps=ps, g=g, n=n: e.tensor_copy(
                        v_tm.ap[:, g * 4:g * 4 + n, :], ps.ap[:, 0:n * 128].rearrange("p (a b) -> p a b", b=128)),
                        reads=[ps], writes=[v_tm])
                    PS.release(ps)

            def pC2(h):
                for g in range((nt + 7) // 8):
                    n = min(8, nt - g * 8)
                    ps = PS.get()
                    def trk(e, ps=ps, g=g, n=n):
                        for j in range(n):
                            t = g * 8 + j
                            ins = e.transpose(ps.bf[:, j * 128:(j + 1) * 128], kd.ap[:, t * 128:(t + 1) * 128], ident_bf.ap[:])
                        return ins
                    S.op("pe", trk, reads=[kd, ident_bf], writes=[ps])
                    S.op("act", lambda e, ps=ps, g=g, n=n: e.activation(
                        kd_tm.ap[:, g * 8:g * 8 + n, :], ps.bf[:, 0:n * 128].rearrange("p (a b) -> p a b", b=128), AF.Copy),
                        reads=[ps], writes=[kd_tm])
                psU = PS.get()
                def mmu(e, psU=psU, nt=nt):
                    for t in range(nt):
                        ins = e.matmul(psU.ap[:, 0:128], kd_tm.ap[:, t, :], v_tm.ap[:, t, :], start=(t == 0), stop=(t == nt - 1))
                    return ins
                S.op("pe", mmu, reads=[kd_tm, v_tm], writes=[psU])
                if mode == "f":
                    S.op("dve", lambda e, psU=psU, h=h: e.tensor_copy(Sf0.ap[:, h, :], psU.ap[:, 0:128]), reads=[psU], writes=[Sf0])
                elif mode == "b":
                    S.op("dve", lambda e, psU=psU, h=h: e.tensor_copy(Sb0.ap[:, h, :], psU.ap[:, 0:128]), reads=[psU], writes=[Sb0])
                else:
                    for (SX, fc) in ((Sf0, mode), (Sb0, 3 + mode)):
                        S.op("dve", lambda e, SX=SX, psU=psU, h=h: e.scalar_tensor_tensor(
                            tmpS.ap[:], SX.ap[:, h, :], Dcol.ap[:, 0:1], psU.ap[:, 0:128], ALU.mult, ALU.add),
                            reads=[SX, Dcol, psU], writes=[tmpS])
                        S.op("dve", lambda e, SX=SX, h=h: e.tensor_tensor(tmpD.ap[:], tmpS.ap[:], SX.ap[:, h, :], ALU.subtract),
                             reads=[tmpS, SX], writes=[tmpD])
                        S.op("dve", lambda e, SX=SX, h=h, fc=fc: e.scalar_tensor_tensor(
                            SX.ap[:, h, :], tmpD.ap[:], flg.ap[:, fc:fc + 1], SX.ap[:, h, :], ALU.mult, ALU.add),
                            reads=[tmpD, flg, SX], writes=[SX])
            pA1(0)
            pA2(0)
            pB(0)
            for h in range(1, 8):
                pA1(h)
                pC1(h - 1)
                pA2(h)
                pC2(h - 1)
                pB(h)
            pC1(7)
            pC2(7)
        for sl in slots:
            slot_body(*sl)

    if stage == 2:
        o_sf = dout("o_sf", [128, 1024])
        o_sb = dout("o_sb", [128, 1024])
        S.dma("sp", o_sf, Sf0.ap[:].rearrange("p a b -> p (a b)"), reads=[Sf0], writes=[S.tr("o1")])
        S.dma("sp", o_sb, Sb0.ap[:].rearrange("p a b -> p (a b)"), reads=[Sb0], writes=[S.tr("o2")])
        S.finish()
        return nc

    y_hg_d = dscr("y_hg_d", [8, 128, NOWN], BF16)
    y_hg_t = [S.tr("yhg%d" % i) for i in range(8)]
    w_in_v = w_in.rearrange("(k p) n -> p k n", p=128)

    def ln_main(src, T, tile0):
        ln_tokens(S, PS, ident_bf, src, T,
                  lambda k, t: hT.ap[:, k, (tile0 + t) * 128:(tile0 + t + 1) * 128], hT_t[tile0:tile0 + T // 128],
                  lambda k: cols.ap[:, 0, k:k + 1], lambda k: cols.ap[:, 1, k:k + 1], cols, xts, work)
    ln_main(x_own, 2048, 2)
    ln_main(x_halo[0:256, :], 256, 0)
    ln_main(x_halo[256:512, :], 256, 18)

    with S.scope():
        STG = Stage(S, 4, 1024, "p3")
        W5s = [S.sb("W5_%d" % i, [128, 8, 5, 128], BF16) for i in range(2)]
        sq = S.sb("sq", [128, NOWN], BF16)
        sog = S.sb("sog", [128, NOWN], BF16)
        G1 = S.sb("G1", [128, 1024], F32)
        G2 = S.sb("G2", [128, 1024], F32)
        G3 = S.sb("G3", [128, 1024], F32)
        G4 = S.sb("G4", [128, 1024], F32)
        qa = [S.sb("qa%d" % d_, [128, NOWN], BF16) for d_ in range(2)]
        kb = [S.sb("kb%d" % d_, [128, NOWN], BF16) for d_ in range(2)]
        kdT = S.sb("kdT", [128, NOWN], BF16)
        kdm = [S.sb("kdm%d" % d_, [64, 32, 128], BF16) for d_ in range(2)]
        dec = [S.sb("dec%d" % d_, [128, 32], F32) for d_ in range(2)]
        Sbf = [S.sb("Sbf%d" % d_, [128, 32, 128], BF16) for d_ in range(2)]
        vtm = S.sb("vtm", [64, 32, 128], BF16)
        scm = [S.sb("scm%d" % i, [64, 512], BF16) for i in range(2)]
        osq = S.sb("osq", [128, 512], BF16)
        lnv = S.sb("lnv", [128, 512], F32)
        rs = S.sb("rs", [128, 512], F32)
        t1 = S.sb("t1", [128, 512], F32)
        yh = kdT
        mask8 = cb.ap[0:64, 256:768]

        def load_w5(h):
            for g in range(5):
                STG.load_cast(w_in_v[:, :, g * 1024 + h * 128:g * 1024 + (h + 1) * 128], 1024, W5s[h % 2].ap[:, :, g, :], W5s[h % 2])

        def hg_head(h):
            W5 = W5s[h % 2]
            if h == 0:
                load_w5(0)

            def proj_fm(g, tok0, n, ps):
                def f(e):
                    for k in range(8):
                        ins = e.matmul(ps.ap[:, 0:n], W5.ap[:, k, g, :], hT.ap[:, k, 256 + tok0:256 + tok0 + n],
                                       start=(k == 0), stop=(k == 7))
                    return ins
                S.op("pe", f, reads=[W5] + hT_t[2 + tok0 // 128:2 + (tok0 + n) // 128], writes=[ps])
            for (g, dst) in ((0, sq), (4, sog)):
                for tb in range(4):
                    ps = PS.get()
                    proj_fm(g, tb * 512, 512, ps)
                    S.op("act", lambda e, ps=ps, tb=tb, dst=dst: e.activation(dst.ap[:, tb * 512:(tb + 1) * 512], ps.ap[:], AF.Silu),
                         reads=[ps], writes=[dst])
            for g8 in range(8):
                ps = PS.get()

                def mmv(e, ps=ps, g8=g8):
                    for j in range(4):
                        c = g8 * 4 + j
                        for k in range(8):
                            ins = e.matmul(ps.ap[0:64, j * 128:(j + 1) * 128], hT.ap[:, k, 256 + c * 64:256 + (c + 1) * 64],
                                           W5.ap[:, k, 3, :], start=(k == 0), stop=(k == 7))
                    return ins
                S.op("pe", mmv, reads=[W5] + hT_t[2 + g8 * 2:2 + g8 * 2 + 2], writes=[ps])
                S.op("dve", lambda e, ps=ps, g8=g8: e.tensor_copy(
                    vtm.ap[:, g8 * 4:g8 * 4 + 4, :], ps.ap[0:64, :].rearrange("p (a b) -> p a b", b=128)), reads=[ps], writes=[vtm])

            def gate_ops(d_, hf):
                ts_ = slice(hf * 1024, (hf + 1) * 1024)
                ops = []

                def add(*a_, **k_):
                    ops.append(lambda: S.op(*a_, **k_))
                for tb in range(2):
                    def fpro(tb=tb):
                        ps = PS.get()
                        proj_fm(1 + d_, hf * 1024 + tb * 512, 512, ps)
                        S.op("act", lambda e: e.activation(G1.ap[:, tb * 512:(tb + 1) * 512], ps.ap[:], AF.Sigmoid),
                             reads=[ps], writes=[G1])
                    ops.append(fpro)
                add("dve", lambda e: e.tensor_scalar(G1.ap[:], G1.ap[:], oml.ap[:, d_, h:h + 1], lbv.ap[:, d_, h:h + 1], ALU.mult, ALU.add),
                     reads=[G1, oml, lbv], writes=[G1])
                add("act", lambda e: e.activation(G2.ap[:], G1.ap[:], AF.Ln), reads=[G1], writes=[G2])
                add("act", lambda e: e.activation(G1.ap[:], G1.ap[:], AF.Identity, bias=1.0, scale=-1.0), reads=[G1], writes=[G1])
                add("dve", lambda e: e.tensor_tensor_scan(G3.ap[:], rmask.ap[:, 0:1024], G2.ap[:], 0.0, ALU.mult, ALU.add),
                     reads=[rmask, G2], writes=[G3])
                g3v = G3.ap[:].rearrange("p (c t) -> p c t", t=64)
                g4v = G4.ap[:].rearrange("p (c t) -> p c t", t=64)
                if d_ == 1:
                    add("dve", lambda e: e.tensor_tensor(g4v, g3v[:, :, 63:64].broadcast_to([128, 16, 64]), g3v, ALU.subtract),
                         reads=[G3], writes=[G4])
                    add("dve", lambda e: e.tensor_tensor(G3.ap[:], G4.ap[:], G2.ap[:], ALU.add), reads=[G4, G2], writes=[G3])
                add("act", lambda e: e.activation(G4.ap[:], G3.ap[:], AF.Exp), reads=[G3], writes=[G4])
                add("act", lambda e: e.activation(G2.ap[:], G3.ap[:], AF.Exp, scale=-1.0), reads=[G3], writes=[G2])
                add("dve", lambda e: e.tensor_tensor(qa[d_].ap[:, ts_], sq.ap[:, ts_], G4.ap[:], ALU.mult),
                     reads=[sq, G4], writes=[qa[d_]])
                add("dve", lambda e: e.tensor_tensor(kb[d_].ap[:, ts_], G1.ap[:], G2.ap[:], ALU.mult),
                     reads=[G1, G2], writes=[kb[d_]])
                ecol = 63 if d_ == 0 else 0
                add("act", lambda e: e.activation(dec[d_].ap[:, hf * 16:(hf + 1) * 16], g4v[:, :, ecol], AF.Copy),
                     reads=[G4], writes=[dec[d_]])
                add("dve", lambda e: e.tensor_tensor(
                    kdT.ap[:, ts_].rearrange("p (c t) -> p c t", t=64), kb[d_].ap[:, ts_].rearrange("p (c t) -> p c t", t=64),
                    g4v[:, :, ecol:ecol + 1].broadcast_to([128, 16, 64]), ALU.mult),
                    reads=[kb[d_], G4], writes=[kdT])

                return ops

            def rest_ops(d_):
                ops = []
                for g in range(4):
                    def trg(g=g):
                        ps = PS.get()

                        def trk(e):
                            for j in range(8):
                                c = g * 8 + j
                                ins = e.transpose(ps.bf[0:64, j * 128:(j + 1) * 128], kdT.ap[:, c * 64:(c + 1) * 64], ident_bf.ap[:])
                            return ins
                        S.op("pe", trk, reads=[kdT, ident_bf], writes=[ps])
                        S.op("act", lambda e: e.activation(
                            kdm[d_].ap[:, g * 8:(g + 1) * 8, :], ps.bf[0:64, :].rearrange("p (a b) -> p a b", b=128), AF.Copy),
                            reads=[ps], writes=[kdm[d_]])
                    ops.append(trg)
                if d_ == 0:
                    ops.append(lambda: S.op("act", lambda e: e.activation(Sbf[0].ap[:, 0, :], Sf0.ap[:, h, :], AF.Copy), reads=[Sf0], writes=[Sbf[0]]))
                    chunks = list(range(0, 31))
                else:
                    ops.append(lambda: S.op("act", lambda e: e.activation(Sbf[1].ap[:, 31, :], Sb0.ap[:, h, :], AF.Copy), reads=[Sb0], writes=[Sbf[1]]))
                    chunks = list(range(31, 0, -1))
                for g0 in range(0, len(chunks), 4):
                    def ug(grp=chunks[g0:g0 + 4]):
                        ps = PS.get()

                        def mmu(e):
                            for j, c in enumerate(grp):
                                ins = e.matmul(ps.ap[:, j * 128:(j + 1) * 128], kdm[d_].ap[:, c, :], vtm.ap[:, c, :], start=True, stop=True)
                            return ins
                        S.op("pe", mmu, reads=[kdm[d_], vtm], writes=[ps])
                        for j, c in enumerate(grp):
                            cn = c + 1 if d_ == 0 else c - 1
                            S.op("dve", lambda e, j=j, c=c, cn=cn: e.scalar_tensor_tensor(
                                Sbf[d_].ap[:, cn, :], Sbf[d_].ap[:, c, :], dec[d_].ap[:, c:c + 1], ps.ap[:, j * 128:(j + 1) * 128],
                                ALU.mult, ALU.add), reads=[Sbf[d_], dec[d_], ps], writes=[Sbf[d_]])
                    ops.append(ug)
                return ops

            for op_ in gate_ops(0, 0) + gate_ops(0, 1):
                op_()
            interleave([rest_ops(0), gate_ops(1, 0) + gate_ops(1, 1)])
            for op_ in rest_ops(1):
                op_()

            if h + 1 < 8:
                load_w5(h + 1)
            psos = {}

            def stX(tb):
                pso = PS.reserve()
                psos[tb] = pso
                pssl = []
                for g2 in range(2):
                    c0 = tb * 8 + g2 * 4
                    pss = PS.reserve()
                    pssl.append(pss)

                    def mms(e, pss=pss, c0=c0):
                        for j in range(4):
                            csl = slice((c0 + j) * 64, (c0 + j + 1) * 64)
                            for dd in range(2):
                                o_ = (j * 2 + dd) * 64
                                ins = e.matmul(pss.ap[0:64, o_:o_ + 64], kb[dd].ap[:, csl], qa[dd].ap[:, csl], start=True, stop=True)
                        return ins
                    S.op("pe", mms, reads=[kb[0], kb[1], qa[0], qa[1]], writes=[pss])
                for g2 in range(2):
                    pss = pssl[g2]
                    sc_ = scm[g2]
                    S.op("dve", lambda e, pss=pss, sc_=sc_: e.tensor_tensor(sc_.ap[:], pss.ap[0:64, :], mask8, ALU.mult),
                         reads=[pss, cb], writes=[sc_])
                    PS.release(pss)
                for g2 in range(2):
                    c0 = tb * 8 + g2 * 4
                    sc_ = scm[g2]

                    def mmo(e, c0=c0, g2=g2, sc_=sc_):
                        for j in range(4):
                            c = c0 + j
                            o64 = slice((g2 * 4 + j) * 64, (g2 * 4 + j + 1) * 64)
                            csl = slice(c * 64, (c + 1) * 64)
                            e.matmul(pso.ap[:, o64], vtm.ap[:, c, :], sc_.ap[:, (j * 2) * 64:(j * 2 + 1) * 64], start=True, stop=False)
                            e.matmul(pso.ap[:, o64], vtm.ap[:, c, :], sc_.ap[:, (j * 2 + 1) * 64:(j * 2 + 2) * 64], start=False, stop=False)
                            e.matmul(pso.ap[:, o64], Sbf[0].ap[:, c, :], qa[0].ap[:, csl], start=False, stop=False)
                            ins = e.matmul(pso.ap[:, o64], Sbf[1].ap[:, c, :], qa[1].ap[:, csl], start=False, stop=True)
                        return ins
                    S.op("pe", mmo, reads=[vtm, sc_, Sbf[0], Sbf[1], qa[0], qa[1]], writes=[pso])

            def stY(tb):
                pso = psos.pop(tb)
                S.op("act", lambda e: e.activation(osq.ap[:], pso.ap[:], AF.Square), reads=[pso], writes=[osq])
                pq = PS.get()
                S.op("pe", lambda e: e.matmul(pq.ap[:], ones_bf.ap[:, 0:128], osq.ap[:], start=True, stop=True),
                     reads=[ones_bf, osq], writes=[pq])
                S.op("act", lambda e: e.activation(lnv.ap[:], pq.ap[:], AF.Ln, bias=EPS, scale=1.0 / 128.0), reads=[pq], writes=[lnv])
                S.op("act", lambda e: e.activation(rs.ap[:], lnv.ap[:], AF.Exp, scale=-0.5), reads=[lnv], writes=[rs])
                S.op("dve", lambda e: e.tensor_tensor(t1.ap[:], pso.ap[:], rs.ap[:], ALU.mult), reads=[pso, rs], writes=[t1])
                S.op("dve", lambda e: e.scalar_tensor_tensor(
                    yh.ap[:, tb * 512:(tb + 1) * 512], t1.ap[:], hgc.ap[:, h:h + 1], sog.ap[:, tb * 512:(tb + 1) * 512],
                    ALU.mult, ALU.mult), reads=[t1, hgc, sog], writes=[yh])
                PS.release(pso)
            for step in range(5):
                if step < 4:
                    stX(step)
                if step >= 1:
                    stY(step - 1)
            S.dma("sp", y_hg_d[h], yh.ap[:], reads=[yh], writes=[y_hg_t[h]])

        for h in range(8):
            hg_head(h)
    hgs.__exit__(None, None, None)
    if stage == 3:
        o_yhg = dout("o_yhg", [8, 128, NOWN], BF16)
        S.dma("sp", o_yhg, y_hg_d, reads=y_hg_t, writes=[S.tr("o1")])
        S.finish()
        return nc

    y_na_d = dscr("y_na_d", [8, 128, NOWN], BF16)
    y_na_t = [S.tr("yna%d" % i) for i in range(8)]
    with S.scope():
        STG = Stage(S, 4, 1024, "p4")
        hcT = S.sb("hcT", [128, 8, 256], BF16)
        hcT_t = [S.tr("hcT0"), S.tr("hcT1")]
        ln_tokens(S, PS, ident_bf, ctx2[0:256, :], 256, lambda k, t: hcT.ap[:, k, t * 128:(t + 1) * 128], hcT_t,
                  lambda k: cols.ap[:, 2, k:k + 1], lambda k: cols.ap[:, 3, k:k + 1], cols, xts, work)
        cosT = S.sb("cosT_sb", [128, NKB], F32)
        sinT = S.sb("sinT_sb", [128, NKB], F32)
        S.dma("sp", cosT.ap[:], cosT_d, writes=[cosT])
        S.dma("sp", sinT.ap[:], sinT_d, writes=[sinT])
        W3 = S.sb("W3", [128, 8, 3, 128], BF16)
        qblk = S.sb("qblk", [128, 32, 128], BF16)
        qpblk = S.sb("qpblk", [128, 32, 128], BF16)
        S.op("pool", lambda e: e.memset(qblk.ap[:], 0.0), writes=[qblk])
        S.op("pool", lambda e: e.memset(qpblk.ap[:], 0.0), writes=[qpblk])
        krT = S.sb("krT", [128, NKB], BF16)
        kcT = S.sb("kcT", [128, 256], BF16)
        pl = S.sb("pl", [128, 512], BF16)
        tqa = S.sb("tqa", [128, 512], F32)
        tqb = S.sb("tqb", [128, 512], F32)
        v_ev = S.sb("v_ev", [128, 20, 2, 65], BF16)
        v_od = S.sb("v_od", [128, 19, 2, 65], BF16)
        v_cx = S.sb("v_cx", [128, 2, 2, 65], BF16)
        for vb in (v_ev, v_od, v_cx):
            S.op("pool", lambda e, vb=vb: e.memset(vb.ap[:], 1.0), writes=[vb])
        Bi = S.sb("Bi", [128, 512], BF16)
        Bs = S.sb("Bs", [128, 7, 768], BF16)
        Pm = [S.sb("Pm%d" % i, [128, 1024], BF16) for i in range(4)]
        PT = [S.sb("PT%d" % i, [128, 8, 128], BF16) for i in range(3)]
        mxs = [S.sb("mx%d" % i, [128, 2], F32) for i in range(3)]
        nmxs = [S.sb("nmx%d" % i, [128, 1], F32) for i in range(3)]
        rcs = [S.sb("rc%d" % i, [64, 2], F32) for i in range(3)]
        ytms = [S.sb("ytm%d" % i, [64, 8, 128], BF16) for i in range(2)]
        ynaT = S.sb("ynaT", [128, NOWN], BF16)

        def na_pair(pr):
            for g in range(3):
                c0 = 5120 + g * 1024 + pr * 128
                STG.load_cast(w_in_v[:, :, c0:c0 + 128], 1024, W3.ap[:, :, g, :], W3)
            STG.load_cast(Bint_d[pr], 512, Bi.ap[:], Bi)
            for sr in range(7):
                STG.load_cast(Bspec_d[pr][:, sr * 768:(sr + 1) * 768], 768, Bs.ap[:, sr, :], Bs)

            def proj(g, src_t, src_trs, tok0, n, ps):
                def f(e):
                    for k in range(8):
                        ins = e.matmul(ps.ap[:, 0:n], W3.ap[:, k, g, :], src_t.ap[:, k, tok0:tok0 + n], start=(k == 0), stop=(k == 7))
                    return ins
                S.op("pe", f, reads=[W3] + src_trs, writes=[ps])

            def rope(ps, tok0, n, scale, out_fn, out_tr, plain_fn=None, plain_tr=None):
                S.op("act", lambda e: e.activation(pl.ap[:, 0:n], ps.ap[:, 0:n], AF.Identity, scale=scale), reads=[ps], writes=[pl])
                ps2 = PS.get()
                S.op("pe", lambda e: e.matmul(ps2.ap[:, 0:n], cb.ap[:, 128:256], pl.ap[:, 0:n], start=True, stop=True),
                     reads=[cb, pl], writes=[ps2])
                S.op("dve", lambda e: e.tensor_tensor(tqa.ap[:, 0:n], pl.ap[:, 0:n], cosT.ap[:, tok0:tok0 + n], ALU.mult),
                     reads=[pl, cosT], writes=[tqa])
                S.op("dve", lambda e: e.tensor_tensor(tqb.ap[:, 0:n], ps2.ap[:, 0:n], sinT.ap[:, tok0:tok0 + n], ALU.mult),
                     reads=[ps2, sinT], writes=[tqb])
                out_fn(tqa, tqb)
                if plain_fn is not None:
                    plain_fn(pl)

            for tb in range(4):
                ps = PS.get()
                proj(0, hT, hT_t[2 + tb * 4:2 + tb * 4 + 4], 256 + tb * 512, 512, ps)

                def qout(a_, b_, tb=tb):
                    for hh in range(2):
                        psl = slice(hh * 64, (hh + 1) * 64)
                        S.op("dve", lambda e, psl=psl: e.tensor_tensor(
                            qblk.ap[psl, tb * 8:(tb + 1) * 8, psl], a_.ap[psl, 0:512].rearrange("p (r q) -> p r q", q=64),
                            b_.ap[psl, 0:512].rearrange("p (r q) -> p r q", q=64), ALU.add), reads=[a_, b_], writes=[qblk])

                def qplain(pl_, tb=tb):
                    for hh in range(2):
                        psl = slice(hh * 64, (hh + 1) * 64)
                        S.op("act", lambda e, psl=psl: e.activation(
                            qpblk.ap[psl, tb * 8:(tb + 1) * 8, psl], pl_.ap[psl, 0:512].rearrange("p (r q) -> p r q", q=64), AF.Copy),
                            reads=[pl_], writes=[qpblk])
                rope(ps, 256 + tb * 512, 512, 0.125, qout, qblk, qplain, qpblk)
            for tb in range(5):
                ps = PS.get()
                proj(1, hT, hT_t[tb * 4:tb * 4 + 4], tb * 512, 512, ps)

                def kout(a_, b_, tb=tb):
                    S.op("dve", lambda e: e.tensor_tensor(krT.ap[:, tb * 512:(tb + 1) * 512], a_.ap[:, 0:512], b_.ap[:, 0:512], ALU.add),
                         reads=[a_, b_], writes=[krT])
                rope(ps, tb * 512, 512, 1.0, kout, krT)
            ps = PS.get()
            proj(1, hcT, hcT_t, 0, 256, ps)
            S.op("act", lambda e, ps=ps: e.activation(kcT.ap[:], ps.ap[:, 0:256], AF.Copy), reads=[ps], writes=[kcT])

            def vproj(src_t, src_trs_fn, tiles, dst):
                for g0 in range(0, len(tiles), 4):
                    grp = tiles[g0:g0 + 4]
                    ps = PS.get()
                    trs = []
                    for (ti, tok0) in grp:
                        trs += src_trs_fn(tok0)

                    def f(e, ps=ps, grp=grp):
                        for j, (ti, tok0) in enumerate(grp):
                            for k in range(8):
                                ins = e.matmul(ps.ap[:, j * 128:(j + 1) * 128], src_t.ap[:, k, tok0:tok0 + 128], W3.ap[:, k, 2, :],
                                               start=(k == 0), stop=(k == 7))
                        return ins
                    S.op("pe", f, reads=[W3] + trs, writes=[ps])
                    n = len(grp)
                    t0 = grp[0][0]
                    S.op("dve", lambda e, ps=ps, n=n, t0=t0: e.tensor_copy(
                        dst.ap[:, t0:t0 + n, :, 0:64], ps.ap[:, 0:n * 128].rearrange("p (a h d) -> p a h d", h=2, d=64)),
                        reads=[ps], writes=[dst])
            vproj(hT, lambda tok0: [hT_t[tok0 // 128]], [(i, i * 128) for i in range(20)], v_ev)
            vproj(hT, lambda tok0: [hT_t[tok0 // 128], hT_t[tok0 // 128 + 1]], [(i, 64 + i * 128) for i in range(19)], v_od)
            vproj(hcT, lambda tok0: [hcT_t[tok0 // 128]], [(i, i * 128) for i in range(2)], v_cx)

            ctx_ = {}

            def rowcfg(lr):
                if lr < 4:
                    return 0, 12, Bs.ap[:, lr, :], Bs
                if lr >= 29:
                    return 27, 12, Bs.ap[:, 4 + lr - 29, :], Bs
                return lr, 8, Bi.ap[:], Bi

            def st0(lr):
                rel0, nkr, Bap, Btr = rowcfg(lr)
                nk = nkr * 64
                psA = PS.get()
                psB = PS.get()
                k0 = rel0 * 64
                ctx_[lr] = dict(psA=psA, psB=psB, nk=nk, nB=nk - 512 + 256, rel0=rel0)

                def qk(e):
                    e.matmul(psA.ap[:, 0:512], qblk.ap[:, lr, :], krT.ap[:, k0:k0 + 512], start=True, stop=False)
                    e.matmul(psA.ap[:, 0:512], ident_bf.ap[:], Bap[:, 0:512], start=False, stop=True)
                    o = 0
                    if nk > 512:
                        e.matmul(psB.ap[:, 0:256], qblk.ap[:, lr, :], krT.ap[:, k0 + 512:k0 + 768], start=True, stop=False)
                        e.matmul(psB.ap[:, 0:256], ident_bf.ap[:], Bap[:, 512:768], start=False, stop=True)
                        o = 256
                    return e.matmul(psB.ap[:, o:o + 256], qpblk.ap[:, lr, :], kcT.ap[:], start=True, stop=True)
                S.op("pe", qk, reads=[qblk, qpblk, krT, kcT, Btr, ident_bf], writes=[psA, psB])

            def st1(lr):
                c = ctx_[lr]
                psA, psB, nB = c["psA"], c["psB"], c["nB"]
                mx_ = mxs[lr % 3]
                nm_ = nmxs[lr % 3]
                P_ = Pm[lr % 4]
                S.op("dve", lambda e: e.tensor_reduce(mx_.ap[:, 0:1], psA.ap[:, 0:512], AX.X, ALU.max), reads=[psA], writes=[mx_])
                S.op("dve", lambda e: e.tensor_reduce(mx_.ap[:, 1:2], psB.ap[:, 0:nB], AX.X, ALU.max), reads=[psB], writes=[mx_])
                S.op("dve", lambda e: e.tensor_scalar(nm_.ap[:], mx_.ap[:, 0:1], mx_.ap[:, 1:2], -1.0, ALU.max, ALU.mult),
                     reads=[mx_], writes=[nm_])
                S.op("act", lambda e: e.activation(P_.ap[:, 0:512], psA.ap[:, 0:512], AF.Exp, bias=nm_.ap[:], scale=1.0),
                     reads=[psA, nm_], writes=[P_])
                S.op("act", lambda e: e.activation(P_.ap[:, 512:512 + nB], psB.ap[:, 0:nB], AF.Exp, bias=nm_.ap[:], scale=1.0),
                     reads=[psB, nm_], writes=[P_])

            def st2(lr):
                c = ctx_[lr]
                nkt = (512 + c["nB"]) // 128
                P_ = Pm[lr % 4]
                PT_ = PT[lr % 3]
                pst = PS.get()

                def trp(e):
                    for jt in range(nkt):
                        ins = e.transpose(pst.bf[:, jt * 128:(jt + 1) * 128], P_.ap[:, jt * 128:(jt + 1) * 128], ident_bf.ap[:])
                    return ins
                S.op("pe", trp, reads=[P_, ident_bf], writes=[pst])
                S.op("dve", lambda e: e.tensor_copy(
                    PT_.ap[:, 0:nkt, :], pst.bf[:, 0:nkt * 128].rearrange("p (a b) -> p a b", b=128)), reads=[pst], writes=[PT_])

            def st3(lr):
                c = ctx_.pop(lr)
                rel0 = c["rel0"]
                nwt = c["nk"] // 128
                PT_ = PT[lr % 3]
                pso = PS.get()
                if rel0 % 2 == 0:
                    vt, vi0 = v_ev, rel0 // 2
                else:
                    vt, vi0 = v_od, (rel0 - 1) // 2

                def pv(e):
                    for hh in range(2):
                        for jt in range(nwt + 2):
                            if jt < nwt:
                                rhs = vt.ap[:, vi0 + jt, hh, :]
                            else:
                                rhs = v_cx.ap[:, jt - nwt, hh, :]
                            ins = e.matmul(pso.ap[0:64, hh * 65:(hh + 1) * 65], PT_.ap[:, jt, hh * 64:(hh + 1) * 64], rhs,
                                           start=(jt == 0), stop=(jt == nwt + 1))
                    return ins
                S.op("pe", pv, reads=[PT_, v_ev, v_od, v_cx], writes=[pso])
                pv3 = pso.ap[0:64, 0:130].rearrange("p (h d) -> p h d", d=65)
                rc_ = rcs[lr % 3]
                yt_ = ytms[(lr // 8) % 2]
                rr = lr % 8
                S.op("dve", lambda e: e.reciprocal(rc_.ap[:], pv3[:, :, 64]), reads=[pso], writes=[rc_])
                S.op("dve", lambda e: e.tensor_tensor(
                    yt_.ap[:, rr, :].rearrange("p (h d) -> p h d", d=64), pv3[:, :, 0:64],
                    rc_.ap[:].rearrange("p (h o) -> p h o", o=1).broadcast_to([64, 2, 64]), ALU.mult),
                    reads=[pso, rc_], writes=[yt_])
                if rr == 7:
                    r8 = lr // 8
                    psy = PS.get()

                    def try_(e):
                        for q_ in range(8):
                            ins = e.transpose(psy.bf[:, q_ * 64:(q_ + 1) * 64], yt_.ap[:, q_, :], ident_bf.ap[0:64, 0:64])
                        return ins
                    S.op("pe", try_, reads=[yt_, ident_bf], writes=[psy])
                    S.op("act", lambda e: e.activation(ynaT.ap[:, r8 * 512:(r8 + 1) * 512], psy.bf[:, 0:512], AF.Copy),
                         reads=[psy], writes=[ynaT])
            for step in range(32 + 4):
                if 0 <= step - 1 < 32:
                    st1(step - 1)
                if 0 <= step - 4 < 32:
                    st3(step - 4)
                if 0 <= step - 3 < 32:
                    st2(step - 3)
                if step < 32:
                    st0(step)
            S.dma("sp", y_na_d[pr], ynaT.ap[:], reads=[ynaT], writes=[y_na_t[pr]])

        for pr in range(8):
            na_pair(pr)
    if stage == 4:
        o_yna = dout("o_yna", [8, 128, NOWN], BF16)
        S.dma("sp", o_yna, y_na_d, reads=y_na_t, writes=[S.tr("o1")])
        S.finish()
        return nc

    x1_t = [S.tr("x1_%d" % i) for i in range(16)]

    def bcast_load(dst_ap, src_row_ap, tr):
        S.dma("sp", dst_ap, src_row_ap.partition_broadcast(128), writes=[tr])

    def ln_stats(src_buf, st, mv, rstd, nmr):
        for i in range(2):
            S.op("dve", lambda e, i=i: e.bn_stats(st.ap[:, i, :], src_buf.ap[:, i * 512:(i + 1) * 512]), reads=[src_buf], writes=[st])
        S.op("dve", lambda e: e.bn_aggr(mv.ap[:], st.ap[:].rearrange("p a b -> p (a b)")), reads=[st], writes=[mv])
        S.op("act", lambda e: e.activation(rstd.ap[:], mv.ap[:, 1:2], AF.Sqrt, bias=EPS, scale=1.0), reads=[mv], writes=[rstd])
        S.op("dve", lambda e: e.reciprocal(rstd.ap[:], rstd.ap[:]), reads=[rstd], writes=[rstd])
        S.op("dve", lambda e: e.scalar_tensor_tensor(nmr.ap[:], mv.ap[:, 0:1], -1.0, rstd.ap[:], ALU.mult, ALU.mult),
             reads=[mv, rstd], writes=[nmr])
    st_, mv_, rstd_, nmr_, _xb = work[0]
    gtm = S.sb("gtm", [128, 16, 65], F32)
    gtm_t = [S.tr("gtm%d" % i) for i in range(16)]
    S.op("pool", lambda e: e.memset(gtm.ap[:], 1.0), writes=gtm_t)
    with S.scope():
        STG = Stage(S, 4, 1024, "p5")
        bc = S.sb("bc5", [128, 3, D], F32)
        bc_t = S.tr("bc5t")
        S.dma("sp", bc.ap[:, 0, :], mod_d[0:1, 2048:3072].partition_broadcast(128), reads=[], writes=[bc_t])
        S.dma("sp", bc.ap[:, 1, :], lnp[0:1, :].partition_broadcast(128), writes=[bc_t])
        S.dma("sp", bc.ap[:, 2, :], lnp[1:2, :].partition_broadcast(128), writes=[bc_t])
        rb = S.sb("rb", [128, 64], F32)
        S.dma("sp", rb.ap[:], rbias.partition_broadcast(128), writes=[rb])
        Wr = S.sb("Wr", [128, 8, 64], F32)
        S.dma("sp", Wr.ap[:], w_r.rearrange("(k p) n -> p k n", p=128), writes=[Wr])
        WoB = S.sb("WoB", [128, 8, D], BF16)
        w_o_v = w_o.rearrange("(k p) n -> p k n", p=128)
        for q8 in range(8):
            STG.load_cast(w_o_v[:, :, q8 * 128:(q8 + 1) * 128], 1024, WoB.ap[:, :, q8 * 128:(q8 + 1) * 128], WoB)
        yhT = S.sb("yhT", [128, 8, 512], BF16)
        ynT = S.sb("ynT", [128, 8, 512], BF16)
        mT = S.sb("mT", [128, 8, 512], BF16)
        Wms = [S.sb("Wm%d" % i, [128, 8, 4, 128], BF16) for i in range(2)]
        sga = S.sb("sga", [128, 512], F32)
        sgb = S.sb("sgb", [128, 512], F32)
        tma = S.sb("tma", [128, 512], F32)
        tmb = S.sb("tmb", [128, 512], F32)
        Ab = [S.sb("Ab%d" % i, [128, D], F32) for i in range(2)]
        Bb = [S.sb("Bb%d" % i, [128, D], F32) for i in range(2)]
        h2fs = [S.sb("h2f%d" % i, [128, 8, 128], F32) for i in range(2)]
        rts = [(S.sb("scr%d" % i, [128, 64], F32), S.sb("sel%d" % i, [128, 64], F32), S.sb("selm%d" % i, [128, 64], F32),
                S.sb("m8_%d" % i, [128, 8, 8], F32), S.sb("gs%d" % i, [128, 8], F32), S.sb("gm8_%d" % i, [128, 8], F32),
                S.sb("pen%d" % i, [128, 8], F32), S.sb("e8_%d" % i, [128, 8], F32), S.sb("wsel%d" % i, [128, 64], F32),
                S.sb("wsum%d" % i, [128, 1], F32), S.sb("gts%d" % i, [128, 64], F32)) for i in range(2)]
        w_a_v = w_a.rearrange("(k p) n -> p k n", p=128)
        w_b_v = w_b.rearrange("(k p) n -> p k n", p=128)
        BIG = 1.0e9

        def quarter(qt):
            tok0 = qt * 512
            S.dma("sp", yhT.ap[:], y_hg_d[:, :, tok0:tok0 + 512].rearrange("h p t -> p h t"), reads=y_hg_t, writes=[yhT])
            S.dma("sp", ynT.ap[:], y_na_d[:, :, tok0:tok0 + 512].rearrange("h p t -> p h t"), reads=y_na_t, writes=[ynT])
            for c in range(8):
                Wm = Wms[c % 2]
                for g, srcv in enumerate((w_in_v[:, :, 8192 + c * 128:8192 + (c + 1) * 128], w_in_v[:, :, 9216 + c * 128:9216 + (c + 1) * 128],
                                          w_a_v[:, :, c * 128:(c + 1) * 128], w_b_v[:, :, c * 128:(c + 1) * 128])):
                    STG.load_cast(srcv, 1024, Wm.ap[:, :, g, :], Wm)
                pss = [PS.get() for _ in range(4)]

                def mm4(e, pss=pss, Wm=Wm):
                    for g in range(4):
                        for k in range(8):
                            if g < 2:
                                rhs = hT.ap[:, k, 256 + tok0:256 + tok0 + 512]
                            else:
                                rhs = (yhT if g == 2 else ynT).ap[:, k, :]
                            ins = e.matmul(pss[g].ap[:], Wm.ap[:, k, g, :], rhs, start=(k == 0), stop=(k == 7))
                    return ins
                S.op("pe", mm4, reads=[Wm, yhT, ynT] + hT_t[2 + qt * 4:2 + qt * 4 + 4], writes=pss)
                S.op("act", lambda e, pss=pss: e.activation(sga.ap[:], pss[0].ap[:], AF.Sigmoid), reads=[pss[0]], writes=[sga])
                S.op("act", lambda e, pss=pss: e.activation(sgb.ap[:], pss[1].ap[:], AF.Sigmoid), reads=[pss[1]], writes=[sgb])
                S.op("dve", lambda e, pss=pss: e.tensor_tensor(tma.ap[:], sga.ap[:], pss[2].ap[:], ALU.mult), reads=[sga, pss[2]], writes=[tma])
                S.op("dve", lambda e, pss=pss: e.tensor_tensor(tmb.ap[:], sgb.ap[:], pss[3].ap[:], ALU.mult), reads=[sgb, pss[3]], writes=[tmb])
                S.op("dve", lambda e, c=c: e.tensor_tensor(mT.ap[:, c, :], tma.ap[:], tmb.ap[:], ALU.add), reads=[tma, tmb], writes=[mT])
            def tile_ops(t4):
                tile_ = qt * 4 + t4
                bs = tile_ % 2
                xt = xts[bs]
                A_ = Ab[bs]
                B_ = Bb[bs]
                h2f = h2fs[bs]
                st_, mv_, rstd_, nmr_, _x = work[bs]
                scr, sel, selm, m8, gs, gm8, pen, e8, wsel, wsum, gts = rts[bs]
                c = {}
                ops = []
                ops.append(lambda: S.dma("sp", xt.ap[:], x_own[tile_ * 128:(tile_ + 1) * 128, :], writes=[xt]))

                def o_mmy():
                    c["psy"] = [PS.reserve(), PS.reserve()]
                    psy = c["psy"]

                    def mmy(e):
                        for hh in range(2):
                            for k in range(8):
                                ins = e.matmul(psy[hh].ap[:], mT.ap[:, k, t4 * 128:(t4 + 1) * 128], WoB.ap[:, k, hh * 512:(hh + 1) * 512],
                                               start=(k == 0), stop=(k == 7))
                        return ins
                    S.op("pe", mmy, reads=[mT, WoB], writes=psy)
                ops.append(o_mmy)
                for hh in range(2):
                    def o_g1(hh=hh):
                        psy = c["psy"]
                        S.op("dve", lambda e: e.tensor_tensor(
                            A_.ap[:, hh * 512:(hh + 1) * 512], psy[hh].ap[:], bc.ap[:, 0, hh * 512:(hh + 1) * 512], ALU.mult),
                            reads=[psy[hh], bc_t], writes=[A_])
                        PS.release(psy[hh])
                    ops.append(o_g1)
                ops.append(lambda: S.op("dve", lambda e: e.scalar_tensor_tensor(A_.ap[:], xt.ap[:], ALPHA, A_.ap[:], ALU.mult, ALU.add),
                                        reads=[xt, A_], writes=[A_]))

                def stats_ops(buf):
                    for i in range(2):
                        ops.append(lambda i=i: S.op("dve", lambda e: e.bn_stats(st_.ap[:, i, :], buf.ap[:, i * 512:(i + 1) * 512]),
                                                    reads=[buf], writes=[st_]))
                    ops.append(lambda: S.op("dve", lambda e: e.bn_aggr(mv_.ap[:], st_.ap[:].rearrange("p a b -> p (a b)")), reads=[st_], writes=[mv_]))
                    ops.append(lambda: S.op("act", lambda e: e.activation(rstd_.ap[:], mv_.ap[:, 1:2], AF.Sqrt, bias=EPS, scale=1.0),
                                            reads=[mv_], writes=[rstd_]))
                    ops.append(lambda: S.op("dve", lambda e: e.reciprocal(rstd_.ap[:], rstd_.ap[:]), reads=[rstd_], writes=[rstd_]))
                    ops.append(lambda: S.op("dve", lambda e: e.scalar_tensor_tensor(nmr_.ap[:], mv_.ap[:, 0:1], -1.0, rstd_.ap[:], ALU.mult, ALU.mult),
                                            reads=[mv_, rstd_], writes=[nmr_]))
                stats_ops(A_)
                ops.append(lambda: S.op("act", lambda e: e.activation(A_.ap[:], A_.ap[:], AF.Identity, bias=nmr_.ap[:], scale=rstd_.ap[:]),
                                        reads=[A_, nmr_, rstd_], writes=[A_]))
                ops.append(lambda: S.op("dve", lambda e: e.tensor_tensor(A_.ap[:], A_.ap[:], bc.ap[:, 1, :], ALU.mult), reads=[A_, bc_t], writes=[A_]))
                ops.append(lambda: S.op("dve", lambda e: e.tensor_tensor(A_.ap[:], A_.ap[:], bc.ap[:, 2, :], ALU.add), reads=[A_, bc_t], writes=[A_]))
                ops.append(lambda: S.dma("sp", x1_d[tile_ * 128:(tile_ + 1) * 128, :], A_.ap[:], reads=[A_], writes=[x1_t[tile_]]))
                stats_ops(A_)
                ops.append(lambda: S.op("act", lambda e: e.activation(B_.ap[:], A_.ap[:], AF.Identity, bias=nmr_.ap[:], scale=rstd_.ap[:]),
                                        reads=[A_, nmr_, rstd_], writes=[B_]))

                def o_trf():
                    c["pst"] = [PS.reserve(), PS.reserve()]
                    pst = c["pst"]

                    def trf(e):
                        for k in range(8):
                            ins = e.transpose(pst[k // 4].ap[:, (k % 4) * 128:(k % 4 + 1) * 128], B_.ap[:, k * 128:(k + 1) * 128], ident_f.ap[:])
                        return ins
                    S.op("pe", trf, reads=[B_, ident_f], writes=pst)
                ops.append(o_trf)
                for k in range(8):
                    def o_ev(k=k):
                        pst = c["pst"]
                        src_ap = pst[k // 4].ap[:, (k % 4) * 128:(k % 4 + 1) * 128]
                        if k % 2 == 0:
                            S.op("act", lambda e: e.activation(
                                h2f.ap[:, k, :], src_ap, AF.Identity, bias=cols.ap[:, 5, k:k + 1], scale=cols.ap[:, 4, k:k + 1]),
                                reads=[pst[k // 4], cols], writes=[h2f])
                        else:
                            S.op("dve", lambda e: e.tensor_scalar(
                                h2f.ap[:, k, :], src_ap, cols.ap[:, 4, k:k + 1], cols.ap[:, 5, k:k + 1], ALU.mult, ALU.add),
                                reads=[pst[k // 4], cols], writes=[h2f])
                        if k == 3:
                            PS.release(pst[0])
                        if k == 7:
                            PS.release(pst[1])
                    ops.append(o_ev)
                ops.append(lambda: S.op("act", lambda e: e.activation(hT.ap[:, :, 256 + tile_ * 128:256 + (tile_ + 1) * 128], h2f.ap[:], AF.Copy),
                                        reads=[h2f], writes=[hT_t[2 + tile_]]))

                def o_mml():
                    c["psl"] = PS.reserve()
                    psl = c["psl"]

                    def mml(e):
                        for k in range(8):
                            ins = e.matmul(psl.ap[:, 0:64], h2f.ap[:, k, :], Wr.ap[:, k, :], start=(k == 0), stop=(k == 7))
                        return ins
                    S.op("pe", mml, reads=[h2f, Wr], writes=[psl])
                ops.append(o_mml)

                def o_sig():
                    psl = c["psl"]
                    S.op("act", lambda e: e.activation(scr.ap[:], psl.ap[:, 0:64], AF.Sigmoid), reads=[psl], writes=[scr])
                    PS.release(psl)
                ops.append(o_sig)
                ops.append(lambda: S.op("dve", lambda e: e.tensor_tensor(sel.ap[:], scr.ap[:], rb.ap[:], ALU.add), reads=[scr, rb], writes=[sel]))
                for g in range(8):
                    ops.append(lambda g=g: S.op("dve", lambda e: e.max(m8.ap[:, g, :], sel.ap[:, g * 8:(g + 1) * 8]), reads=[sel], writes=[m8]))
                ops.append(lambda: S.op("dve", lambda e: e.tensor_tensor(gs.ap[:], m8.ap[:, :, 0], m8.ap[:, :, 1], ALU.add), reads=[m8], writes=[gs]))
                ops.append(lambda: S.op("dve", lambda e: e.max(gm8.ap[:], gs.ap[:]), reads=[gs], writes=[gm8]))
                ops.append(lambda: S.op("dve", lambda e: e.tensor_scalar(pen.ap[:], gs.ap[:], gm8.ap[:, 3:4], None, ALU.is_ge), reads=[gs, gm8], writes=[pen]))
                ops.append(lambda: S.op("dve", lambda e: e.tensor_scalar(pen.ap[:], pen.ap[:], BIG, -BIG, ALU.mult, ALU.add), reads=[pen], writes=[pen]))
                ops.append(lambda: S.op("dve", lambda e: e.tensor_tensor(
                    selm.ap[:].rearrange("p (g j) -> p g j", j=8), sel.ap[:].rearrange("p (g j) -> p g j", j=8),
                    pen.ap[:].rearrange("p (g o) -> p g o", o=1).broadcast_to([128, 8, 8]), ALU.add), reads=[sel, pen], writes=[selm]))
                ops.append(lambda: S.op("dve", lambda e: e.max(e8.ap[:], selm.ap[:]), reads=[selm], writes=[e8]))
                ops.append(lambda: S.op("dve", lambda e: e.tensor_scalar(wsel.ap[:], selm.ap[:], e8.ap[:, 7:8], None, ALU.is_ge), reads=[selm, e8], writes=[wsel]))
                ops.append(lambda: S.op("dve", lambda e: e.tensor_tensor(wsel.ap[:], wsel.ap[:], scr.ap[:], ALU.mult), reads=[wsel, scr], writes=[wsel]))
                ops.append(lambda: S.op("dve", lambda e: e.tensor_reduce(wsum.ap[:], wsel.ap[:], AX.X, ALU.add), reads=[wsel], writes=[wsum]))
                ops.append(lambda: S.op("dve", lambda e: e.reciprocal(wsum.ap[:], wsum.ap[:]), reads=[wsum], writes=[wsum]))
                ops.append(lambda: S.op("dve", lambda e: e.tensor_scalar(gtm.ap[:, tile_, 0:64], wsel.ap[:], wsum.ap[:, 0:1], 2.5, ALU.mult, ALU.mult),
                                        reads=[wsel, wsum], writes=[gtm_t[tile_]]))
                return ops
            for pair in range(2):
                interleave([tile_ops(pair * 2), tile_ops(pair * 2 + 1)])
        for qt in range(4):
            quarter(qt)
    if stage == 5:
        o_x1 = dout("o_x1", [NOWN, D])
        o_g = dout("o_g", [128, 16 * 65])
        S.dma("sp", o_x1, x1_d, reads=x1_t, writes=[S.tr("o1")])
        S.dma("sp", o_g, gtm.ap[:].rearrange("p a b -> p (a b)"), reads=gtm_t, writes=[S.tr("o2")])
        o_h2 = dout("o_h2", [128, 8 * NOWN], BF16)
        S.dma("sp", o_h2.rearrange("p (k t) -> p k t", k=8), hT.ap[:, :, 256:256 + NOWN], reads=hT_t, writes=[S.tr("o3")])
        S.finish()
        return nc

    outer = S.scope()
    outer.__enter__()
    y2 = S.sb("y2", [128, 16, D], F32)
    y2_t = [S.tr("y2_%d" % i) for i in range(16)]
    with S.scope():
        STG = Stage(S, 4, 2048, "p6")
        Wg = [S.sb("Wg%d" % i, [128, 8, 256], BF16) for i in range(2)]
        Wu = [S.sb("Wu%d" % i, [128, 8, 256], BF16) for i in range(2)]
        Wd = [S.sb("Wd%d" % i, [128, 2, D], BF16) for i in range(2)]
        sa = [S.sb("sa%d" % i, [128, 512], F32) for i in range(4)]
        actT = [S.sb("actT%d" % i, [128, 2, 512], BF16) for i in range(3)]

        def load_expert(e):
            b = e % 2
            STG.load_cast(w_eg[e].rearrange("(k p) n -> p k n", p=128), 2048, Wg[b].ap[:], Wg[b])
            STG.load_cast(w_eu[e].rearrange("(k p) n -> p k n", p=128), 2048, Wu[b].ap[:], Wu[b])
            STG.load_cast(w_ed[e].rearrange("(k p) n -> p k n", p=128), 2048, Wd[b].ap[:], Wd[b])

        NE = 65
        uctx = {}

        def u0_ops(u):
            e, tb = u // 4, u % 4
            b = e % 2
            pp = []
            uctx[u] = pp
            ops = []
            for i in range(8):
                def chunk(i=i):
                    fc, sub = i // 4, i % 4
                    if sub == 0:
                        pp.append((PS.reserve(), PS.reserve()))
                    psa, psu = pp[fc]
                    dstp = psa if sub < 2 else psu
                    W = Wg[b] if sub < 2 else Wu[b]
                    k0 = (sub % 2) * 4

                    def mm(en):
                        for k in range(k0, k0 + 4):
                            ins = en.matmul(dstp.ap[:], W.ap[:, k, fc * 128:(fc + 1) * 128], hT.ap[:, k, 256 + tb * 512:256 + (tb + 1) * 512],
                                            start=(k == 0), stop=(k == 7))
                        return ins
                    S.op("pe", mm, reads=[W] + hT_t[2 + tb * 4:2 + tb * 4 + 4], writes=[dstp])
                ops.append(chunk)
            return ops

        def u1(u):
            pp = uctx.pop(u)
            A = actT[u % 3]
            for fc in range(2):
                psa, psu = pp[fc]
                s_ = sa[(u % 2) * 2 + fc]
                S.op("act", lambda en, psa=psa, s_=s_: en.activation(s_.ap[:], psa.ap[:], AF.Silu), reads=[psa], writes=[s_])
                S.op("dve", lambda en, psu=psu, s_=s_, fc=fc: en.tensor_tensor(A.ap[:, fc, :], s_.ap[:], psu.ap[:], ALU.mult),
                     reads=[s_, psu], writes=[A])
                PS.release(psa)
                PS.release(psu)

        def u2_ops(u):
            e, tb = u // 4, u % 4
            b = e % 2
            A = actT[u % 3]
            ops = []
            for tt in range(4):
                for dh in range(2):
                    def dn(tt=tt, dh=dh):
                        tile_ = tb * 4 + tt
                        psd = PS.get()

                        def mmd(en):
                            for fc in range(2):
                                ins = en.matmul(psd.ap[:], A.ap[:, fc, tt * 128:(tt + 1) * 128], Wd[b].ap[:, fc, dh * 512:(dh + 1) * 512],
                                                start=(fc == 0), stop=(fc == 1))
                            return ins
                        S.op("pe", mmd, reads=[A, Wd[b]], writes=[psd])
                        dst = y2.ap[:, tile_, dh * 512:(dh + 1) * 512]
                        gcol = gtm.ap[:, tile_, e:e + 1]
                        if e == 0:
                            S.op("act", lambda en: en.activation(dst, psd.ap[:], AF.Identity, scale=gcol),
                                 reads=[psd, gtm_t[tile_]], writes=[y2_t[tile_]])
                        else:
                            S.op("dve", lambda en: en.scalar_tensor_tensor(dst, psd.ap[:], gcol, dst, ALU.mult, ALU.add),
                                 reads=[psd, gtm_t[tile_], y2_t[tile_]], writes=[y2_t[tile_]])
                    ops.append(dn)
            return ops
        load_expert(0)
        NU = NE * 4
        for step in range(NU + 2):
            if 0 <= step - 1 < NU:
                u1(step - 1)
            la = u2_ops(step - 2) if 0 <= step - 2 < NU else []
            lb = u0_ops(step) if step < NU else []
            interleave([la, lb])
            if step < NU and step % 4 == 1 and step // 4 + 1 < NE:
                load_expert(step // 4 + 1)

    with S.scope():
        bc7 = S.sb("bc7", [128, 3, D], F32)
        bc7_t = S.tr("bc7t")
        S.dma("sp", bc7.ap[:, 0, :], mod_d[0:1, 5120:6144].partition_broadcast(128), writes=[bc7_t])
        S.dma("sp", bc7.ap[:, 1, :], lnp[2:3, :].partition_broadcast(128), writes=[bc7_t])
        S.dma("sp", bc7.ap[:, 2, :], lnp[3:4, :].partition_broadcast(128), writes=[bc7_t])
        tmp7s = [S.sb("tmp7_%d" % i, [128, D], F32) for i in range(2)]
        r7s = [S.sb("r7_%d" % i, [128, D], F32) for i in range(2)]
        o7 = [S.sb("o7_%d" % i, [128, D], F32) for i in range(2)]
        out_t = S.tr("out")
        def fin_ops(tile_):
            bs = tile_ % 2
            xt = xts[bs]
            tmp7 = tmp7s[bs]
            r7 = r7s[bs]
            ob = o7[bs]
            st_, mv_, rstd_, nmr_, _x = work[bs]
            ops = []

            def add(*a_, **k_):
                ops.append(lambda: S.op(*a_, **k_))
            ops.append(lambda: S.dma("sp", xt.ap[:], x1_d[tile_ * 128:(tile_ + 1) * 128, :], reads=[x1_t[tile_]], writes=[xt]))
            add("dve", lambda e: e.tensor_tensor(tmp7.ap[:], y2.ap[:, tile_, :], bc7.ap[:, 0, :], ALU.mult),
                reads=[y2_t[tile_], bc7_t], writes=[tmp7])
            add("dve", lambda e: e.scalar_tensor_tensor(r7.ap[:], xt.ap[:], ALPHA, tmp7.ap[:], ALU.mult, ALU.add),
                reads=[xt, tmp7], writes=[r7])
            for i in range(2):
                add("dve", lambda e, i=i: e.bn_stats(st_.ap[:, i, :], r7.ap[:, i * 512:(i + 1) * 512]), reads=[r7], writes=[st_])
            add("dve", lambda e: e.bn_aggr(mv_.ap[:], st_.ap[:].rearrange("p a b -> p (a b)")), reads=[st_], writes=[mv_])
            add("act", lambda e: e.activation(rstd_.ap[:], mv_.ap[:, 1:2], AF.Sqrt, bias=EPS, scale=1.0), reads=[mv_], writes=[rstd_])
            add("dve", lambda e: e.reciprocal(rstd_.ap[:], rstd_.ap[:]), reads=[rstd_], writes=[rstd_])
            add("dve", lambda e: e.scalar_tensor_tensor(nmr_.ap[:], mv_.ap[:, 0:1], -1.0, rstd_.ap[:], ALU.mult, ALU.mult),
                reads=[mv_, rstd_], writes=[nmr_])
            add("act", lambda e: e.activation(r7.ap[:], r7.ap[:], AF.Identity, bias=nmr_.ap[:], scale=rstd_.ap[:]),
                reads=[r7, nmr_, rstd_], writes=[r7])
            add("dve", lambda e: e.tensor_tensor(tmp7.ap[:], r7.ap[:], bc7.ap[:, 1, :], ALU.mult), reads=[r7, bc7_t], writes=[tmp7])
            add("dve", lambda e: e.tensor_tensor(ob.ap[:], tmp7.ap[:], bc7.ap[:, 2, :], ALU.add), reads=[tmp7, bc7_t], writes=[ob])
            ops.append(lambda: S.dma("sp", out_d[tile_ * 128:(tile_ + 1) * 128, :], ob.ap[:], reads=[ob], writes=[out_t]))
            return ops
        for pair in range(8):
            interleave([fin_ops(2 * pair), fin_ops(2 * pair + 1)])
    outer.__exit__(None, None, None)
    S.finish()
    return nc


def _consts():
    ident = np.eye(128, dtype=np.float32)
    Rm = np.zeros((128, 128), np.float32)
    for m in range(128):
        w = m % 32
        if w < 16:
            Rm[m + 16, m] = -1.0
        else:
            Rm[m - 16, m] = 1.0
    j = np.arange(128)[:, None]
    i = np.arange(128)[None, :]
    valid = (j < 64) & (i < 64)
    mF = (valid & (j <= i)).astype(np.float32)[:, 0:64]
    mB = (valid & (j >= i)).astype(np.float32)[:, 0:64]
    cb = np.concatenate([ident, Rm] + [mF, mB] * 4, axis=1).astype(ml_dtypes.bfloat16)
    return cb, ident


def col_layout(v):
    return np.ascontiguousarray(v.reshape(-1, 128).T)


def prep_inputs(inp, cid):
    b, j = cid // 4, cid % 4
    x = inp["x"][b]
    tok0 = 2048 * j
    m = {}
    m["x_own"] = np.ascontiguousarray(x[tok0:tok0 + 2048])
    halo = np.zeros((512, D), np.float32)
    if j > 0:
        halo[0:256] = x[tok0 - 256:tok0]
    if j < 3:
        halo[256:512] = x[tok0 + 2048:tok0 + 2048 + 256]
    m["x_halo"] = halo
    ctx = inp["ctx"][b]
    m["ctx2"] = np.ascontiguousarray(np.concatenate([ctx, ctx[::-1]], axis=0))
    segs = []
    wfs = []
    lbrows = [inp["hg_lb_fwd"][0], inp["hg_lb_fwd"][1], inp["hg_lb_bwd"][0], inp["hg_lb_bwd"][1]]
    fl = np.zeros((128, 6), np.float32)
    w_in = inp["w_in"][0]
    order = [("f", s) for s in range(0, j)] + [("b", s) for s in range(3, j, -1)]
    for si, (d_, s) in enumerate(order):
        seg = x[2048 * s:2048 * (s + 1)]
        if d_ == "f":
            segs.append(seg)
            wfs.append(w_in[:, 1024:2048])
            lbrows += [inp["hg_lb_fwd"][0], inp["hg_lb_fwd"][1]]
            fl[:, si] = 1.0
        else:
            segs.append(seg[::-1])
            wfs.append(w_in[:, 2048:3072])
            lbrows += [inp["hg_lb_bwd"][0], inp["hg_lb_bwd"][1]]
            fl[:, 3 + si] = 1.0
    m["x_slots"] = np.ascontiguousarray(np.concatenate(segs, axis=0))
    m["wf_slots"] = np.ascontiguousarray(np.stack(wfs))
    lbr = np.stack(lbrows)
    m["lbT"] = np.ascontiguousarray(lbr.reshape(10, 8, 128).transpose(2, 0, 1).reshape(128, 80))
    m["flags"] = fl
    cv = np.stack([inp["c"][b], inp["c_ctx"]])
    m["cvecT"] = np.ascontiguousarray(cv.reshape(2, 8, 128).transpose(2, 1, 0).reshape(128, 16))
    m["w_ada"] = inp["w_ada"][0]
    m["b_row"] = inp["b_ada"][0:1]
    m["b_col"] = col_layout(inp["b_ada"][0])
    m["w_in"] = w_in
    m["hgT"] = col_layout(inp["hg_norm_g"][0])
    m["w_a"] = inp["w_branch_a"][0]
    m["w_b"] = inp["w_branch_b"][0]
    m["w_o"] = inp["w_out"][0]
    m["lnp"] = np.ascontiguousarray(np.stack([inp["ln1_g"][0], inp["ln1_b"][0], inp["ln2_g"][0], inp["ln2_b"][0]]))
    m["w_r"] = inp["w_router"][0]
    m["rbias"] = inp["router_bias"][0:1]
    m["w_eg"] = inp["_w_eg"]
    m["w_eu"] = inp["_w_eu"]
    m["w_ed"] = inp["_w_ed"]
    t = np.arange(NKB)
    g = 2048 * j - 256 + t
    row = np.floor_divide(g, 64).astype(np.float32)
    col = np.mod(g, 64).astype(np.float32)
    inv = np.power(np.float32(10000.0), -np.arange(0, 32, 2, dtype=np.float32) / np.float32(32)).astype(np.float32)
    dd = np.arange(64)
    pos = np.where((dd < 32)[:, None], row[None, :], col[None, :]).astype(np.float32)
    ang = (pos * inv[dd % 16][:, None]).astype(np.float32)
    m["cosT"] = np.ascontiguousarray(np.tile(np.cos(ang).astype(np.float32), (2, 1)))
    m["sinT"] = np.ascontiguousarray(np.tile(np.sin(ang).astype(np.float32), (2, 1)))
    rpb = inp["na_rpb"][0]
    qq = np.arange(64)[:, None]
    kc = np.arange(64)[None, :]
    cs = np.clip(qq - 8, 0, 48)
    inwin = (kc >= cs) & (kc < cs + 16)
    dc = np.clip(kc - qq + 15, 0, 30)
    NEG = np.float32(-1e30)

    def Bfor(head, drs):
        out = np.full((64, len(drs), 64), NEG, np.float32)
        for s_, dr in enumerate(drs):
            if dr is not None and 0 <= dr <= 14:
                out[:, s_, :] = np.where(inwin, rpb[head, dr][dc], NEG)
        return out.reshape(64, -1)
    Bint = np.zeros((8, 128, 512), np.float32)
    Bspec = np.zeros((8, 128, 7, 768), np.float32)
    for pr in range(8):
        for hh in range(2):
            head = pr * 2 + hh
            Bint[pr, hh * 64:(hh + 1) * 64] = Bfor(head, list(range(3, 11)))
            for si, lr in enumerate([0, 1, 2, 3, 29, 30, 31]):
                r = 32 * j + lr
                rs_ = int(np.clip(r - 4, 0, 120))
                rel0 = 0 if lr < 4 else 27
                drs = []
                for s_ in range(12):
                    kg = 32 * j - 4 + rel0 + s_
                    drs.append(kg - r + 7 if rs_ <= kg < rs_ + 8 else None)
                Bspec[pr, hh * 64:(hh + 1) * 64, si] = Bfor(head, drs)
    m["Bint"] = Bint
    m["Bspec"] = np.ascontiguousarray(Bspec.reshape(8, 128, 7 * 768))
    cb, ident = _consts()
    m["consts_bf"] = cb
    m["ident_f"] = ident
    return m


_CACHE = {}


def kernel(**inputs):
    inp = {k: np.asarray(v) for k, v in inputs.items()}
    inp["_w_eg"] = np.ascontiguousarray(np.concatenate([inp["w_e_gate"][0], inp["w_sh_gate"]], axis=0))
    inp["_w_eu"] = np.ascontiguousarray(np.concatenate([inp["w_e_up"][0], inp["w_sh_up"]], axis=0))
    inp["_w_ed"] = np.ascontiguousarray(np.concatenate([inp["w_e_down"][0], inp["w_sh_down"]], axis=0))
    if "nc" not in _CACHE:
        _CACHE["nc"] = build_program(99)
    nc = _CACHE["nc"]
    maps = [prep_inputs(inp, c) for c in range(8)]
    res = run_bass_kernel_spmd(nc, maps, core_ids=list(range(8)))
    out = np.zeros((2, 8192, D), np.float32)
    for c in range(8):
        b, j = c // 4, c % 4
        out[b, 2048 * j:2048 * (j + 1)] = res.results[c]["out"]
    return out
```
